# Optimizing a Trainium2 kernel written in Bass

```python
import jax, jax.numpy as jnp
from jax import lax
import numpy as np

D_MODEL = 1024
BATCH = 4
SEQ = 8192
DEPTH = 2

D_MIX = D_MODEL
SB_WIDTH = D_MIX // 2
CONV_WIDTH = D_MIX - SB_WIDTH
SB_HEAD_DIM = 64
SB_HEADS = SB_WIDTH // SB_HEAD_DIM
CONV_K = 3
Q_BLOCK = 128
D_FF = 7 * D_MODEL // 2
N_EXPERTS = 8
TOP_K = 2
D_PLE = 256
N_DENSE = (DEPTH + 1) // 2
N_MOE = DEPTH // 2
D_IN_PROJ = 3 * SB_WIDTH + 3 * CONV_WIDTH
EPS = 1e-6

kernel_name = "hybrid_stickbreak_shortconv_moe_ple"


def rmsnorm(x, g):
    xf = x.astype(jnp.float32)
    y = xf * lax.rsqrt(jnp.mean(xf * xf, axis=-1, keepdims=True) + EPS)
    return (y * g.astype(jnp.float32)).astype(x.dtype)


def stick_breaking_attention(q, k, v):
    bsz, nh, s_len, dh = q.shape
    dtype = q.dtype
    qf = q.astype(jnp.float32)
    kf = k.astype(jnp.float32)
    vf = v.astype(jnp.float32)
    scale = 1.0 / np.sqrt(dh).astype(np.float32)
    n_blocks = s_len // Q_BLOCK
    q_blocks = qf.reshape(bsz, nh, n_blocks, Q_BLOCK, dh).transpose(2, 0, 1, 3, 4)
    key_pos = jnp.arange(s_len)

    def one_block(args):
        q_blk, blk_idx = args
        z = jnp.einsum('bhqd,bhkd->bhqk', q_blk, kf) * scale
        q_pos = blk_idx * Q_BLOCK + jnp.arange(Q_BLOCK)
        causal = key_pos[None, :] < q_pos[:, None]
        log_beta = jax.nn.log_sigmoid(z)
        log_one_minus = jnp.where(causal, jax.nn.log_sigmoid(-z), 0.0)
        later = lax.cumsum(log_one_minus, axis=3, reverse=True) - log_one_minus
        weights = jnp.where(causal, jnp.exp(log_beta + later), 0.0)
        return jnp.einsum('bhqk,bhkd->bhqd', weights, vf)

    out = lax.map(one_block, (q_blocks, jnp.arange(n_blocks)))
    out = out.transpose(1, 2, 0, 3, 4).reshape(bsz, nh, s_len, dh)
    return out.astype(dtype)


def causal_depthwise_conv(u, w, bias):
    ch = u.shape[-1]
    rhs = w.reshape(CONV_K, 1, ch).astype(u.dtype)
    out = lax.conv_general_dilated(
        u, rhs, window_strides=(1,), padding=[(CONV_K - 1, 0)],
        dimension_numbers=('NWC', 'WIO', 'NWC'), feature_group_count=ch)
    return out + bias.astype(u.dtype)


def swiglu(x, w1, w3, w2):
    return (jax.nn.silu(x @ w1) * (x @ w3)) @ w2


def moe_swiglu(x, router_w, w1, w3, w2):
    bsz, s_len, d = x.shape
    xt = x.reshape(bsz * s_len, d)
    logits = (xt @ router_w).astype(jnp.float32)
    top_vals, top_idx = lax.top_k(logits, TOP_K)
    gates = jax.nn.softmax(top_vals, axis=-1)
    combine = jnp.sum(jax.nn.one_hot(top_idx, N_EXPERTS, dtype=jnp.float32)
                      * gates[..., None], axis=1).astype(x.dtype)
    out = jnp.zeros_like(xt)
    for e in range(N_EXPERTS):
        out = out + combine[:, e:e + 1] * swiglu(xt, w1[e], w3[e], w2[e])
    return out.reshape(bsz, s_len, d)


def setup_inputs(seed: int = 0) -> dict:
    key = jax.random.key(seed)
    ks = jax.random.split(key, 24)

    def nrm(k, shape, scale):
        return jax.random.normal(k, shape, jnp.float32) * scale

    def gain(k, shape):
        return 1.0 + 0.02 * jax.random.normal(k, shape, jnp.float32)

    return {
        "x": nrm(ks[0], (BATCH, SEQ, D_MODEL), 1.0),
        "p": nrm(ks[1], (DEPTH, BATCH, SEQ, D_PLE), 1.0),
        "mix_norm_g": gain(ks[2], (DEPTH, D_MODEL)),
        "w_in": nrm(ks[3], (DEPTH, D_MODEL, D_IN_PROJ), D_MODEL ** -0.5),
        "q_norm_g": gain(ks[4], (DEPTH, SB_HEAD_DIM)),
        "k_norm_g": gain(ks[5], (DEPTH, SB_HEAD_DIM)),
        "conv_w": nrm(ks[6], (DEPTH, CONV_K, CONV_WIDTH), CONV_K ** -0.5),
        "conv_b": nrm(ks[7], (DEPTH, CONV_WIDTH), 0.02),
        "attn_out_g": gain(ks[8], (DEPTH, SB_WIDTH)),
        "conv_out_g": gain(ks[9], (DEPTH, CONV_WIDTH)),
        "w_o": nrm(ks[10], (DEPTH, D_MIX, D_MODEL), D_MIX ** -0.5),
        "ffn_norm_g": gain(ks[11], (DEPTH, D_MODEL)),
        "dense_w1": nrm(ks[12], (N_DENSE, D_MODEL, D_FF), D_MODEL ** -0.5),
        "dense_w3": nrm(ks[13], (N_DENSE, D_MODEL, D_FF), D_MODEL ** -0.5),
        "dense_w2": nrm(ks[14], (N_DENSE, D_FF, D_MODEL), D_FF ** -0.5),
        "router_w": nrm(ks[15], (N_MOE, D_MODEL, N_EXPERTS), D_MODEL ** -0.5),
        "moe_w1": nrm(ks[16], (N_MOE, N_EXPERTS, D_MODEL, D_FF), D_MODEL ** -0.5),
        "moe_w3": nrm(ks[17], (N_MOE, N_EXPERTS, D_MODEL, D_FF), D_MODEL ** -0.5),
        "moe_w2": nrm(ks[18], (N_MOE, N_EXPERTS, D_FF, D_MODEL), D_FF ** -0.5),
        "ple_norm_g": gain(ks[19], (DEPTH, D_MODEL)),
        "ple_gate_w": nrm(ks[20], (DEPTH, D_MODEL, D_MODEL), D_MODEL ** -0.5),
        "ple_proj_w": nrm(ks[21], (DEPTH, D_PLE, D_MODEL), D_PLE ** -0.5),
    }


def reference(x, p, mix_norm_g, w_in, q_norm_g, k_norm_g, conv_w, conv_b,
              attn_out_g, conv_out_g, w_o, ffn_norm_g, dense_w1, dense_w3, dense_w2,
              router_w, moe_w1, moe_w3, moe_w2, ple_norm_g, ple_gate_w, ple_proj_w):
    bsz, s_len, _ = x.shape
    h = x
    for i in range(DEPTH):
        a = rmsnorm(h, mix_norm_g[i])
        proj = a @ w_in[i]
        q, k, v = (proj[..., 0:SB_WIDTH],
                   proj[..., SB_WIDTH:2 * SB_WIDTH],
                   proj[..., 2 * SB_WIDTH:3 * SB_WIDTH])
        off = 3 * SB_WIDTH
        u, c_gate, b_gate = (proj[..., off:off + CONV_WIDTH],
                             proj[..., off + CONV_WIDTH:off + 2 * CONV_WIDTH],
                             proj[..., off + 2 * CONV_WIDTH:off + 3 * CONV_WIDTH])

        def heads(t):
            return t.reshape(bsz, s_len, SB_HEADS, SB_HEAD_DIM).transpose(0, 2, 1, 3)
        qh = rmsnorm(heads(q), q_norm_g[i])
        kh = rmsnorm(heads(k), k_norm_g[i])
        vh = heads(v)
        attn = stick_breaking_attention(qh, kh, vh)
        attn = attn.transpose(0, 2, 1, 3).reshape(bsz, s_len, SB_WIDTH)

        conv = b_gate * causal_depthwise_conv(c_gate * u, conv_w[i], conv_b[i])

        mixed = jnp.concatenate([rmsnorm(attn, attn_out_g[i]),
                                 rmsnorm(conv, conv_out_g[i])], axis=-1)
        h = h + mixed @ w_o[i]

        f = rmsnorm(h, ffn_norm_g[i])
        if i % 2 == 0:
            j = i // 2
            h = h + swiglu(f, dense_w1[j], dense_w3[j], dense_w2[j])
        else:
            j = i // 2
            h = h + moe_swiglu(f, router_w[j], moe_w1[j], moe_w3[j], moe_w2[j])

        gate = jax.nn.sigmoid(rmsnorm(h, ple_norm_g[i]) @ ple_gate_w[i])
        h = h + gate * (p[i] @ ple_proj_w[i])
    return h
```

```python
import numpy as np
import ml_dtypes
import concourse.bass as bass
import concourse.mybir as mybir
from concourse.bass_utils import run_bass_kernel_spmd

F32 = mybir.dt.float32
BF16 = mybir.dt.bfloat16
AF = mybir.ActivationFunctionType
ALU = mybir.AluOpType

D = 1024
T = 4096
NB = 8
TB = 512
DFF = 3584
NFF = 28
NE = 8
EPS = 1e-6
NCORES = 8

V_MIX = 0
V_FFN = 8
V_PLE = 16
V_ATT = 24
V_CVG = 28
V_CW = 32
V_CB = 44
V_QG = 48
V_KG = 49
NV = 50

C_MNEG = 0
C_M2NEG = 128
C_ONES = 256
C_BLK = 384
C_IDENT = 512
C_MASK = 640
NCONST = 640 + 2048


class SemC:
    def __init__(self, sem, unit):
        self.sem = sem
        self.unit = unit
        self.count = 0
        self.retired = False


class Buf:
    __slots__ = ("name", "last_w", "readers")

    def __init__(self, name):
        self.name = name
        self.last_w = None
        self.readers = {}


class Tracker:
    ENG = ("tensor", "scalar", "vector", "gpsimd", "sync")

    def __init__(self, nc):
        self.nc = nc
        self.streams = {e: [] for e in self.ENG}
        self.sc = {}
        self.waited = {e: {} for e in self.ENG}
        self.dma_sems = []
        self.npartial = 0
        self._dcache = {}
        self._cms = []
        self.epoch = 0
        for e in self.ENG:
            self._new_eng_sem(e)

    def _new_eng_sem(self, e):
        cm = self.nc.semaphore("s_%s_%d" % (e, self.epoch))
        s = cm.__enter__()
        self._cms.append(cm)
        self.sc[e] = SemC(s, 1)

    def maybe_epoch(self, limit=20000):
        if max(self.sc[e].count for e in self.ENG) < limit:
            return
        self.barrier()
        self.epoch += 1
        for e in self.ENG:
            self.sc[e].retired = True
            self._new_eng_sem(e)

    def new_dma_sem(self, name):
        if name in self._dcache:
            return self._dcache[name]
        cm = self.nc.semaphore("d_" + name)
        s = cm.__enter__()
        self._cms.append(cm)
        sc = SemC(s, 16)
        self.dma_sems.append(sc)
        self._dcache[name] = sc
        return sc

    def close(self):
        for cm in reversed(self._cms):
            cm.__exit__(None, None, None)

    def emit(self, eng, fn, reads=(), writes=(), dsem=None):
        deps = []
        for b in reads:
            if b.last_w is not None:
                deps.append(b.last_w)
        for b in writes:
            if b.last_w is not None:
                deps.append(b.last_w)
            deps.extend(b.readers.items())
        own = self.sc[eng]
        waits = {}
        wd = self.waited[eng]
        for sc, val in deps:
            if sc.retired:
                continue
            if sc is own and eng == "tensor" and dsem is None:
                continue
            if wd.get(sc, 0) >= val:
                continue
            if waits.get(sc, 0) < val:
                waits[sc] = val
        st = self.streams[eng]
        for sc, val in waits.items():
            if sc.unit == 16 and val < sc.count:
                self.npartial += 1
                val = sc.count
            wd[sc] = val
            st.append(("w", sc.sem, val))
        sc = dsem if dsem is not None else own
        sc.count += sc.unit
        val = sc.count
        st.append(("i", fn, sc.sem, sc.unit))
        for b in reads:
            if b.readers.get(sc, 0) < val:
                b.readers[sc] = val
        for b in writes:
            b.last_w = (sc, val)
            b.readers = {}

    def barrier(self):
        allsc = [self.sc[e] for e in self.ENG] + self.dma_sems
        for e in self.ENG:
            wd = self.waited[e]
            for sc in allsc:
                if sc.retired or (sc is self.sc[e] and e == "tensor"):
                    continue
                if sc.count > 0 and wd.get(sc, 0) < sc.count:
                    wd[sc] = sc.count
                    self.streams[e].append(("w", sc.sem, sc.count))

    def replay(self, eng_name, e):
        for it in self.streams[eng_name]:
            if it[0] == "w":
                e.wait_ge(it[1], it[2])
            else:
                it[1](e).then_inc(it[2], it[3])


class Ctx:
    pass


_UID = [0]


def _alloc(K, stack, kind, name, shape, dt):
    _UID[0] += 1
    name = "%s_u%d" % (name, _UID[0])
    cm = (K.nc.sbuf_tensor if kind == "s" else K.nc.psum_tensor)(name, shape, dt)
    t = cm.__enter__()
    stack.append(cm)
    return t


def _free(stack):
    while stack:
        stack.pop().__exit__(None, None, None)


def build_fused():
    nc = bass.Bass("TRN2", target_bir_lowering=False)
    K = Ctx()
    K.nc = nc
    dr = {}

    def dram(name, shape, dt, kind):
        dr[name] = nc.dram_tensor(name, list(shape), dt, kind=kind).ap()
        return dr[name]

    dram("consts", [128, NCONST], F32, "ExternalInput")
    dram("is2", [128, 1], F32, "ExternalInput")
    dram("vecs", [128, 2 * NV], F32, "ExternalInput")
    dram("xT", [D, 2 * T], F32, "ExternalInput")
    dram("pT0", [256, 2 * T], F32, "ExternalInput")
    dram("pT1", [256, T], F32, "ExternalInput")
    dram("w_in", [2, D, 3072], F32, "ExternalInput")
    dram("w_o", [2, D, D], F32, "ExternalInput")
    dram("ple_gate_w", [2, D, D], F32, "ExternalInput")
    dram("ple_proj_w", [2, 256, D], F32, "ExternalInput")
    dram("dw1", [1, D, DFF], F32, "ExternalInput")
    dram("dw3", [1, D, DFF], F32, "ExternalInput")
    dram("dw2", [1, DFF, D], F32, "ExternalInput")
    dram("mw1", [NE, D, DFF], F32, "ExternalInput")
    dram("mw3", [NE, D, DFF], F32, "ExternalInput")
    dram("mw2", [NE, DFF, D], F32, "ExternalInput")
    dram("router_w", [D, NE], F32, "ExternalInput")
    dram("h1T", [D, 2 * T], F32, "Internal")
    dram("qT", [128, 4, 2 * T], BF16, "Internal")
    dram("kT", [128, 4, 2 * T], BF16, "Internal")
    dram("V", [2 * T, 512], BF16, "Internal")
    dram("yT", [128, 4, 2 * T], BF16, "Internal")
    dram("attnT", [128, 4, 2 * T], BF16, "Internal")
    dram("hTo", [D, T], F32, "ExternalOutput")

    tr = Tracker(nc)
    K.tr = tr
    K.dr = dr
    K.dbuf = {}

    def DB(name, blk=0):
        k = (name, blk)
        if k not in K.dbuf:
            K.dbuf[k] = Buf("%s_%s" % k)
        return K.dbuf[k]
    K.DB = DB

    base = []
    K.PS = _alloc(K, base, "p", "psall", [128, 8, 512], F32)
    K.P = [K.PS[:, i, :] for i in range(8)]
    K.PB = [Buf("ps%d" % i) for i in range(8)]
    K.cst = _alloc(K, base, "s", "cst", [128, NCONST], F32)
    K.cstb = _alloc(K, base, "s", "cstb", [128, 512], BF16)
    K.vec2 = _alloc(K, base, "s", "vec2", [128, 2 * NV], F32)
    K.is2 = _alloc(K, base, "s", "is2s", [128, 1], F32)
    K.Bc = Buf("consts")
    sem_c = tr.new_dma_sem("c")
    tr.emit("sync", lambda e: e.dma_start(out=K.cst[:], in_=dr["consts"][:, :]), writes=[K.Bc], dsem=sem_c)
    tr.emit("sync", lambda e: e.dma_start(out=K.vec2[:], in_=dr["vecs"][:, :]), writes=[K.Bc], dsem=sem_c)
    tr.emit("sync", lambda e: e.dma_start(out=K.is2[:], in_=dr["is2"][:, :]), writes=[K.Bc], dsem=sem_c)
    tr.emit("vector", lambda e: e.tensor_copy(out=K.cstb[:], in_=K.cst[:, 0:512]), reads=[K.Bc], writes=[K.Bc])

    for layer in range(2):
        K.vec = K.vec2[:, layer * NV:(layer + 1) * NV]
        L = Ctx()
        L.layer = layer
        L.hsrc = dr["xT"] if layer == 0 else dr["h1T"]
        L.w_in = dr["w_in"][layer]
        L.qblocks = list(range(16)) if layer == 0 else list(range(8, 16))
        L.blocks = L.qblocks
        L.hdst = dr["h1T"] if layer == 0 else dr["hTo"]
        L.o_off = 0 if layer == 0 else 8
        L.pT = dr["pT0"] if layer == 0 else dr["pT1"]
        L.p_off = 0 if layer == 0 else 8
        L.w_o = dr["w_o"][layer]
        L.wg = dr["ple_gate_w"][layer]
        L.wp = dr["ple_proj_w"][layer]
        if layer == 0:
            L.w1, L.w3, L.w2 = dr["dw1"], dr["dw3"], dr["dw2"]
        else:
            L.w1, L.w3, L.w2 = dr["mw1"], dr["mw3"], dr["mw2"]
        phase_A(K, L)
        tr.barrier()
        phase_B1(K, L)
        tr.barrier()
        phase_B2(K, L)
        tr.barrier()

    with nc.Block() as block:
        @block.sync
        def _(e):
            tr.replay("sync", e)

        @block.scalar
        def _(e):
            tr.replay("scalar", e)

        @block.vector
        def _(e):
            tr.replay("vector", e)

        @block.gpsimd
        def _(e):
            tr.replay("gpsimd", e)

        @block.tensor
        def _(e):
            tr.replay("tensor", e)
    _free(base)
    tr.close()
    return nc


def rms_stats(K, src, srcbuf, nch, ncols, sq, sqbuf, inv_n, dst_rstd, dstbuf, tmp, tmpbuf, pbank, ones_ap):
    tr = K.tr
    P, PB = K.P, K.PB
    for c in range(nch):
        tr.emit("scalar", lambda e, c=c: e.activation(out=sq[:, c, 0:ncols], in_=src[:, c, 0:ncols], func=AF.Square),
                reads=[srcbuf], writes=[sqbuf])
    for c in range(nch):
        tr.emit("tensor", lambda e, c=c: e.matmul(P[pbank][:, 0:ncols], ones_ap, sq[:, c, 0:ncols],
                                                   start=(c == 0), stop=(c == nch - 1)),
                reads=[sqbuf, K.Bc], writes=[PB[pbank]])
    tr.emit("scalar", lambda e: e.activation(out=tmp[:, 0:ncols], in_=P[pbank][:, 0:ncols], func=AF.Ln,
                                             bias=EPS, scale=inv_n),
            reads=[PB[pbank], K.Bc], writes=[tmpbuf])
    tr.emit("scalar", lambda e: e.activation(out=dst_rstd[:, 0:ncols], in_=tmp[:, 0:ncols], func=AF.Exp, scale=-0.5),
            reads=[tmpbuf], writes=[dstbuf])


def setup_eps(K, stack):
    pass


def phase_A(K, L):
    nc, tr, dr, P, PB, DB = K.nc, K.tr, K.dr, K.P, K.PB, K.DB
    st = []
    setup_eps(K, st)
    Win = _alloc(K, st, "s", "Win", [128, 8, 3072], BF16)
    BWin = Buf("Win")
    hb = [_alloc(K, st, "s", "hb%d" % i, [128, 8, TB], F32) for i in range(2)]
    Bhb = [Buf("hb%d" % i) for i in range(2)]
    sq = _alloc(K, st, "s", "sq", [128, 8, TB], BF16)
    Bsq = Buf("sq")
    aT = [_alloc(K, st, "s", "aT%d" % i, [128, 8, TB], BF16) for i in range(2)]
    BaT = [Buf("aT%d" % i) for i in range(2)]
    tmp = _alloc(K, st, "s", "tmpA", [128, TB], F32)
    Btmp = Buf("tmpA")
    sqq = [_alloc(K, st, "s", "sqq%d" % i, [128, 1, TB], BF16) for i in range(2)]
    Bsqq = [Buf("sqq%d" % i) for i in range(2)]
    rsq = [_alloc(K, st, "s", "rsq%d" % i, [128, TB], F32) for i in range(2)]
    Brsq = [Buf("rsq%d" % i) for i in range(2)]
    tmq = [_alloc(K, st, "s", "tmq%d" % i, [128, TB], F32) for i in range(2)]
    Btmq = [Buf("tmq%d" % i) for i in range(2)]
    qb_ = [_alloc(K, st, "s", "qblk%d" % i, [128, 4, TB], BF16) for i in range(2)]
    Bqb = [Buf("qblk%d" % i) for i in range(2)]
    kb_ = [_alloc(K, st, "s", "kblk%d" % i, [128, 4, TB], BF16) for i in range(2)]
    Bkb = [Buf("kblk%d" % i) for i in range(2)]
    vb_ = [_alloc(K, st, "s", "vblk%d" % i, [128, 4, 512], BF16) for i in range(2)]
    Bvb = [Buf("vblk%d" % i) for i in range(2)]
    usb = [_alloc(K, st, "s", "usb%d" % i, [128, TB], F32) for i in range(2)]
    Busb = [Buf("usb%d" % i) for i in range(2)]
    cu = _alloc(K, st, "s", "cu", [128, 4, TB + 2], F32)
    Bcu = Buf("cu")
    acc = [_alloc(K, st, "s", "acc%d" % i, [128, TB], F32) for i in range(2)]
    Bacc = [Buf("acc%d" % i) for i in range(2)]
    y = _alloc(K, st, "s", "y", [128, 4, TB], F32)
    By = Buf("y")
    sqy = _alloc(K, st, "s", "sqy", [128, 4, TB], BF16)
    Bsqy = Buf("sqy")
    rsy = _alloc(K, st, "s", "rsy", [128, TB], F32)
    Brsy = Buf("rsy")
    yn = [_alloc(K, st, "s", "yn%d" % i, [128, 4, TB], BF16) for i in range(2)]
    Byn = [Buf("yn%d" % i) for i in range(2)]

    ones_bf = K.cstb[:, 256:384]
    blk_bf = K.cstb[:, 384:512]
    vec = K.vec

    sW = tr.new_dma_sem("Win")
    wv = L.w_in.rearrange("(c p) n -> p c n", p=128)
    for g in range(6):
        tr.emit("gpsimd", lambda e, g=g: e.dma_start(out=Win[:, :, g * 512:(g + 1) * 512], in_=wv[:, :, g * 512:(g + 1) * 512]),
                writes=[BWin], dsem=sW)

    hv = L.hsrc.rearrange("(c p) t -> p c t", p=128)
    s_h = [tr.new_dma_sem("hb%d" % i) for i in range(2)]
    s_q = [tr.new_dma_sem("q%d" % i) for i in range(2)]
    s_k = [tr.new_dma_sem("k%d" % i) for i in range(2)]
    s_v = [tr.new_dma_sem("v%d" % i) for i in range(2)]
    s_y = [tr.new_dma_sem("y%d" % i) for i in range(2)]

    pr = [0]

    def pbank():
        pr[0] = (pr[0] + 1) % 4
        return 2 + pr[0]

    def norm_and_aT(src, srcbuf, ncols, dst, dstbuf):
        rms_stats(K, src, srcbuf, 8, ncols, sq, Bsq, 1.0 / D, P[1], PB[1], tmp, Btmp, 0, ones_bf)
        for c in range(8):
            tr.emit("vector", lambda e, c=c: e.scalar_tensor_tensor(
                out=dst[:, c, 0:ncols], in0=src[:, c, 0:ncols], scalar=vec[:, V_MIX + c:V_MIX + c + 1],
                in1=P[1][:, 0:ncols], op0=ALU.mult, op1=ALU.mult),
                reads=[srcbuf, PB[1], K.Bc], writes=[dstbuf])

    def proj(col0, a, abuf, ncols):
        pb = pbank()
        for kc in range(8):
            tr.emit("tensor", lambda e, kc=kc, pb=pb: e.matmul(P[pb][:, 0:ncols], Win[:, kc, col0:col0 + 128], a[:, kc, 0:ncols],
                                                               start=(kc == 0), stop=(kc == 7)),
                    reads=[BWin, abuf], writes=[PB[pb]])
        return pb

    tr.emit("vector", lambda e: e.memset(cu[:, :, 0:2], 0.0), writes=[Bcu])
    for b in range(2 * NB):
        tr.maybe_epoch()
        s = b % 2
        ts = slice(b * TB, (b + 1) * TB)
        tr.emit("sync", lambda e, s=s, ts=ts: e.dma_start(out=hb[s][:], in_=hv[:, :, ts]),
                reads=[DB("hsrc", b)], writes=[Bhb[s]], dsem=s_h[s])
        norm_and_aT(hb[s], Bhb[s], TB, aT[s], BaT[s])
        full = (L.layer == 0) or (b >= NB - 1)
        for which, dst, dbuf, gcol in (("q", qb_[s], Bqb[s], V_QG), ("k", kb_[s], Bkb[s], V_KG)):
            if which == "q" and not full:
                continue
            c0 = 0 if which == "q" else 512
            for c in range(4):
                i2 = c % 2
                pq = proj(c0 + c * 128, aT[s], BaT[s], TB)
                tr.emit("scalar", lambda e, pq=pq, i2=i2: e.activation(out=sqq[i2][:, 0, :], in_=P[pq][:, :], func=AF.Square),
                        reads=[PB[pq]], writes=[Bsqq[i2]])
                tr.emit("tensor", lambda e, i2=i2: e.matmul(P[6 + i2][:, :], blk_bf, sqq[i2][:, 0, :], start=True, stop=True),
                        reads=[Bsqq[i2], K.Bc], writes=[PB[6 + i2]])
                tr.emit("scalar", lambda e, i2=i2: e.activation(out=tmq[i2][:], in_=P[6 + i2][:, :], func=AF.Ln, bias=EPS, scale=1.0 / 64),
                        reads=[PB[6 + i2], K.Bc], writes=[Btmq[i2]])
                tr.emit("scalar", lambda e, i2=i2: e.activation(out=rsq[i2][:], in_=tmq[i2][:], func=AF.Exp, scale=-0.5),
                        reads=[Btmq[i2]], writes=[Brsq[i2]])
                tr.emit("vector", lambda e, pq=pq, i2=i2, c=c, dst=dst, gcol=gcol: e.scalar_tensor_tensor(
                    out=dst[:, c, :], in0=P[pq][:, :], scalar=vec[:, gcol:gcol + 1], in1=rsq[i2][:],
                    op0=ALU.mult, op1=ALU.mult),
                    reads=[PB[pq], Brsq[i2], K.Bc], writes=[dbuf])
        if full:
            tr.emit("sync", lambda e, s=s, ts=ts: e.dma_start(out=dr["qT"][:, :, ts], in_=qb_[s][:]),
                    reads=[Bqb[s]], writes=[DB("qT", b)], dsem=s_q[s])
        tr.emit("sync", lambda e, s=s, ts=ts: e.dma_start(out=dr["kT"][:, :, ts], in_=kb_[s][:]),
                reads=[Bkb[s]], writes=[DB("kT", b)], dsem=s_k[s])
        for tt in range(4):
            pb = pbank()
            for kc in range(8):
                tr.emit("tensor", lambda e, kc=kc, pb=pb, tt=tt, s=s: e.matmul(P[pb][:, :], aT[s][:, kc, tt * 128:(tt + 1) * 128], Win[:, kc, 1024:1536],
                                                                               start=(kc == 0), stop=(kc == 7)),
                        reads=[BWin, BaT[s]], writes=[PB[pb]])
            tr.emit("vector", lambda e, pb=pb, tt=tt, s=s: e.tensor_copy(out=vb_[s][:, tt, :], in_=P[pb][:, :]),
                    reads=[PB[pb]], writes=[Bvb[s]])
        tr.emit("sync", lambda e, s=s, b=b: e.dma_start(out=dr["V"][b * TB:(b + 1) * TB, :].rearrange("(t p) n -> p t n", p=128), in_=vb_[s][:]),
                reads=[Bvb[s]], writes=[DB("V", b)], dsem=s_v[s])
        if not full:
            continue
        for c in range(4):
            i2 = c % 2
            pu = proj(1536 + c * 128, aT[s], BaT[s], TB)
            pc = proj(2048 + c * 128, aT[s], BaT[s], TB)
            pg = proj(2560 + c * 128, aT[s], BaT[s], TB)
            tr.emit("scalar", lambda e, pu=pu, i2=i2: e.activation(out=usb[i2][:], in_=P[pu][:, :], func=AF.Copy),
                    reads=[PB[pu]], writes=[Busb[i2]])
            tr.emit("vector", lambda e, c=c, pc=pc, i2=i2: e.tensor_tensor(out=cu[:, c, 2:TB + 2], in0=P[pc][:, :], in1=usb[i2][:], op=ALU.mult),
                    reads=[PB[pc], Busb[i2]], writes=[Bcu])
            tr.emit("gpsimd", lambda e, c=c, i2=i2: e.tensor_scalar(out=acc[i2][:], in0=cu[:, c, 2:TB + 2],
                                                                     scalar1=vec[:, V_CW + 8 + c:V_CW + 9 + c], scalar2=vec[:, V_CB + c:V_CB + c + 1],
                                                                     op0=ALU.mult, op1=ALU.add),
                    reads=[Bcu, K.Bc], writes=[Bacc[i2]])
            tr.emit("vector", lambda e, c=c, i2=i2: e.scalar_tensor_tensor(out=acc[i2][:], in0=cu[:, c, 1:TB + 1], scalar=vec[:, V_CW + 4 + c:V_CW + 5 + c],
                                                                           in1=acc[i2][:], op0=ALU.mult, op1=ALU.add),
                    reads=[Bcu, Bacc[i2], K.Bc], writes=[Bacc[i2]])
            tr.emit("vector", lambda e, c=c, i2=i2: e.scalar_tensor_tensor(out=acc[i2][:], in0=cu[:, c, 0:TB], scalar=vec[:, V_CW + c:V_CW + 1 + c],
                                                                           in1=acc[i2][:], op0=ALU.mult, op1=ALU.add),
                    reads=[Bcu, Bacc[i2], K.Bc], writes=[Bacc[i2]])
            tr.emit("vector", lambda e, c=c, pg=pg, i2=i2: e.tensor_tensor(out=y[:, c, :], in0=P[pg][:, :], in1=acc[i2][:], op=ALU.mult),
                    reads=[PB[pg], Bacc[i2]], writes=[By])
            if b == NB - 1:
                tr.emit("gpsimd", lambda e, c=c: e.tensor_scalar(out=cu[:, c, 0:2], in0=cu[:, c, TB:TB + 2], scalar1=K.is2[:, 0:1],
                                                                  scalar2=None, op0=ALU.mult),
                        reads=[Bcu, K.Bc], writes=[Bcu])
            else:
                tr.emit("gpsimd", lambda e, c=c: e.tensor_copy(out=cu[:, c, 0:2], in_=cu[:, c, TB:TB + 2]),
                        reads=[Bcu], writes=[Bcu])
        rms_stats(K, y, By, 4, TB, sqy, Bsqy, 1.0 / 512, rsy, Brsy, tmp, Btmp, 6, ones_bf)
        for c in range(4):
            tr.emit("vector", lambda e, c=c, s=s: e.scalar_tensor_tensor(out=yn[s][:, c, :], in0=y[:, c, :], scalar=vec[:, V_CVG + c:V_CVG + c + 1],
                                                                         in1=rsy[:], op0=ALU.mult, op1=ALU.mult),
                    reads=[By, Brsy, K.Bc], writes=[Byn[s]])
        tr.emit("sync", lambda e, s=s, ts=ts: e.dma_start(out=dr["yT"][:, :, ts], in_=yn[s][:]),
                reads=[Byn[s]], writes=[DB("yT", b)], dsem=s_y[s])
    _free(st)


def phase_B1(K, L):
    nc, tr, dr, P, PB, DB = K.nc, K.tr, K.dr, K.P, K.PB, K.DB
    st = []
    setup_eps(K, st)
    NKT = 2 * T // 128
    kTa = _alloc(K, st, "s", "kTa", [128, 4, 2 * T], BF16)
    BkT = Buf("kTa")
    Va = _alloc(K, st, "s", "Va", [128, NKT, 512], BF16)
    BVa = Buf("Va")
    qblk = _alloc(K, st, "s", "qblk", [128, 4, TB], BF16)
    Bq = Buf("qblk")
    E = [_alloc(K, st, "s", "E%d" % i, [128, 2, TB], F32) for i in range(3)]
    BE = [Buf("E%d" % i) for i in range(3)]
    Lp = [_alloc(K, st, "s", "Lp%d" % i, [128, 2, TB], BF16) for i in range(2)]
    BLp = [Buf("Lp%d" % i) for i in range(2)]
    Xe = [_alloc(K, st, "s", "Xe%d" % i, [128, 2, TB], F32) for i in range(2)]
    BXe = [Buf("Xe%d" % i) for i in range(2)]
    Aw = [_alloc(K, st, "s", "Aw%d" % i, [128, 2, TB], BF16) for i in range(2)]
    BAw = [[Buf("Aw%d%d" % (i, ch)) for ch in range(2)] for i in range(2)]
    ablk = _alloc(K, st, "s", "ablk", [128, 4, TB], F32)
    Bab = Buf("ablk")
    sqa = _alloc(K, st, "s", "sqa", [128, 4, TB], BF16)
    Bsqa = Buf("sqa")
    rsa = _alloc(K, st, "s", "rsa", [128, TB], F32)
    Brsa = Buf("rsa")
    tmp = _alloc(K, st, "s", "tmpB", [128, TB], F32)
    Btmp = Buf("tmpB")
    an = [_alloc(K, st, "s", "an%d" % i, [128, 4, TB], BF16) for i in range(2)]
    Ban = [Buf("an%d" % i) for i in range(2)]

    Mneg = K.cstb[:, 0:128]
    M2neg = K.cstb[:, 128:256]
    ones_bf = K.cstb[:, 256:384]
    vec = K.vec

    s_kv = tr.new_dma_sem("kvV")
    s_kk = tr.new_dma_sem("kvK")
    for g in range(4):
        tr.emit("sync", lambda e, g=g: e.dma_start(out=kTa[:, g, :], in_=dr["kT"][:, g, :]),
                reads=[DB("kTall")], writes=[BkT], dsem=s_kk)
    Vv = dr["V"].rearrange("(t p) n -> p t n", p=128)
    for g in range(8):
        tr.emit("sync", lambda e, g=g: e.dma_start(out=Va[:, g * 8:(g + 1) * 8, :], in_=Vv[:, g * 8:(g + 1) * 8, :]),
                reads=[DB("Vall")], writes=[BVa], dsem=s_kv)
    for g in range(4):
        tr.emit("gpsimd", lambda e, g=g: e.tensor_scalar(out=Va[:, g * 8:(g + 1) * 8, :], in0=Va[:, g * 8:(g + 1) * 8, :],
                                                          scalar1=K.is2[:, 0:1], scalar2=None, op0=ALU.mult),
                reads=[BVa, K.Bc], writes=[BVa])

    s_q = tr.new_dma_sem("qb")
    s_an = [tr.new_dma_sem("an%d" % i) for i in range(2)]
    PS = K.PS
    XB = [4, 5]
    OB = [6, 7]
    elem = ["vector", "gpsimd"]
    prs = [slice(0, 64), slice(64, 128)]

    for qb in L.qblocks:
        ts = slice(qb * TB, (qb + 1) * TB)
        tr.emit("sync", lambda e, ts=ts: e.dma_start(out=qblk[:], in_=dr["qT"][:, :, ts]),
                reads=[DB("qT", qb)], writes=[Bq], dsem=s_q)
        own0 = qb * 4
        tiles = [(own0 + j, j) for j in (3, 2, 1, 0)] + [(kt, None) for kt in range(own0 - 1, -1, -1)]
        nst = len(tiles)
        for hp in range(4):
            def zmm(k, hp=hp):
                kt = tiles[k][0]
                for ch in range(2):
                    zb = 2 * (k % 2) + ch
                    tr.emit("tensor", lambda e, ch=ch, kt=kt, zb=zb, hp=hp: e.matmul(P[zb][:, :], kTa[prs[ch], hp, kt * 128:(kt + 1) * 128],
                                                                                     qblk[prs[ch], hp, :], start=True, stop=True),
                            reads=[BkT, Bq], writes=[PB[zb]])

            def expx_and_mult(k):
                sl = k % 2
                s3 = k % 3
                tr.emit("scalar", lambda e, sl=sl: e.activation(out=Xe[sl][:], in_=PS[:, 4:6, :], func=AF.Exp),
                        reads=[PB[4], PB[5]], writes=[BXe[sl]])
                for ch in range(2):
                    tr.emit("vector", lambda e, ch=ch, sl=sl, s3=s3: e.tensor_tensor(out=Aw[sl][:, ch, :], in0=E[s3][:, ch, :], in1=Xe[sl][:, ch, :], op=ALU.mult),
                            reads=[BE[s3], BXe[sl]], writes=[BAw[sl][ch]])

            def av(k, last, hp=hp):
                sl = k % 2
                pkt = tiles[k][0]
                for ch in range(2):
                    h = 2 * hp + ch
                    tr.emit("tensor", lambda e, ch=ch, sl=sl, pkt=pkt, h=h, k=k, last=last: e.matmul(
                        P[OB[ch]][prs[ch], :], Va[:, pkt, h * 64:(h + 1) * 64], Aw[sl][:, ch, :], start=(k == 0), stop=last),
                        reads=[BVa, BAw[sl][ch]], writes=[PB[OB[ch]]])

            zmm(0)
            for k in range(nst):
                tr.maybe_epoch()
                sl = k % 2
                kt, dj = tiles[k]
                if k + 1 < nst:
                    zmm(k + 1)
                s3 = k % 3
                tr.emit("scalar", lambda e, sl=sl, s3=s3: e.activation(out=E[s3][:], in_=PS[:, 2 * sl:2 * sl + 2, :], func=AF.Exp, scale=0.125),
                        reads=[PB[2 * sl], PB[2 * sl + 1]], writes=[BE[s3]])
                if dj is not None:
                    for ch in range(2):
                        tr.emit("vector", lambda e, ch=ch, s3=s3, dj=dj: e.tensor_tensor(out=E[s3][:, ch, :], in0=E[s3][:, ch, :],
                                                                                          in1=K.cst[:, C_MASK + dj * 512:C_MASK + (dj + 1) * 512], op=ALU.mult),
                                reads=[BE[s3], K.Bc], writes=[BE[s3]])
                if k > 0:
                    expx_and_mult(k - 1)
                tr.emit("scalar", lambda e, sl=sl, s3=s3: e.activation(out=Lp[sl][:], in_=E[s3][:], func=AF.Ln, bias=1.0, scale=1.0),
                        reads=[BE[s3]], writes=[BLp[sl]])
                for ch in range(2):
                    if k > 0:
                        tr.emit("tensor", lambda e, ch=ch, sl=sl: e.matmul(P[XB[ch]][:, :], M2neg, Lp[1 - sl][:, ch, :], start=False, stop=False),
                                reads=[BLp[1 - sl], K.Bc], writes=[PB[XB[ch]]])
                    tr.emit("tensor", lambda e, ch=ch, sl=sl, k=k, nst=nst: e.matmul(P[XB[ch]][:, :], Mneg, Lp[sl][:, ch, :], start=(k == 0), stop=(k == nst - 1)),
                            reads=[BLp[sl], K.Bc], writes=[PB[XB[ch]]])
                if k > 0:
                    av(k - 1, False)
            expx_and_mult(nst - 1)
            av(nst - 1, True)
            for ch in range(2):
                tr.emit("vector" if ch == 0 else "scalar",
                        (lambda e, ch=ch, hp=hp: e.tensor_copy(out=ablk[prs[ch], hp, :], in_=P[OB[ch]][prs[ch], :])) if ch == 0 else
                        (lambda e, ch=ch, hp=hp: e.activation(out=ablk[prs[ch], hp, :], in_=P[OB[ch]][prs[ch], :], func=AF.Copy)),
                        reads=[PB[OB[ch]]], writes=[Bab])
        s = qb % 2
        rms_stats(K, ablk, Bab, 4, TB, sqa, Bsqa, 1.0 / 512, rsa, Brsa, tmp, Btmp, 0, ones_bf)
        for c in range(4):
            tr.emit("vector", lambda e, c=c, s=s: e.scalar_tensor_tensor(out=an[s][:, c, :], in0=ablk[:, c, :], scalar=vec[:, V_ATT + c:V_ATT + c + 1],
                                                                         in1=rsa[:], op0=ALU.mult, op1=ALU.mult),
                    reads=[Bab, Brsa, K.Bc], writes=[Ban[s]])
        tr.emit("sync", lambda e, s=s, ts=ts: e.dma_start(out=dr["attnT"][:, :, ts], in_=an[s][:]),
                reads=[Ban[s]], writes=[DB("attnT", qb)], dsem=s_an[s])
    _free(st)


def phase_B2(K, L):
    layer = L.layer
    nc, tr, dr, P, PB, DB = K.nc, K.tr, K.dr, K.P, K.PB, K.DB
    st = []
    setup_eps(K, st)
    moe = layer == 1
    hb = _alloc(K, st, "s", "hbB", [128, 8, TB], F32)
    Bhb = Buf("hbB")
    an = _alloc(K, st, "s", "anB", [128, 4, TB], BF16)
    Ban = Buf("anB")
    yn = _alloc(K, st, "s", "ynB", [128, 4, TB], BF16)
    Byn = Buf("ynB")
    Wsq = _alloc(K, st, "s", "Wsq", [128, 8, D], BF16)
    BWsq = Buf("Wsq")
    Wp = _alloc(K, st, "s", "Wp", [128, 2, D], BF16)
    BWp = Buf("Wp")
    pTb = _alloc(K, st, "s", "pTb", [128, 2, TB], BF16)
    BpT = Buf("pTb")
    sq = _alloc(K, st, "s", "sqB", [128, 8, TB], BF16)
    Bsq = Buf("sqB")
    tmp = _alloc(K, st, "s", "tmpC", [128, TB], F32)
    Btmp = Buf("tmpC")
    fT = _alloc(K, st, "s", "fT", [128, 8, TB], BF16)
    BfT = Buf("fT")
    act = _alloc(K, st, "s", "act", [128, NFF, TB], BF16)
    Bact = Buf("act")
    W13 = [[_alloc(K, st, "s", "W%d_%d" % (w, i), [128, 8, 256], BF16) for i in range(2)] for w in range(2)]
    BW13 = [[Buf("W%d_%d" % (w, i)) for i in range(2)] for w in range(2)]
    W2 = _alloc(K, st, "s", "W2", [128, NFF, D], BF16)
    BW2 = [[Buf("W2_%d_%d" % (hf, i)) for i in range(7)] for hf in range(2)]
    sil = [_alloc(K, st, "s", "sil%d" % i, [128, TB], F32) for i in range(2)]
    Bsil = [Buf("sil%d" % i) for i in range(2)]
    if moe:
        feT = [_alloc(K, st, "s", "feT%d" % i, [128, 8, TB], BF16) for i in range(2)]
        BfeT = [Buf("feT%d" % i) for i in range(2)]
        f32t = [_alloc(K, st, "s", "f32t%d" % i, [128, 8, 128], F32) for i in range(2)]
        Bf32 = [Buf("f32t%d" % i) for i in range(2)]
        Wr = _alloc(K, st, "s", "Wr", [128, 8, NE], F32)
        BWr = Buf("Wr")
        lg = _alloc(K, st, "s", "lg", [128, 4, NE], F32)
        top = _alloc(K, st, "s", "top", [128, 4, 8], F32)
        nm1 = _alloc(K, st, "s", "nm1", [128, 4], F32)
        msk = _alloc(K, st, "s", "msk", [128, 4, NE], F32)
        ex = _alloc(K, st, "s", "ex", [128, 4, NE], F32)
        den = _alloc(K, st, "s", "den", [128, 4], F32)
        comb = _alloc(K, st, "s", "comb", [128, 4, NE], F32)
        Brt = Buf("router")
        dg = [_alloc(K, st, "s", "dg%d" % i, [128, 128], F32) for i in range(2)]
        Bdg = [Buf("dg%d" % i) for i in range(2)]

    ones_bf = K.cstb[:, 256:384]
    ones_f = K.cst[:, C_ONES:C_ONES + 128]
    ident_f = K.cst[:, C_IDENT:C_IDENT + 128]
    vec = K.vec

    s_h = tr.new_dma_sem("hB")
    s_an = tr.new_dma_sem("anB")
    s_yn = tr.new_dma_sem("ynB")
    s_p = tr.new_dma_sem("pTb")
    s_ws = tr.new_dma_sem("Wsq")
    s_wg = tr.new_dma_sem("Wgt")
    s_wp = tr.new_dma_sem("Wp")
    s_w13 = [[tr.new_dma_sem("W%d_%d" % (w, i)) for i in range(2)] for w in range(2)]
    s_w2 = [[tr.new_dma_sem("W2_%d_%d" % (hf, i)) for i in range(7)] for hf in range(2)]

    hv = L.hsrc.rearrange("(c p) t -> p c t", p=128)
    hov = L.hdst.rearrange("(c p) t -> p c t", p=128)
    wov = L.w_o.rearrange("(c p) n -> p c n", p=128)
    wgv = L.wg.rearrange("(c p) n -> p c n", p=128)
    wpv = L.wp.rearrange("(c p) n -> p c n", p=128)
    pv = L.pT.rearrange("(c p) t -> p c t", p=128)

    tr.emit("gpsimd", lambda e: e.dma_start(out=Wp[:], in_=wpv), writes=[BWp], dsem=s_wp)
    if moe:
        s_wr = tr.new_dma_sem("Wr")
        tr.emit("sync", lambda e: e.dma_start(out=Wr[:], in_=dr["router_w"].rearrange("(c p) n -> p c n", p=128)),
                writes=[BWr], dsem=s_wr)

    pr = [0]

    def pbank():
        pr[0] = (pr[0] + 1) % 6
        return 2 + pr[0]

    def load_sq(wview):
        for g in range(2):
            tr.emit("gpsimd", lambda e, g=g: e.dma_start(out=Wsq[:, g * 4:(g + 1) * 4, :], in_=wview[:, g * 4:(g + 1) * 4, :]),
                    writes=[BWsq], dsem=s_ws)

    def norm_to(gcol0, dst, dbuf):
        rms_stats(K, hb, Bhb, 8, TB, sq, Bsq, 1.0 / D, P[1], PB[1], tmp, Btmp, 0, ones_bf)
        for c in range(8):
            tr.emit("vector", lambda e, c=c: e.scalar_tensor_tensor(out=dst[:, c, :], in0=hb[:, c, :], scalar=vec[:, gcol0 + c:gcol0 + c + 1],
                                                                     in1=P[1][:, :], op0=ALU.mult, op1=ALU.mult),
                    reads=[Bhb, PB[1], K.Bc], writes=[dbuf])

    def wviews(e_idx):
        return (L.w1[e_idx].rearrange("(c p) n -> p c n", p=128), L.w3[e_idx].rearrange("(c p) n -> p c n", p=128),
                L.w2[e_idx].rearrange("(c p) n -> p c n", p=128))

    def load_w13(e_idx, grp):
        w1v, w3v, _ = wviews(e_idx)
        sl = grp % 2
        cs = slice(grp * 256, (grp + 1) * 256)
        tr.emit("gpsimd", lambda e, sl=sl, cs=cs: e.dma_start(out=W13[0][sl][:], in_=w1v[:, :, cs]), writes=[BW13[0][sl]], dsem=s_w13[0][sl])
        tr.emit("gpsimd", lambda e, sl=sl, cs=cs: e.dma_start(out=W13[1][sl][:], in_=w3v[:, :, cs]), writes=[BW13[1][sl]], dsem=s_w13[1][sl])

    def load_w2(e_idx, hf, g):
        w2v = wviews(e_idx)[2]
        tr.emit("gpsimd", lambda e, g=g, hf=hf: e.dma_start(out=W2[:, g * 4:(g + 1) * 4, hf * 512:(hf + 1) * 512],
                                                             in_=w2v[:, g * 4:(g + 1) * 4, hf * 512:(hf + 1) * 512]),
                writes=[BW2[hf][g]], dsem=s_w2[hf][g])

    def prefetch_first(e_idx):
        load_w13(e_idx, 0)
        load_w13(e_idx, 1)
        for hf in range(2):
            for g in range(7):
                load_w2(e_idx, hf, g)

    def stage1(e_idx, xin, xbuf, xg, xgbuf):
        pr[0] = 5
        for grp in range(14):
            tr.maybe_epoch()
            sl = grp % 2
            for j in range(2):
                ff = grp * 2 + j
                pg = pbank()
                pu = pbank()
                for kc in range(8):
                    tr.emit("tensor", lambda e, kc=kc, pg=pg, sl=sl, j=j: e.matmul(P[pg][:, :], W13[0][sl][:, kc, j * 128:(j + 1) * 128], xg[:, kc, :],
                                                                                   start=(kc == 0), stop=(kc == 7)),
                            reads=[BW13[0][sl], xgbuf], writes=[PB[pg]])
                for kc in range(8):
                    tr.emit("tensor", lambda e, kc=kc, pu=pu, sl=sl, j=j: e.matmul(P[pu][:, :], W13[1][sl][:, kc, j * 128:(j + 1) * 128], xin[:, kc, :],
                                                                                   start=(kc == 0), stop=(kc == 7)),
                            reads=[BW13[1][sl], xbuf], writes=[PB[pu]])
                i2 = ff % 2
                tr.emit("scalar", lambda e, pg=pg, i2=i2: e.activation(out=sil[i2][:], in_=P[pg][:, :], func=AF.Silu),
                        reads=[PB[pg]], writes=[Bsil[i2]])
                tr.emit("vector", lambda e, pu=pu, i2=i2, ff=ff: e.tensor_tensor(out=act[:, ff, :], in0=P[pu][:, :], in1=sil[i2][:], op=ALU.mult),
                        reads=[PB[pu], Bsil[i2]], writes=[Bact])
            if grp + 2 < 14:
                load_w13(e_idx, grp + 2)

    def stage2_pass(hf, nxt):
        for g in range(7):
            for f4 in range(4):
                ff = g * 4 + f4
                for o4 in range(4):
                    oc = hf * 4 + o4
                    tr.emit("tensor", lambda e, ff=ff, o4=o4, oc=oc: e.matmul(P[4 + o4][:, :], W2[:, ff, oc * 128:(oc + 1) * 128], act[:, ff, :],
                                                                              start=(ff == 0), stop=(ff == NFF - 1)),
                            reads=[BW2[hf][g], Bact], writes=[PB[4 + o4]])
            if nxt is not None:
                load_w2(nxt, hf, g)
        for o4 in range(4):
            oc = hf * 4 + o4
            tr.emit("vector", lambda e, o4=o4, oc=oc: e.tensor_tensor(out=hb[:, oc, :], in0=P[4 + o4][:, :], in1=hb[:, oc, :], op=ALU.add),
                    reads=[PB[4 + o4], Bhb], writes=[Bhb])

    def prep_fe(ei):
        for tt in range(4):
            i2 = tt % 2
            tr.emit("vector", lambda e, tt=tt, i2=i2, ei=ei: e.tensor_scalar(out=dg[i2][:], in0=ident_f, scalar1=comb[:, tt, ei:ei + 1],
                                                                             scalar2=None, op0=ALU.mult),
                    reads=[Brt, K.Bc], writes=[Bdg[i2]])
            tr.emit("tensor", lambda e, tt=tt, i2=i2: e.matmul(P[0][:, tt * 128:(tt + 1) * 128], ones_f, dg[i2][:], start=True, stop=True),
                    reads=[Bdg[i2], K.Bc], writes=[PB[0]])
        fb = ei % 2
        for c in range(8):
            tr.emit("vector", lambda e, c=c, fb=fb: e.tensor_tensor(out=feT[fb][:, c, :], in0=P[0][:, :], in1=fT[:, c, :], op=ALU.mult),
                    reads=[PB[0], BfT], writes=[BfeT[fb]])

    def ffn_block(experts):
        for n, ei in enumerate(experts):
            nxt = experts[n + 1] if n + 1 < len(experts) else None
            if moe:
                if n == 0:
                    prep_fe(ei)
                stage1(ei, feT[ei % 2], BfeT[ei % 2], fT, BfT)
            else:
                stage1(ei, fT, BfT, fT, BfT)
            if nxt is not None:
                load_w13(nxt, 0)
                load_w13(nxt, 1)
            stage2_pass(0, nxt)
            if moe and nxt is not None:
                prep_fe(nxt)
            stage2_pass(1, nxt)

    load_sq(wov)
    Wgt = act[:, 0:16, :].rearrange("p (a b) c -> p a (b c)", b=2)

    for b in L.blocks:
        ts = slice(b * TB, (b + 1) * TB)
        tsp = slice((b - L.p_off) * TB, (b - L.p_off + 1) * TB)
        tso = slice((b - L.o_off) * TB, (b - L.o_off + 1) * TB)
        tr.emit("sync", lambda e, ts=ts: e.dma_start(out=hb[:], in_=hv[:, :, ts]), reads=[DB("hsrc", b)], writes=[Bhb], dsem=s_h)
        tr.emit("sync", lambda e, ts=ts: e.dma_start(out=an[:], in_=dr["attnT"][:, :, ts]), reads=[DB("attnT", b)], writes=[Ban], dsem=s_an)
        tr.emit("sync", lambda e, ts=ts: e.dma_start(out=yn[:], in_=dr["yT"][:, :, ts]), reads=[DB("yT", b)], writes=[Byn], dsem=s_yn)
        tr.emit("gpsimd", lambda e, tsp=tsp: e.dma_start(out=pTb[:], in_=pv[:, :, tsp]), writes=[BpT], dsem=s_p)
        prefetch_first(0)
        for oc in range(8):
            pb = pbank()
            for c in range(4):
                tr.emit("tensor", lambda e, c=c, pb=pb, oc=oc: e.matmul(P[pb][:, :], Wsq[:, c, oc * 128:(oc + 1) * 128], an[:, c, :],
                                                                        start=(c == 0), stop=False),
                        reads=[BWsq, Ban], writes=[PB[pb]])
            for c in range(4):
                tr.emit("tensor", lambda e, c=c, pb=pb, oc=oc: e.matmul(P[pb][:, :], Wsq[:, 4 + c, oc * 128:(oc + 1) * 128], yn[:, c, :],
                                                                        start=False, stop=(c == 3)),
                        reads=[BWsq, Byn], writes=[PB[pb]])
            tr.emit("vector", lambda e, pb=pb, oc=oc: e.tensor_tensor(out=hb[:, oc, :], in0=P[pb][:, :], in1=hb[:, oc, :], op=ALU.add),
                    reads=[PB[pb], Bhb], writes=[Bhb])
        norm_to(V_FFN, fT, BfT)
        if not moe:
            ffn_block([0])
        else:
            for tt in range(4):
                i2 = tt % 2
                for c in range(8):
                    tr.emit("vector", lambda e, c=c, tt=tt, i2=i2: e.scalar_tensor_tensor(
                        out=f32t[i2][:, c, :], in0=hb[:, c, tt * 128:(tt + 1) * 128], scalar=vec[:, V_FFN + c:V_FFN + c + 1],
                        in1=P[1][:, tt * 128:(tt + 1) * 128], op0=ALU.mult, op1=ALU.mult),
                        reads=[Bhb, PB[1], K.Bc], writes=[Bf32[i2]])
                pb = pbank()
                for c in range(8):
                    tr.emit("tensor", lambda e, c=c, pb=pb, i2=i2: e.matmul(P[pb][:, 0:NE], f32t[i2][:, c, :], Wr[:, c, :], start=(c == 0), stop=(c == 7)),
                            reads=[Bf32[i2], BWr], writes=[PB[pb]])
                tr.emit("vector", lambda e, pb=pb, tt=tt: e.tensor_copy(out=lg[:, tt, :], in_=P[pb][:, 0:NE]), reads=[PB[pb]], writes=[Brt])
                tr.emit("vector", lambda e, tt=tt: e.max(out=top[:, tt, :], in_=lg[:, tt, :]), reads=[Brt], writes=[Brt])
                tr.emit("vector", lambda e, tt=tt: e.tensor_scalar(out=nm1[:, tt:tt + 1], in0=top[:, tt, 0:1], scalar1=-1.0, scalar2=None, op0=ALU.mult),
                        reads=[Brt], writes=[Brt])
                tr.emit("vector", lambda e, tt=tt: e.tensor_scalar(out=msk[:, tt, :], in0=lg[:, tt, :], scalar1=top[:, tt, 1:2], scalar2=None, op0=ALU.is_ge),
                        reads=[Brt], writes=[Brt])
                tr.emit("scalar", lambda e, tt=tt: e.activation(out=ex[:, tt, :], in_=lg[:, tt, :], func=AF.Exp, bias=nm1[:, tt:tt + 1], scale=1.0),
                        reads=[Brt], writes=[Brt])
                tr.emit("vector", lambda e, tt=tt: e.tensor_tensor(out=ex[:, tt, :], in0=ex[:, tt, :], in1=msk[:, tt, :], op=ALU.mult),
                        reads=[Brt], writes=[Brt])
                tr.emit("vector", lambda e, tt=tt: e.tensor_reduce(out=den[:, tt:tt + 1], in_=ex[:, tt, :], axis=mybir.AxisListType.X, op=ALU.add),
                        reads=[Brt], writes=[Brt])
                tr.emit("vector", lambda e, tt=tt: e.reciprocal(out=den[:, tt:tt + 1], in_=den[:, tt:tt + 1]), reads=[Brt], writes=[Brt])
                tr.emit("vector", lambda e, tt=tt: e.tensor_scalar(out=comb[:, tt, :], in0=ex[:, tt, :], scalar1=den[:, tt:tt + 1], scalar2=None, op0=ALU.mult),
                        reads=[Brt], writes=[Brt])
            ffn_block(list(range(NE)))
        for g in range(2):
            tr.emit("gpsimd", lambda e, g=g: e.dma_start(out=Wgt[:, g * 4:(g + 1) * 4, :], in_=wgv[:, g * 4:(g + 1) * 4, :]),
                    writes=[Bact], dsem=s_wg)
        norm_to(V_PLE, fT, BfT)
        for oc in range(8):
            pg = pbank()
            pp = pbank()
            for c in range(8):
                tr.emit("tensor", lambda e, c=c, pg=pg, oc=oc: e.matmul(P[pg][:, :], Wgt[:, c, oc * 128:(oc + 1) * 128], fT[:, c, :],
                                                                        start=(c == 0), stop=(c == 7)),
                        reads=[Bact, BfT], writes=[PB[pg]])
            for c in range(2):
                tr.emit("tensor", lambda e, c=c, pp=pp, oc=oc: e.matmul(P[pp][:, :], Wp[:, c, oc * 128:(oc + 1) * 128], pTb[:, c, :],
                                                                        start=(c == 0), stop=(c == 1)),
                        reads=[BWp, BpT], writes=[PB[pp]])
            i2 = oc % 2
            tr.emit("scalar", lambda e, pg=pg, i2=i2: e.activation(out=sil[i2][:], in_=P[pg][:, :], func=AF.Sigmoid),
                    reads=[PB[pg]], writes=[Bsil[i2]])
            tr.emit("vector", lambda e, pp=pp, i2=i2: e.tensor_tensor(out=sil[i2][:], in0=P[pp][:, :], in1=sil[i2][:], op=ALU.mult),
                    reads=[PB[pp], Bsil[i2]], writes=[Bsil[i2]])
            tr.emit("gpsimd", lambda e, oc=oc, i2=i2: e.tensor_tensor(out=hb[:, oc, :], in0=hb[:, oc, :], in1=sil[i2][:], op=ALU.add),
                    reads=[Bhb, Bsil[i2]], writes=[Bhb])
        tr.emit("sync", lambda e, tso=tso: e.dma_start(out=hov[:, :, tso], in_=hb[:]), reads=[Bhb], writes=[DB("hdst", b)], dsem=s_h)
    _free(st)


def _consts():
    c = np.zeros((128, NCONST), np.float32)
    jp = np.arange(128)[:, None]
    j = np.arange(128)[None, :]
    c[:, C_MNEG:C_MNEG + 128] = np.where(jp >= j, -1.0, 0.0)
    c[:, C_M2NEG:C_M2NEG + 128] = np.where(jp < j, -1.0, 0.0)
    c[:, C_ONES:C_ONES + 128] = 1.0
    c[0:64, C_BLK:C_BLK + 64] = 1.0
    c[64:128, C_BLK + 64:C_BLK + 128] = 1.0
    c[:, C_IDENT:C_IDENT + 128] = np.eye(128, dtype=np.float32)
    t = np.arange(512)[None, :]
    for dj in range(4):
        s = dj * 128 + np.arange(128)[:, None]
        c[:, C_MASK + dj * 512:C_MASK + (dj + 1) * 512] = (t > s).astype(np.float32)
    return c


def _vecs(i, mix_norm_g, ffn_norm_g, ple_norm_g, attn_out_g, conv_out_g, conv_w, conv_b, q_norm_g, k_norm_g):
    v = np.zeros((128, NV), np.float32)
    v[:, V_MIX:V_MIX + 8] = mix_norm_g[i].reshape(8, 128).T
    v[:, V_FFN:V_FFN + 8] = ffn_norm_g[i].reshape(8, 128).T
    v[:, V_PLE:V_PLE + 8] = ple_norm_g[i].reshape(8, 128).T
    v[:, V_ATT:V_ATT + 4] = attn_out_g[i].reshape(4, 128).T
    v[:, V_CVG:V_CVG + 4] = conv_out_g[i].reshape(4, 128).T
    for k in range(3):
        v[:, V_CW + 4 * k:V_CW + 4 * k + 4] = conv_w[i, k].reshape(4, 128).T
    v[:, V_CB:V_CB + 4] = conv_b[i].reshape(4, 128).T
    v[:, V_QG] = np.tile(q_norm_g[i], 2)
    v[:, V_KG] = np.tile(k_norm_g[i], 2)
    return v


_PROG = []


def kernel(x, p, mix_norm_g, w_in, q_norm_g, k_norm_g, conv_w, conv_b, attn_out_g, conv_out_g, w_o,
           ffn_norm_g, dense_w1, dense_w3, dense_w2, router_w, moe_w1, moe_w3, moe_w2,
           ple_norm_g, ple_gate_w, ple_proj_w):
    f = lambda a: np.ascontiguousarray(np.asarray(a, dtype=np.float32))
    x, p = f(x), f(p)
    consts = _consts()
    cores = list(range(NCORES))
    args = (f(mix_norm_g), f(ffn_norm_g), f(ple_norm_g), f(attn_out_g), f(conv_out_g), f(conv_w), f(conv_b), f(q_norm_g), f(k_norm_g))
    vecs = np.ascontiguousarray(np.concatenate([_vecs(0, *args), _vecs(1, *args)], axis=1))
    shared = {"consts": consts, "vecs": vecs, "w_in": f(w_in), "w_o": f(w_o), "ple_gate_w": f(ple_gate_w), "ple_proj_w": f(ple_proj_w),
              "dw1": f(dense_w1), "dw3": f(dense_w3), "dw2": f(dense_w2), "mw1": f(moe_w1[0]), "mw3": f(moe_w3[0]), "mw2": f(moe_w2[0]),
              "router_w": f(router_w[0])}
    in_maps = []
    for c in cores:
        b, hf = c // 2, c % 2
        if hf == 1:
            xT = np.ascontiguousarray(x[b].T)
            pT0 = np.ascontiguousarray(p[0, b].T)
        else:
            xT = np.ascontiguousarray(np.concatenate([np.zeros((D, T), np.float32), x[b, :T].T], axis=1))
            pT0 = np.ascontiguousarray(np.concatenate([np.zeros((256, T), np.float32), p[0, b, :T].T], axis=1))
        pT1 = np.ascontiguousarray(p[1, b, hf * T:(hf + 1) * T].T)
        d = dict(shared)
        d.update({"is2": np.full((128, 1), float(hf), np.float32), "xT": xT, "pT0": pT0, "pT1": pT1})
        in_maps.append(d)
    if not _PROG:
        _PROG.append(build_fused())
    res = run_bass_kernel_spmd(_PROG[0], in_maps, core_ids=cores).results
    out = np.empty((4, 2 * T, D), np.float32)
    for c in cores:
        out[c // 2, (c % 2) * T:(c % 2 + 1) * T, :] = np.asarray(res[c]["hTo"]).T
    return out
```

```python
import numpy as np
import ml_dtypes
import concourse.bass as bass
import concourse.mybir as mybir
from concourse.bass_utils import run_bass_kernel_spmd

F32 = mybir.dt.float32
BF16 = mybir.dt.bfloat16
AF = mybir.ActivationFunctionType
ALU = mybir.AluOpType

D = 1024
T = 4096
NB = 8
TB = 512
DFF = 3584
NFF = 28
NE = 8
EPS = 1e-6
NCORES = 8

V_MIX = 0
V_FFN = 8
V_PLE = 16
V_ATT = 24
V_CVG = 28
V_CW = 32
V_CB = 44
V_QG = 48
V_KG = 49
NV = 50

C_MNEG = 0
C_M2NEG = 128
C_ONES = 256
C_BLK = 384
C_IDENT = 512
C_MASK = 640
NCONST = 640 + 2048


class SemC:
    def __init__(self, sem, unit):
        self.sem = sem
        self.unit = unit
        self.count = 0
        self.retired = False


class Buf:
    __slots__ = ("name", "last_w", "readers")

    def __init__(self, name):
        self.name = name
        self.last_w = None
        self.readers = {}


class Tracker:
    ENG = ("tensor", "scalar", "vector", "gpsimd", "sync")

    def __init__(self, nc):
        self.nc = nc
        self.streams = {e: [] for e in self.ENG}
        self.sc = {}
        self.waited = {e: {} for e in self.ENG}
        self.dma_sems = []
        self.npartial = 0
        self._dcache = {}
        self._cms = []
        self.epoch = 0
        for e in self.ENG:
            self._new_eng_sem(e)

    def _new_eng_sem(self, e):
        cm = self.nc.semaphore("s_%s_%d" % (e, self.epoch))
        s = cm.__enter__()
        self._cms.append(cm)
        self.sc[e] = SemC(s, 1)

    def maybe_epoch(self, limit=20000):
        if max(self.sc[e].count for e in self.ENG) < limit:
            return
        self.barrier()
        self.epoch += 1
        for e in self.ENG:
            self.sc[e].retired = True
            self._new_eng_sem(e)

    def new_dma_sem(self, name):
        if name in self._dcache:
            return self._dcache[name]
        cm = self.nc.semaphore("d_" + name)
        s = cm.__enter__()
        self._cms.append(cm)
        sc = SemC(s, 16)
        self.dma_sems.append(sc)
        self._dcache[name] = sc
        return sc

    def close(self):
        for cm in reversed(self._cms):
            cm.__exit__(None, None, None)

    def emit(self, eng, fn, reads=(), writes=(), dsem=None):
        deps = []
        for b in reads:
            if b.last_w is not None:
                deps.append(b.last_w)
        for b in writes:
            if b.last_w is not None:
                deps.append(b.last_w)
            deps.extend(b.readers.items())
        own = self.sc[eng]
        waits = {}
        wd = self.waited[eng]
        for sc, val in deps:
            if sc.retired:
                continue
            if sc is own and eng == "tensor" and dsem is None:
                continue
            if wd.get(sc, 0) >= val:
                continue
            if waits.get(sc, 0) < val:
                waits[sc] = val
        st = self.streams[eng]
        for sc, val in waits.items():
            if sc.unit == 16 and val < sc.count:
                self.npartial += 1
                val = sc.count
            wd[sc] = val
            st.append(("w", sc.sem, val))
        sc = dsem if dsem is not None else own
        sc.count += sc.unit
        val = sc.count
        st.append(("i", fn, sc.sem, sc.unit))
        for b in reads:
            if b.readers.get(sc, 0) < val:
                b.readers[sc] = val
        for b in writes:
            b.last_w = (sc, val)
            b.readers = {}

    def barrier(self):
        allsc = [self.sc[e] for e in self.ENG] + self.dma_sems
        for e in self.ENG:
            wd = self.waited[e]
            for sc in allsc:
                if sc.retired or (sc is self.sc[e] and e == "tensor"):
                    continue
                if sc.count > 0 and wd.get(sc, 0) < sc.count:
                    wd[sc] = sc.count
                    self.streams[e].append(("w", sc.sem, sc.count))

    def replay(self, eng_name, e):
        for it in self.streams[eng_name]:
            if it[0] == "w":
                e.wait_ge(it[1], it[2])
            else:
                it[1](e).then_inc(it[2], it[3])


class Ctx:
    pass


_UID = [0]


def _alloc(K, stack, kind, name, shape, dt):
    _UID[0] += 1
    name = "%s_u%d" % (name, _UID[0])
    cm = (K.nc.sbuf_tensor if kind == "s" else K.nc.psum_tensor)(name, shape, dt)
    t = cm.__enter__()
    stack.append(cm)
    return t


def _free(stack):
    while stack:
        stack.pop().__exit__(None, None, None)


def build_fused():
    nc = bass.Bass("TRN2", target_bir_lowering=False)
    K = Ctx()
    K.nc = nc
    dr = {}

    def dram(name, shape, dt, kind):
        dr[name] = nc.dram_tensor(name, list(shape), dt, kind=kind).ap()
        return dr[name]

    dram("consts", [128, NCONST], F32, "ExternalInput")
    dram("is2", [128, 1], F32, "ExternalInput")
    dram("vecs", [128, 2 * NV], F32, "ExternalInput")
    dram("xT", [D, 2 * T], F32, "ExternalInput")
    dram("pT0", [256, 2 * T], F32, "ExternalInput")
    dram("pT1", [256, T], F32, "ExternalInput")
    dram("w_in", [2, D, 3072], F32, "ExternalInput")
    dram("w_o", [2, D, D], F32, "ExternalInput")
    dram("ple_gate_w", [2, D, D], F32, "ExternalInput")
    dram("ple_proj_w", [2, 256, D], F32, "ExternalInput")
    dram("dw1", [1, D, DFF], F32, "ExternalInput")
    dram("dw3", [1, D, DFF], F32, "ExternalInput")
    dram("dw2", [1, DFF, D], F32, "ExternalInput")
    dram("mw1", [NE, D, DFF], F32, "ExternalInput")
    dram("mw3", [NE, D, DFF], F32, "ExternalInput")
    dram("mw2", [NE, DFF, D], F32, "ExternalInput")
    dram("router_w", [D, NE], F32, "ExternalInput")
    dram("h1T", [D, 2 * T], F32, "Internal")
    dram("qT", [128, 4, 2 * T], BF16, "Internal")
    dram("kT", [128, 4, 2 * T], BF16, "Internal")
    dram("V", [2 * T, 512], BF16, "Internal")
    dram("yT", [128, 4, 2 * T], BF16, "Internal")
    dram("attnT", [128, 4, 2 * T], BF16, "Internal")
    dram("hTo", [D, T], F32, "ExternalOutput")

    tr = Tracker(nc)
    K.tr = tr
    K.dr = dr
    K.dbuf = {}

    def DB(name, blk=0):
        k = (name, blk)
        if k not in K.dbuf:
            K.dbuf[k] = Buf("%s_%s" % k)
        return K.dbuf[k]
    K.DB = DB

    base = []
    K.PS = _alloc(K, base, "p", "psall", [128, 8, 512], F32)
    K.P = [K.PS[:, i, :] for i in range(8)]
    K.PB = [Buf("ps%d" % i) for i in range(8)]
    K.cst = _alloc(K, base, "s", "cst", [128, NCONST], F32)
    K.cstb = _alloc(K, base, "s", "cstb", [128, 512], BF16)
    K.vec2 = _alloc(K, base, "s", "vec2", [128, 2 * NV], F32)
    K.is2 = _alloc(K, base, "s", "is2s", [128, 1], F32)
    K.Bc = Buf("consts")
    sem_c = tr.new_dma_sem("c")
    tr.emit("sync", lambda e: e.dma_start(out=K.cst[:], in_=dr["consts"][:, :]), writes=[K.Bc], dsem=sem_c)
    tr.emit("sync", lambda e: e.dma_start(out=K.vec2[:], in_=dr["vecs"][:, :]), writes=[K.Bc], dsem=sem_c)
    tr.emit("sync", lambda e: e.dma_start(out=K.is2[:], in_=dr["is2"][:, :]), writes=[K.Bc], dsem=sem_c)
    tr.emit("vector", lambda e: e.tensor_copy(out=K.cstb[:], in_=K.cst[:, 0:512]), reads=[K.Bc], writes=[K.Bc])

    for layer in range(2):
        K.vec = K.vec2[:, layer * NV:(layer + 1) * NV]
        L = Ctx()
        L.layer = layer
        L.hsrc = dr["xT"] if layer == 0 else dr["h1T"]
        L.w_in = dr["w_in"][layer]
        L.qblocks = list(range(16)) if layer == 0 else list(range(8, 16))
        L.blocks = L.qblocks
        L.hdst = dr["h1T"] if layer == 0 else dr["hTo"]
        L.o_off = 0 if layer == 0 else 8
        L.pT = dr["pT0"] if layer == 0 else dr["pT1"]
        L.p_off = 0 if layer == 0 else 8
        L.w_o = dr["w_o"][layer]
        L.wg = dr["ple_gate_w"][layer]
        L.wp = dr["ple_proj_w"][layer]
        if layer == 0:
            L.w1, L.w3, L.w2 = dr["dw1"], dr["dw3"], dr["dw2"]
        else:
            L.w1, L.w3, L.w2 = dr["mw1"], dr["mw3"], dr["mw2"]
        phase_A(K, L)
        tr.barrier()
        phase_B1(K, L)
        tr.barrier()
        phase_B2(K, L)
        tr.barrier()

    with nc.Block() as block:
        @block.sync
        def _(e):
            tr.replay("sync", e)

        @block.scalar
        def _(e):
            tr.replay("scalar", e)

        @block.vector
        def _(e):
            tr.replay("vector", e)

        @block.gpsimd
        def _(e):
            tr.replay("gpsimd", e)

        @block.tensor
        def _(e):
            tr.replay("tensor", e)
    _free(base)
    tr.close()
    return nc


def rms_stats(K, src, srcbuf, nch, ncols, sq, sqbuf, inv_n, dst_rstd, dstbuf, tmp, tmpbuf, pbank, ones_ap):
    tr = K.tr
    P, PB = K.P, K.PB
    for c in range(nch):
        tr.emit("scalar", lambda e, c=c: e.activation(out=sq[:, c, 0:ncols], in_=src[:, c, 0:ncols], func=AF.Square),
                reads=[srcbuf], writes=[sqbuf])
    for c in range(nch):
        tr.emit("tensor", lambda e, c=c: e.matmul(P[pbank][:, 0:ncols], ones_ap, sq[:, c, 0:ncols],
                                                   start=(c == 0), stop=(c == nch - 1)),
                reads=[sqbuf, K.Bc], writes=[PB[pbank]])
    tr.emit("scalar", lambda e: e.activation(out=tmp[:, 0:ncols], in_=P[pbank][:, 0:ncols], func=AF.Ln,
                                             bias=EPS, scale=inv_n),
            reads=[PB[pbank], K.Bc], writes=[tmpbuf])
    tr.emit("scalar", lambda e: e.activation(out=dst_rstd[:, 0:ncols], in_=tmp[:, 0:ncols], func=AF.Exp, scale=-0.5),
            reads=[tmpbuf], writes=[dstbuf])


def setup_eps(K, stack):
    pass


def phase_A(K, L):
    nc, tr, dr, P, PB, DB = K.nc, K.tr, K.dr, K.P, K.PB, K.DB
    st = []
    setup_eps(K, st)
    Win = _alloc(K, st, "s", "Win", [128, 8, 3072], BF16)
    BWin = Buf("Win")
    hb = [_alloc(K, st, "s", "hb%d" % i, [128, 8, TB], F32) for i in range(2)]
    Bhb = [Buf("hb%d" % i) for i in range(2)]
    sq = _alloc(K, st, "s", "sq", [128, 8, TB], BF16)
    Bsq = Buf("sq")
    aT = [_alloc(K, st, "s", "aT%d" % i, [128, 8, TB], BF16) for i in range(2)]
    BaT = [Buf("aT%d" % i) for i in range(2)]
    tmp = _alloc(K, st, "s", "tmpA", [128, TB], F32)
    Btmp = Buf("tmpA")
    sqq = [_alloc(K, st, "s", "sqq%d" % i, [128, 1, TB], BF16) for i in range(2)]
    Bsqq = [Buf("sqq%d" % i) for i in range(2)]
    rsq = [_alloc(K, st, "s", "rsq%d" % i, [128, TB], F32) for i in range(2)]
    Brsq = [Buf("rsq%d" % i) for i in range(2)]
    tmq = [_alloc(K, st, "s", "tmq%d" % i, [128, TB], F32) for i in range(2)]
    Btmq = [Buf("tmq%d" % i) for i in range(2)]
    qb_ = [_alloc(K, st, "s", "qblk%d" % i, [128, 4, TB], BF16) for i in range(2)]
    Bqb = [Buf("qblk%d" % i) for i in range(2)]
    kb_ = [_alloc(K, st, "s", "kblk%d" % i, [128, 4, TB], BF16) for i in range(2)]
    Bkb = [Buf("kblk%d" % i) for i in range(2)]
    vb_ = [_alloc(K, st, "s", "vblk%d" % i, [128, 4, 512], BF16) for i in range(2)]
    Bvb = [Buf("vblk%d" % i) for i in range(2)]
    usb = [_alloc(K, st, "s", "usb%d" % i, [128, TB], F32) for i in range(2)]
    Busb = [Buf("usb%d" % i) for i in range(2)]
    cu = _alloc(K, st, "s", "cu", [128, 4, TB + 2], F32)
    Bcu = Buf("cu")
    acc = [_alloc(K, st, "s", "acc%d" % i, [128, TB], F32) for i in range(2)]
    Bacc = [Buf("acc%d" % i) for i in range(2)]
    y = _alloc(K, st, "s", "y", [128, 4, TB], F32)
    By = Buf("y")
    sqy = _alloc(K, st, "s", "sqy", [128, 4, TB], BF16)
    Bsqy = Buf("sqy")
    rsy = _alloc(K, st, "s", "rsy", [128, TB], F32)
    Brsy = Buf("rsy")
    yn = [_alloc(K, st, "s", "yn%d" % i, [128, 4, TB], BF16) for i in range(2)]
    Byn = [Buf("yn%d" % i) for i in range(2)]

    ones_bf = K.cstb[:, 256:384]
    blk_bf = K.cstb[:, 384:512]
    vec = K.vec

    sW = tr.new_dma_sem("Win")
    wv = L.w_in.rearrange("(c p) n -> p c n", p=128)
    for g in range(6):
        tr.emit("gpsimd", lambda e, g=g: e.dma_start(out=Win[:, :, g * 512:(g + 1) * 512], in_=wv[:, :, g * 512:(g + 1) * 512]),
                writes=[BWin], dsem=sW)

    hv = L.hsrc.rearrange("(c p) t -> p c t", p=128)
    s_h = [tr.new_dma_sem("hb%d" % i) for i in range(2)]
    s_q = [tr.new_dma_sem("q%d" % i) for i in range(2)]
    s_k = [tr.new_dma_sem("k%d" % i) for i in range(2)]
    s_v = [tr.new_dma_sem("v%d" % i) for i in range(2)]
    s_y = [tr.new_dma_sem("y%d" % i) for i in range(2)]

    pr = [0]

    def pbank():
        pr[0] = (pr[0] + 1) % 4
        return 2 + pr[0]

    def norm_and_aT(src, srcbuf, ncols, dst, dstbuf):
        rms_stats(K, src, srcbuf, 8, ncols, sq, Bsq, 1.0 / D, P[1], PB[1], tmp, Btmp, 0, ones_bf)
        for c in range(8):
            tr.emit("vector", lambda e, c=c: e.scalar_tensor_tensor(
                out=dst[:, c, 0:ncols], in0=src[:, c, 0:ncols], scalar=vec[:, V_MIX + c:V_MIX + c + 1],
                in1=P[1][:, 0:ncols], op0=ALU.mult, op1=ALU.mult),
                reads=[srcbuf, PB[1], K.Bc], writes=[dstbuf])

    def proj(col0, a, abuf, ncols):
        pb = pbank()
        for kc in range(8):
            tr.emit("tensor", lambda e, kc=kc, pb=pb: e.matmul(P[pb][:, 0:ncols], Win[:, kc, col0:col0 + 128], a[:, kc, 0:ncols],
                                                               start=(kc == 0), stop=(kc == 7)),
                    reads=[BWin, abuf], writes=[PB[pb]])
        return pb

    tr.emit("vector", lambda e: e.memset(cu[:, :, 0:2], 0.0), writes=[Bcu])
    for b in range(2 * NB):
        tr.maybe_epoch()
        s = b % 2
        ts = slice(b * TB, (b + 1) * TB)
        tr.emit("sync", lambda e, s=s, ts=ts: e.dma_start(out=hb[s][:], in_=hv[:, :, ts]),
                reads=[DB("hsrc", b)], writes=[Bhb[s]], dsem=s_h[s])
        norm_and_aT(hb[s], Bhb[s], TB, aT[s], BaT[s])
        full = (L.layer == 0) or (b >= NB - 1)
        for which, dst, dbuf, gcol in (("q", qb_[s], Bqb[s], V_QG), ("k", kb_[s], Bkb[s], V_KG)):
            if which == "q" and not full:
                continue
            c0 = 0 if which == "q" else 512
            for c in range(4):
                i2 = c % 2
                pq = proj(c0 + c * 128, aT[s], BaT[s], TB)
                tr.emit("scalar", lambda e, pq=pq, i2=i2: e.activation(out=sqq[i2][:, 0, :], in_=P[pq][:, :], func=AF.Square),
                        reads=[PB[pq]], writes=[Bsqq[i2]])
                tr.emit("tensor", lambda e, i2=i2: e.matmul(P[6 + i2][:, :], blk_bf, sqq[i2][:, 0, :], start=True, stop=True),
                        reads=[Bsqq[i2], K.Bc], writes=[PB[6 + i2]])
                tr.emit("scalar", lambda e, i2=i2: e.activation(out=tmq[i2][:], in_=P[6 + i2][:, :], func=AF.Ln, bias=EPS, scale=1.0 / 64),
                        reads=[PB[6 + i2], K.Bc], writes=[Btmq[i2]])
                tr.emit("scalar", lambda e, i2=i2: e.activation(out=rsq[i2][:], in_=tmq[i2][:], func=AF.Exp, scale=-0.5),
                        reads=[Btmq[i2]], writes=[Brsq[i2]])
                tr.emit("vector", lambda e, pq=pq, i2=i2, c=c, dst=dst, gcol=gcol: e.scalar_tensor_tensor(
                    out=dst[:, c, :], in0=P[pq][:, :], scalar=vec[:, gcol:gcol + 1], in1=rsq[i2][:],
                    op0=ALU.mult, op1=ALU.mult),
                    reads=[PB[pq], Brsq[i2], K.Bc], writes=[dbuf])
        if full:
            tr.emit("sync", lambda e, s=s, ts=ts: e.dma_start(out=dr["qT"][:, :, ts], in_=qb_[s][:]),
                    reads=[Bqb[s]], writes=[DB("qT", b)], dsem=s_q[s])
        tr.emit("sync", lambda e, s=s, ts=ts: e.dma_start(out=dr["kT"][:, :, ts], in_=kb_[s][:]),
                reads=[Bkb[s]], writes=[DB("kT", b)], dsem=s_k[s])
        for tt in range(4):
            pb = pbank()
            for kc in range(8):
                tr.emit("tensor", lambda e, kc=kc, pb=pb, tt=tt, s=s: e.matmul(P[pb][:, :], aT[s][:, kc, tt * 128:(tt + 1) * 128], Win[:, kc, 1024:1536],
                                                                               start=(kc == 0), stop=(kc == 7)),
                        reads=[BWin, BaT[s]], writes=[PB[pb]])
            tr.emit("vector", lambda e, pb=pb, tt=tt, s=s: e.tensor_copy(out=vb_[s][:, tt, :], in_=P[pb][:, :]),
                    reads=[PB[pb]], writes=[Bvb[s]])
        tr.emit("sync", lambda e, s=s, b=b: e.dma_start(out=dr["V"][b * TB:(b + 1) * TB, :].rearrange("(t p) n -> p t n", p=128), in_=vb_[s][:]),
                reads=[Bvb[s]], writes=[DB("V", b)], dsem=s_v[s])
        if not full:
            continue
        for c in range(4):
            i2 = c % 2
            pu = proj(1536 + c * 128, aT[s], BaT[s], TB)
            pc = proj(2048 + c * 128, aT[s], BaT[s], TB)
            pg = proj(2560 + c * 128, aT[s], BaT[s], TB)
            tr.emit("scalar", lambda e, pu=pu, i2=i2: e.activation(out=usb[i2][:], in_=P[pu][:, :], func=AF.Copy),
                    reads=[PB[pu]], writes=[Busb[i2]])
            tr.emit("vector", lambda e, c=c, pc=pc, i2=i2: e.tensor_tensor(out=cu[:, c, 2:TB + 2], in0=P[pc][:, :], in1=usb[i2][:], op=ALU.mult),
                    reads=[PB[pc], Busb[i2]], writes=[Bcu])
            tr.emit("gpsimd", lambda e, c=c, i2=i2: e.tensor_scalar(out=acc[i2][:], in0=cu[:, c, 2:TB + 2],
                                                                     scalar1=vec[:, V_CW + 8 + c:V_CW + 9 + c], scalar2=vec[:, V_CB + c:V_CB + c + 1],
                                                                     op0=ALU.mult, op1=ALU.add),
                    reads=[Bcu, K.Bc], writes=[Bacc[i2]])
            tr.emit("vector", lambda e, c=c, i2=i2: e.scalar_tensor_tensor(out=acc[i2][:], in0=cu[:, c, 1:TB + 1], scalar=vec[:, V_CW + 4 + c:V_CW + 5 + c],
                                                                           in1=acc[i2][:], op0=ALU.mult, op1=ALU.add),
                    reads=[Bcu, Bacc[i2], K.Bc], writes=[Bacc[i2]])
            tr.emit("vector", lambda e, c=c, i2=i2: e.scalar_tensor_tensor(out=acc[i2][:], in0=cu[:, c, 0:TB], scalar=vec[:, V_CW + c:V_CW + 1 + c],
                                                                           in1=acc[i2][:], op0=ALU.mult, op1=ALU.add),
                    reads=[Bcu, Bacc[i2], K.Bc], writes=[Bacc[i2]])
            tr.emit("vector", lambda e, c=c, pg=pg, i2=i2: e.tensor_tensor(out=y[:, c, :], in0=P[pg][:, :], in1=acc[i2][:], op=ALU.mult),
                    reads=[PB[pg], Bacc[i2]], writes=[By])
            if b == NB - 1:
                tr.emit("gpsimd", lambda e, c=c: e.tensor_scalar(out=cu[:, c, 0:2], in0=cu[:, c, TB:TB + 2], scalar1=K.is2[:, 0:1],
                                                                  scalar2=None, op0=ALU.mult),
                        reads=[Bcu, K.Bc], writes=[Bcu])
            else:
                tr.emit("gpsimd", lambda e, c=c: e.tensor_copy(out=cu[:, c, 0:2], in_=cu[:, c, TB:TB + 2]),
                        reads=[Bcu], writes=[Bcu])
        rms_stats(K, y, By, 4, TB, sqy, Bsqy, 1.0 / 512, rsy, Brsy, tmp, Btmp, 6, ones_bf)
        for c in range(4):
            tr.emit("vector", lambda e, c=c, s=s: e.scalar_tensor_tensor(out=yn[s][:, c, :], in0=y[:, c, :], scalar=vec[:, V_CVG + c:V_CVG + c + 1],
                                                                         in1=rsy[:], op0=ALU.mult, op1=ALU.mult),
                    reads=[By, Brsy, K.Bc], writes=[Byn[s]])
        tr.emit("sync", lambda e, s=s, ts=ts: e.dma_start(out=dr["yT"][:, :, ts], in_=yn[s][:]),
                reads=[Byn[s]], writes=[DB("yT", b)], dsem=s_y[s])
    _free(st)


def phase_B1(K, L):
    nc, tr, dr, P, PB, DB = K.nc, K.tr, K.dr, K.P, K.PB, K.DB
    st = []
    setup_eps(K, st)
    NKT = 2 * T // 128
    kTa = _alloc(K, st, "s", "kTa", [128, 4, 2 * T], BF16)
    BkT = Buf("kTa")
    Va = _alloc(K, st, "s", "Va", [128, NKT, 512], BF16)
    BVa = Buf("Va")
    qz = [_alloc(K, st, "s", "qz%d" % i, [128, 4, TB], BF16) for i in range(2)]
    Bq = Buf("qz")
    tr.emit("vector", lambda e: e.memset(qz[0][64:128, :, :], 0.0), writes=[Bq])
    tr.emit("vector", lambda e: e.memset(qz[1][0:64, :, :], 0.0), writes=[Bq])
    E = [_alloc(K, st, "s", "E%d" % i, [128, 2, TB], F32) for i in range(3)]
    BE = [Buf("E%d" % i) for i in range(3)]
    Lp = [_alloc(K, st, "s", "Lp%d" % i, [128, 2, TB], BF16) for i in range(2)]
    BLp = [Buf("Lp%d" % i) for i in range(2)]
    Xe = [_alloc(K, st, "s", "Xe%d" % i, [128, 2, TB], F32) for i in range(2)]
    BXe = [Buf("Xe%d" % i) for i in range(2)]
    Aw = [_alloc(K, st, "s", "Aw%d" % i, [128, 2, TB], BF16) for i in range(2)]
    BAw = [[Buf("Aw%d%d" % (i, ch)) for ch in range(2)] for i in range(2)]
    ablk = _alloc(K, st, "s", "ablk", [128, 4, TB], F32)
    Bab = Buf("ablk")
    sqa = _alloc(K, st, "s", "sqa", [128, 4, TB], BF16)
    Bsqa = Buf("sqa")
    rsa = _alloc(K, st, "s", "rsa", [128, TB], F32)
    Brsa = Buf("rsa")
    tmp = _alloc(K, st, "s", "tmpB", [128, TB], F32)
    Btmp = Buf("tmpB")
    an = [_alloc(K, st, "s", "an%d" % i, [128, 4, TB], BF16) for i in range(2)]
    Ban = [Buf("an%d" % i) for i in range(2)]

    Mneg = K.cstb[:, 0:128]
    M2neg = K.cstb[:, 128:256]
    ones_bf = K.cstb[:, 256:384]
    vec = K.vec

    s_kv = tr.new_dma_sem("kvV")
    s_kk = tr.new_dma_sem("kvK")
    for g in range(4):
        tr.emit("sync", lambda e, g=g: e.dma_start(out=kTa[:, g, :], in_=dr["kT"][:, g, :]),
                reads=[DB("kTall")], writes=[BkT], dsem=s_kk)
    Vv = dr["V"].rearrange("(t p) n -> p t n", p=128)
    for g in range(8):
        tr.emit("sync", lambda e, g=g: e.dma_start(out=Va[:, g * 8:(g + 1) * 8, :], in_=Vv[:, g * 8:(g + 1) * 8, :]),
                reads=[DB("Vall")], writes=[BVa], dsem=s_kv)
    for g in range(4):
        tr.emit("gpsimd", lambda e, g=g: e.tensor_scalar(out=Va[:, g * 8:(g + 1) * 8, :], in0=Va[:, g * 8:(g + 1) * 8, :],
                                                          scalar1=K.is2[:, 0:1], scalar2=None, op0=ALU.mult),
                reads=[BVa, K.Bc], writes=[BVa])

    s_q = tr.new_dma_sem("qb")
    s_an = [tr.new_dma_sem("an%d" % i) for i in range(2)]
    PS = K.PS
    XB = [4, 5]
    OB = [6, 7]
    elem = ["vector", "gpsimd"]
    prs = [slice(0, 64), slice(64, 128)]

    for qb in L.qblocks:
        ts = slice(qb * TB, (qb + 1) * TB)
        tr.emit("sync", lambda e, ts=ts: e.dma_start(out=qz[0][0:64, :, :], in_=dr["qT"][0:64, :, ts]),
                reads=[DB("qT", qb)], writes=[Bq], dsem=s_q)
        tr.emit("sync", lambda e, ts=ts: e.dma_start(out=qz[1][64:128, :, :], in_=dr["qT"][64:128, :, ts]),
                reads=[DB("qT", qb)], writes=[Bq], dsem=s_q)
        own0 = qb * 4
        tiles = [(own0 + j, j) for j in (3, 2, 1, 0)] + [(kt, None) for kt in range(own0 - 1, -1, -1)]
        nst = len(tiles)
        for hp in range(4):
            def zmm(k, hp=hp):
                kt = tiles[k][0]
                for ch in range(2):
                    zb = 2 * (k % 2) + ch
                    tr.emit("tensor", lambda e, ch=ch, kt=kt, zb=zb, hp=hp: e.matmul(P[zb][:, :], kTa[:, hp, kt * 128:(kt + 1) * 128],
                                                                                     qz[ch][:, hp, :], start=True, stop=True),
                            reads=[BkT, Bq], writes=[PB[zb]])

            def expx_and_mult(k):
                sl = k % 2
                s3 = k % 3
                tr.emit("scalar", lambda e, sl=sl: e.activation(out=Xe[sl][:], in_=PS[:, 4:6, :], func=AF.Exp),
                        reads=[PB[4], PB[5]], writes=[BXe[sl]])
                for ch in range(2):
                    tr.emit("vector", lambda e, ch=ch, sl=sl, s3=s3: e.tensor_tensor(out=Aw[sl][:, ch, :], in0=E[s3][:, ch, :], in1=Xe[sl][:, ch, :], op=ALU.mult),
                            reads=[BE[s3], BXe[sl]], writes=[BAw[sl][ch]])

            def av(k, last, hp=hp):
                sl = k % 2
                pkt = tiles[k][0]
                for ch in range(2):
                    h = 2 * hp + ch
                    tr.emit("tensor", lambda e, ch=ch, sl=sl, pkt=pkt, h=h, k=k, last=last, hp=hp: e.matmul(
                        P[OB[ch]][:, :], Va[:, pkt, hp * 128:(hp + 1) * 128], Aw[sl][:, ch, :], start=(k == 0), stop=last),
                        reads=[BVa, BAw[sl][ch]], writes=[PB[OB[ch]]])

            zmm(0)
            for k in range(nst):
                tr.maybe_epoch()
                sl = k % 2
                kt, dj = tiles[k]
                if k + 1 < nst:
                    zmm(k + 1)
                s3 = k % 3
                tr.emit("scalar", lambda e, sl=sl, s3=s3: e.activation(out=E[s3][:], in_=PS[:, 2 * sl:2 * sl + 2, :], func=AF.Exp, scale=0.125),
                        reads=[PB[2 * sl], PB[2 * sl + 1]], writes=[BE[s3]])
                if dj is not None:
                    for ch in range(2):
                        tr.emit("vector", lambda e, ch=ch, s3=s3, dj=dj: e.tensor_tensor(out=E[s3][:, ch, :], in0=E[s3][:, ch, :],
                                                                                          in1=K.cst[:, C_MASK + dj * 512:C_MASK + (dj + 1) * 512], op=ALU.mult),
                                reads=[BE[s3], K.Bc], writes=[BE[s3]])
                if k > 0:
                    expx_and_mult(k - 1)
                tr.emit("scalar", lambda e, sl=sl, s3=s3: e.activation(out=Lp[sl][:], in_=E[s3][:], func=AF.Ln, bias=1.0, scale=1.0),
                        reads=[BE[s3]], writes=[BLp[sl]])
                for ch in range(2):
                    if k > 0:
                        tr.emit("tensor", lambda e, ch=ch, sl=sl: e.matmul(P[XB[ch]][:, :], M2neg, Lp[1 - sl][:, ch, :], start=False, stop=False),
                                reads=[BLp[1 - sl], K.Bc], writes=[PB[XB[ch]]])
                    tr.emit("tensor", lambda e, ch=ch, sl=sl, k=k, nst=nst: e.matmul(P[XB[ch]][:, :], Mneg, Lp[sl][:, ch, :], start=(k == 0), stop=(k == nst - 1)),
                            reads=[BLp[sl], K.Bc], writes=[PB[XB[ch]]])
                if k > 0:
                    av(k - 1, False)
            expx_and_mult(nst - 1)
            av(nst - 1, True)
            for ch in range(2):
                tr.emit("vector" if ch == 0 else "scalar",
                        (lambda e, ch=ch, hp=hp: e.tensor_copy(out=ablk[prs[ch], hp, :], in_=P[OB[ch]][prs[ch], :])) if ch == 0 else
                        (lambda e, ch=ch, hp=hp: e.activation(out=ablk[prs[ch], hp, :], in_=P[OB[ch]][prs[ch], :], func=AF.Copy)),
                        reads=[PB[OB[ch]]], writes=[Bab])
        s = qb % 2
        rms_stats(K, ablk, Bab, 4, TB, sqa, Bsqa, 1.0 / 512, rsa, Brsa, tmp, Btmp, 0, ones_bf)
        for c in range(4):
            tr.emit("vector", lambda e, c=c, s=s: e.scalar_tensor_tensor(out=an[s][:, c, :], in0=ablk[:, c, :], scalar=vec[:, V_ATT + c:V_ATT + c + 1],
                                                                         in1=rsa[:], op0=ALU.mult, op1=ALU.mult),
                    reads=[Bab, Brsa, K.Bc], writes=[Ban[s]])
        tr.emit("sync", lambda e, s=s, ts=ts: e.dma_start(out=dr["attnT"][:, :, ts], in_=an[s][:]),
                reads=[Ban[s]], writes=[DB("attnT", qb)], dsem=s_an[s])
    _free(st)


def phase_B2(K, L):
    layer = L.layer
    nc, tr, dr, P, PB, DB = K.nc, K.tr, K.dr, K.P, K.PB, K.DB
    st = []
    setup_eps(K, st)
    moe = layer == 1
    hb = _alloc(K, st, "s", "hbB", [128, 8, TB], F32)
    Bhb = Buf("hbB")
    an = _alloc(K, st, "s", "anB", [128, 4, TB], BF16)
    Ban = Buf("anB")
    yn = _alloc(K, st, "s", "ynB", [128, 4, TB], BF16)
    Byn = Buf("ynB")
    Wsq = _alloc(K, st, "s", "Wsq", [128, 8, D], BF16)
    BWsq = Buf("Wsq")
    Wp = _alloc(K, st, "s", "Wp", [128, 2, D], BF16)
    BWp = Buf("Wp")
    pTb = _alloc(K, st, "s", "pTb", [128, 2, TB], BF16)
    BpT = Buf("pTb")
    sq = _alloc(K, st, "s", "sqB", [128, 8, TB], BF16)
    Bsq = Buf("sqB")
    tmp = _alloc(K, st, "s", "tmpC", [128, TB], F32)
    Btmp = Buf("tmpC")
    fT = _alloc(K, st, "s", "fT", [128, 8, TB], BF16)
    BfT = Buf("fT")
    act = _alloc(K, st, "s", "act", [128, NFF, TB], BF16)
    Bact = Buf("act")
    W13 = [[_alloc(K, st, "s", "W%d_%d" % (w, i), [128, 8, 256], BF16) for i in range(2)] for w in range(2)]
    BW13 = [[Buf("W%d_%d" % (w, i)) for i in range(2)] for w in range(2)]
    W2 = _alloc(K, st, "s", "W2", [128, NFF, D], BF16)
    BW2 = [[Buf("W2_%d_%d" % (hf, i)) for i in range(7)] for hf in range(2)]
    sil = [_alloc(K, st, "s", "sil%d" % i, [128, TB], F32) for i in range(2)]
    Bsil = [Buf("sil%d" % i) for i in range(2)]
    if moe:
        feT = [_alloc(K, st, "s", "feT%d" % i, [128, 8, TB], BF16) for i in range(2)]
        BfeT = [Buf("feT%d" % i) for i in range(2)]
        f32t = [_alloc(K, st, "s", "f32t%d" % i, [128, 8, 128], F32) for i in range(2)]
        Bf32 = [Buf("f32t%d" % i) for i in range(2)]
        Wr = _alloc(K, st, "s", "Wr", [128, 8, NE], F32)
        BWr = Buf("Wr")
        lg = _alloc(K, st, "s", "lg", [128, 4, NE], F32)
        top = _alloc(K, st, "s", "top", [128, 4, 8], F32)
        nm1 = _alloc(K, st, "s", "nm1", [128, 4], F32)
        msk = _alloc(K, st, "s", "msk", [128, 4, NE], F32)
        ex = _alloc(K, st, "s", "ex", [128, 4, NE], F32)
        den = _alloc(K, st, "s", "den", [128, 4], F32)
        comb = _alloc(K, st, "s", "comb", [128, 4, NE], F32)
        Brt = Buf("router")
        dg = [_alloc(K, st, "s", "dg%d" % i, [128, 128], F32) for i in range(2)]
        Bdg = [Buf("dg%d" % i) for i in range(2)]

    ones_bf = K.cstb[:, 256:384]
    ones_f = K.cst[:, C_ONES:C_ONES + 128]
    ident_f = K.cst[:, C_IDENT:C_IDENT + 128]
    vec = K.vec

    s_h = tr.new_dma_sem("hB")
    s_an = tr.new_dma_sem("anB")
    s_yn = tr.new_dma_sem("ynB")
    s_p = tr.new_dma_sem("pTb")
    s_ws = tr.new_dma_sem("Wsq")
    s_wg = tr.new_dma_sem("Wgt")
    s_wp = tr.new_dma_sem("Wp")
    s_w13 = [[tr.new_dma_sem("W%d_%d" % (w, i)) for i in range(2)] for w in range(2)]
    s_w2 = [[tr.new_dma_sem("W2_%d_%d" % (hf, i)) for i in range(7)] for hf in range(2)]

    hv = L.hsrc.rearrange("(c p) t -> p c t", p=128)
    hov = L.hdst.rearrange("(c p) t -> p c t", p=128)
    wov = L.w_o.rearrange("(c p) n -> p c n", p=128)
    wgv = L.wg.rearrange("(c p) n -> p c n", p=128)
    wpv = L.wp.rearrange("(c p) n -> p c n", p=128)
    pv = L.pT.rearrange("(c p) t -> p c t", p=128)

    tr.emit("gpsimd", lambda e: e.dma_start(out=Wp[:], in_=wpv), writes=[BWp], dsem=s_wp)
    if moe:
        s_wr = tr.new_dma_sem("Wr")
        tr.emit("sync", lambda e: e.dma_start(out=Wr[:], in_=dr["router_w"].rearrange("(c p) n -> p c n", p=128)),
                writes=[BWr], dsem=s_wr)

    pr = [0]

    def pbank():
        pr[0] = (pr[0] + 1) % 6
        return 2 + pr[0]

    def load_sq(wview):
        for g in range(2):
            tr.emit("gpsimd", lambda e, g=g: e.dma_start(out=Wsq[:, g * 4:(g + 1) * 4, :], in_=wview[:, g * 4:(g + 1) * 4, :]),
                    writes=[BWsq], dsem=s_ws)

    def norm_to(gcol0, dst, dbuf):
        rms_stats(K, hb, Bhb, 8, TB, sq, Bsq, 1.0 / D, P[1], PB[1], tmp, Btmp, 0, ones_bf)
        for c in range(8):
            tr.emit("vector", lambda e, c=c: e.scalar_tensor_tensor(out=dst[:, c, :], in0=hb[:, c, :], scalar=vec[:, gcol0 + c:gcol0 + c + 1],
                                                                     in1=P[1][:, :], op0=ALU.mult, op1=ALU.mult),
                    reads=[Bhb, PB[1], K.Bc], writes=[dbuf])

    def wviews(e_idx):
        return (L.w1[e_idx].rearrange("(c p) n -> p c n", p=128), L.w3[e_idx].rearrange("(c p) n -> p c n", p=128),
                L.w2[e_idx].rearrange("(c p) n -> p c n", p=128))

    def load_w13(e_idx, grp):
        w1v, w3v, _ = wviews(e_idx)
        sl = grp % 2
        cs = slice(grp * 256, (grp + 1) * 256)
        tr.emit("gpsimd", lambda e, sl=sl, cs=cs: e.dma_start(out=W13[0][sl][:], in_=w1v[:, :, cs]), writes=[BW13[0][sl]], dsem=s_w13[0][sl])
        tr.emit("gpsimd", lambda e, sl=sl, cs=cs: e.dma_start(out=W13[1][sl][:], in_=w3v[:, :, cs]), writes=[BW13[1][sl]], dsem=s_w13[1][sl])

    def load_w2(e_idx, hf, g):
        w2v = wviews(e_idx)[2]
        tr.emit("gpsimd", lambda e, g=g, hf=hf: e.dma_start(out=W2[:, g * 4:(g + 1) * 4, hf * 512:(hf + 1) * 512],
                                                             in_=w2v[:, g * 4:(g + 1) * 4, hf * 512:(hf + 1) * 512]),
                writes=[BW2[hf][g]], dsem=s_w2[hf][g])

    def prefetch_first(e_idx):
        load_w13(e_idx, 0)
        load_w13(e_idx, 1)
        for hf in range(2):
            for g in range(7):
                load_w2(e_idx, hf, g)

    def stage1(e_idx, xin, xbuf, xg, xgbuf):
        pr[0] = 5
        for grp in range(14):
            tr.maybe_epoch()
            sl = grp % 2
            for j in range(2):
                ff = grp * 2 + j
                pg = pbank()
                pu = pbank()
                for kc in range(8):
                    tr.emit("tensor", lambda e, kc=kc, pg=pg, sl=sl, j=j: e.matmul(P[pg][:, :], W13[0][sl][:, kc, j * 128:(j + 1) * 128], xg[:, kc, :],
                                                                                   start=(kc == 0), stop=(kc == 7)),
                            reads=[BW13[0][sl], xgbuf], writes=[PB[pg]])
                for kc in range(8):
                    tr.emit("tensor", lambda e, kc=kc, pu=pu, sl=sl, j=j: e.matmul(P[pu][:, :], W13[1][sl][:, kc, j * 128:(j + 1) * 128], xin[:, kc, :],
                                                                                   start=(kc == 0), stop=(kc == 7)),
                            reads=[BW13[1][sl], xbuf], writes=[PB[pu]])
                i2 = ff % 2
                tr.emit("scalar", lambda e, pg=pg, i2=i2: e.activation(out=sil[i2][:], in_=P[pg][:, :], func=AF.Silu),
                        reads=[PB[pg]], writes=[Bsil[i2]])
                tr.emit("vector", lambda e, pu=pu, i2=i2, ff=ff: e.tensor_tensor(out=act[:, ff, :], in0=P[pu][:, :], in1=sil[i2][:], op=ALU.mult),
                        reads=[PB[pu], Bsil[i2]], writes=[Bact])
            if grp + 2 < 14:
                load_w13(e_idx, grp + 2)

    def stage2_pass(hf, nxt):
        for g in range(7):
            for f4 in range(4):
                ff = g * 4 + f4
                for o4 in range(4):
                    oc = hf * 4 + o4
                    tr.emit("tensor", lambda e, ff=ff, o4=o4, oc=oc: e.matmul(P[4 + o4][:, :], W2[:, ff, oc * 128:(oc + 1) * 128], act[:, ff, :],
                                                                              start=(ff == 0), stop=(ff == NFF - 1)),
                            reads=[BW2[hf][g], Bact], writes=[PB[4 + o4]])
            if nxt is not None:
                load_w2(nxt, hf, g)
        for o4 in range(4):
            oc = hf * 4 + o4
            tr.emit("vector", lambda e, o4=o4, oc=oc: e.tensor_tensor(out=hb[:, oc, :], in0=P[4 + o4][:, :], in1=hb[:, oc, :], op=ALU.add),
                    reads=[PB[4 + o4], Bhb], writes=[Bhb])

    def prep_fe(ei):
        for tt in range(4):
            i2 = tt % 2
            tr.emit("vector", lambda e, tt=tt, i2=i2, ei=ei: e.tensor_scalar(out=dg[i2][:], in0=ident_f, scalar1=comb[:, tt, ei:ei + 1],
                                                                             scalar2=None, op0=ALU.mult),
                    reads=[Brt, K.Bc], writes=[Bdg[i2]])
            tr.emit("tensor", lambda e, tt=tt, i2=i2: e.matmul(P[0][:, tt * 128:(tt + 1) * 128], ones_f, dg[i2][:], start=True, stop=True),
                    reads=[Bdg[i2], K.Bc], writes=[PB[0]])
        fb = ei % 2
        for c in range(8):
            tr.emit("vector", lambda e, c=c, fb=fb: e.tensor_tensor(out=feT[fb][:, c, :], in0=P[0][:, :], in1=fT[:, c, :], op=ALU.mult),
                    reads=[PB[0], BfT], writes=[BfeT[fb]])

    def ffn_block(experts):
        for n, ei in enumerate(experts):
            nxt = experts[n + 1] if n + 1 < len(experts) else None
            if moe:
                if n == 0:
                    prep_fe(ei)
                stage1(ei, feT[ei % 2], BfeT[ei % 2], fT, BfT)
            else:
                stage1(ei, fT, BfT, fT, BfT)
            if nxt is not None:
                load_w13(nxt, 0)
                load_w13(nxt, 1)
            stage2_pass(0, nxt)
            if moe and nxt is not None:
                prep_fe(nxt)
            stage2_pass(1, nxt)

    load_sq(wov)
    Wgt = act[:, 0:16, :].rearrange("p (a b) c -> p a (b c)", b=2)

    for b in L.blocks:
        ts = slice(b * TB, (b + 1) * TB)
        tsp = slice((b - L.p_off) * TB, (b - L.p_off + 1) * TB)
        tso = slice((b - L.o_off) * TB, (b - L.o_off + 1) * TB)
        tr.emit("sync", lambda e, ts=ts: e.dma_start(out=hb[:], in_=hv[:, :, ts]), reads=[DB("hsrc", b)], writes=[Bhb], dsem=s_h)
        tr.emit("sync", lambda e, ts=ts: e.dma_start(out=an[:], in_=dr["attnT"][:, :, ts]), reads=[DB("attnT", b)], writes=[Ban], dsem=s_an)
        tr.emit("sync", lambda e, ts=ts: e.dma_start(out=yn[:], in_=dr["yT"][:, :, ts]), reads=[DB("yT", b)], writes=[Byn], dsem=s_yn)
        tr.emit("gpsimd", lambda e, tsp=tsp: e.dma_start(out=pTb[:], in_=pv[:, :, tsp]), writes=[BpT], dsem=s_p)
        prefetch_first(0)
        for oc in range(8):
            pb = pbank()
            for c in range(4):
                tr.emit("tensor", lambda e, c=c, pb=pb, oc=oc: e.matmul(P[pb][:, :], Wsq[:, c, oc * 128:(oc + 1) * 128], an[:, c, :],
                                                                        start=(c == 0), stop=False),
                        reads=[BWsq, Ban], writes=[PB[pb]])
            for c in range(4):
                tr.emit("tensor", lambda e, c=c, pb=pb, oc=oc: e.matmul(P[pb][:, :], Wsq[:, 4 + c, oc * 128:(oc + 1) * 128], yn[:, c, :],
                                                                        start=False, stop=(c == 3)),
                        reads=[BWsq, Byn], writes=[PB[pb]])
            tr.emit("vector", lambda e, pb=pb, oc=oc: e.tensor_tensor(out=hb[:, oc, :], in0=P[pb][:, :], in1=hb[:, oc, :], op=ALU.add),
                    reads=[PB[pb], Bhb], writes=[Bhb])
        norm_to(V_FFN, fT, BfT)
        if not moe:
            ffn_block([0])
        else:
            for tt in range(4):
                i2 = tt % 2
                for c in range(8):
                    tr.emit("vector", lambda e, c=c, tt=tt, i2=i2: e.scalar_tensor_tensor(
                        out=f32t[i2][:, c, :], in0=hb[:, c, tt * 128:(tt + 1) * 128], scalar=vec[:, V_FFN + c:V_FFN + c + 1],
                        in1=P[1][:, tt * 128:(tt + 1) * 128], op0=ALU.mult, op1=ALU.mult),
                        reads=[Bhb, PB[1], K.Bc], writes=[Bf32[i2]])
                pb = pbank()
                for c in range(8):
                    tr.emit("tensor", lambda e, c=c, pb=pb, i2=i2: e.matmul(P[pb][:, 0:NE], f32t[i2][:, c, :], Wr[:, c, :], start=(c == 0), stop=(c == 7)),
                            reads=[Bf32[i2], BWr], writes=[PB[pb]])
                tr.emit("vector", lambda e, pb=pb, tt=tt: e.tensor_copy(out=lg[:, tt, :], in_=P[pb][:, 0:NE]), reads=[PB[pb]], writes=[Brt])
                tr.emit("vector", lambda e, tt=tt: e.max(out=top[:, tt, :], in_=lg[:, tt, :]), reads=[Brt], writes=[Brt])
                tr.emit("vector", lambda e, tt=tt: e.tensor_scalar(out=nm1[:, tt:tt + 1], in0=top[:, tt, 0:1], scalar1=-1.0, scalar2=None, op0=ALU.mult),
                        reads=[Brt], writes=[Brt])
                tr.emit("vector", lambda e, tt=tt: e.tensor_scalar(out=msk[:, tt, :], in0=lg[:, tt, :], scalar1=top[:, tt, 1:2], scalar2=None, op0=ALU.is_ge),
                        reads=[Brt], writes=[Brt])
                tr.emit("scalar", lambda e, tt=tt: e.activation(out=ex[:, tt, :], in_=lg[:, tt, :], func=AF.Exp, bias=nm1[:, tt:tt + 1], scale=1.0),
                        reads=[Brt], writes=[Brt])
                tr.emit("vector", lambda e, tt=tt: e.tensor_tensor(out=ex[:, tt, :], in0=ex[:, tt, :], in1=msk[:, tt, :], op=ALU.mult),
                        reads=[Brt], writes=[Brt])
                tr.emit("vector", lambda e, tt=tt: e.tensor_reduce(out=den[:, tt:tt + 1], in_=ex[:, tt, :], axis=mybir.AxisListType.X, op=ALU.add),
                        reads=[Brt], writes=[Brt])
                tr.emit("vector", lambda e, tt=tt: e.reciprocal(out=den[:, tt:tt + 1], in_=den[:, tt:tt + 1]), reads=[Brt], writes=[Brt])
                tr.emit("vector", lambda e, tt=tt: e.tensor_scalar(out=comb[:, tt, :], in0=ex[:, tt, :], scalar1=den[:, tt:tt + 1], scalar2=None, op0=ALU.mult),
                        reads=[Brt], writes=[Brt])
            ffn_block(list(range(NE)))
        for g in range(2):
            tr.emit("gpsimd", lambda e, g=g: e.dma_start(out=Wgt[:, g * 4:(g + 1) * 4, :], in_=wgv[:, g * 4:(g + 1) * 4, :]),
                    writes=[Bact], dsem=s_wg)
        norm_to(V_PLE, fT, BfT)
        for oc in range(8):
            pg = pbank()
            pp = pbank()
            for c in range(8):
                tr.emit("tensor", lambda e, c=c, pg=pg, oc=oc: e.matmul(P[pg][:, :], Wgt[:, c, oc * 128:(oc + 1) * 128], fT[:, c, :],
                                                                        start=(c == 0), stop=(c == 7)),
                        reads=[Bact, BfT], writes=[PB[pg]])
            for c in range(2):
                tr.emit("tensor", lambda e, c=c, pp=pp, oc=oc: e.matmul(P[pp][:, :], Wp[:, c, oc * 128:(oc + 1) * 128], pTb[:, c, :],
                                                                        start=(c == 0), stop=(c == 1)),
                        reads=[BWp, BpT], writes=[PB[pp]])
            i2 = oc % 2
            tr.emit("scalar", lambda e, pg=pg, i2=i2: e.activation(out=sil[i2][:], in_=P[pg][:, :], func=AF.Sigmoid),
                    reads=[PB[pg]], writes=[Bsil[i2]])
            tr.emit("vector", lambda e, pp=pp, i2=i2: e.tensor_tensor(out=sil[i2][:], in0=P[pp][:, :], in1=sil[i2][:], op=ALU.mult),
                    reads=[PB[pp], Bsil[i2]], writes=[Bsil[i2]])
            tr.emit("gpsimd", lambda e, oc=oc, i2=i2: e.tensor_tensor(out=hb[:, oc, :], in0=hb[:, oc, :], in1=sil[i2][:], op=ALU.add),
                    reads=[Bhb, Bsil[i2]], writes=[Bhb])
        tr.emit("sync", lambda e, tso=tso: e.dma_start(out=hov[:, :, tso], in_=hb[:]), reads=[Bhb], writes=[DB("hdst", b)], dsem=s_h)
    _free(st)


def _consts():
    c = np.zeros((128, NCONST), np.float32)
    jp = np.arange(128)[:, None]
    j = np.arange(128)[None, :]
    c[:, C_MNEG:C_MNEG + 128] = np.where(jp >= j, -1.0, 0.0)
    c[:, C_M2NEG:C_M2NEG + 128] = np.where(jp < j, -1.0, 0.0)
    c[:, C_ONES:C_ONES + 128] = 1.0
    c[0:64, C_BLK:C_BLK + 64] = 1.0
    c[64:128, C_BLK + 64:C_BLK + 128] = 1.0
    c[:, C_IDENT:C_IDENT + 128] = np.eye(128, dtype=np.float32)
    t = np.arange(512)[None, :]
    for dj in range(4):
        s = dj * 128 + np.arange(128)[:, None]
        c[:, C_MASK + dj * 512:C_MASK + (dj + 1) * 512] = (t > s).astype(np.float32)
    return c


def _vecs(i, mix_norm_g, ffn_norm_g, ple_norm_g, attn_out_g, conv_out_g, conv_w, conv_b, q_norm_g, k_norm_g):
    v = np.zeros((128, NV), np.float32)
    v[:, V_MIX:V_MIX + 8] = mix_norm_g[i].reshape(8, 128).T
    v[:, V_FFN:V_FFN + 8] = ffn_norm_g[i].reshape(8, 128).T
    v[:, V_PLE:V_PLE + 8] = ple_norm_g[i].reshape(8, 128).T
    v[:, V_ATT:V_ATT + 4] = attn_out_g[i].reshape(4, 128).T
    v[:, V_CVG:V_CVG + 4] = conv_out_g[i].reshape(4, 128).T
    for k in range(3):
        v[:, V_CW + 4 * k:V_CW + 4 * k + 4] = conv_w[i, k].reshape(4, 128).T
    v[:, V_CB:V_CB + 4] = conv_b[i].reshape(4, 128).T
    v[:, V_QG] = np.tile(q_norm_g[i], 2)
    v[:, V_KG] = np.tile(k_norm_g[i], 2)
    return v


_PROG = []


def kernel(x, p, mix_norm_g, w_in, q_norm_g, k_norm_g, conv_w, conv_b, attn_out_g, conv_out_g, w_o,
           ffn_norm_g, dense_w1, dense_w3, dense_w2, router_w, moe_w1, moe_w3, moe_w2,
           ple_norm_g, ple_gate_w, ple_proj_w):
    f = lambda a: np.ascontiguousarray(np.asarray(a, dtype=np.float32))
    x, p = f(x), f(p)
    consts = _consts()
    cores = list(range(NCORES))
    args = (f(mix_norm_g), f(ffn_norm_g), f(ple_norm_g), f(attn_out_g), f(conv_out_g), f(conv_w), f(conv_b), f(q_norm_g), f(k_norm_g))
    vecs = np.ascontiguousarray(np.concatenate([_vecs(0, *args), _vecs(1, *args)], axis=1))
    shared = {"consts": consts, "vecs": vecs, "w_in": f(w_in), "w_o": f(w_o), "ple_gate_w": f(ple_gate_w), "ple_proj_w": f(ple_proj_w),
              "dw1": f(dense_w1), "dw3": f(dense_w3), "dw2": f(dense_w2), "mw1": f(moe_w1[0]), "mw3": f(moe_w3[0]), "mw2": f(moe_w2[0]),
              "router_w": f(router_w[0])}
    in_maps = []
    for c in cores:
        b, hf = c // 2, c % 2
        if hf == 1:
            xT = np.ascontiguousarray(x[b].T)
            pT0 = np.ascontiguousarray(p[0, b].T)
        else:
            xT = np.ascontiguousarray(np.concatenate([np.zeros((D, T), np.float32), x[b, :T].T], axis=1))
            pT0 = np.ascontiguousarray(np.concatenate([np.zeros((256, T), np.float32), p[0, b, :T].T], axis=1))
        pT1 = np.ascontiguousarray(p[1, b, hf * T:(hf + 1) * T].T)
        d = dict(shared)
        d.update({"is2": np.full((128, 1), float(hf), np.float32), "xT": xT, "pT0": pT0, "pT1": pT1})
        in_maps.append(d)
    if not _PROG:
        _PROG.append(build_fused())
    res = run_bass_kernel_spmd(_PROG[0], in_maps, core_ids=cores).results
    out = np.empty((4, 2 * T, D), np.float32)
    for c in cores:
        out[c // 2, (c % 2) * T:(c % 2 + 1) * T, :] = np.asarray(res[c]["hTo"]).T
    return out
```

```python
import numpy as np
import ml_dtypes
import concourse.bass as bass
import concourse.mybir as mybir
from concourse.bass_utils import run_bass_kernel_spmd

F32 = mybir.dt.float32
BF16 = mybir.dt.bfloat16
AF = mybir.ActivationFunctionType
ALU = mybir.AluOpType

D = 1024
T = 4096
NB = 8
TB = 512
DFF = 3584
NFF = 28
NE = 8
EPS = 1e-6
NCORES = 8

V_MIX = 0
V_FFN = 8
V_PLE = 16
V_ATT = 24
V_CVG = 28
V_CW = 32
V_CB = 44
V_QG = 48
V_KG = 49
NV = 50

C_MNEG = 0
C_M2NEG = 128
C_ONES = 256
C_BLK = 384
C_IDENT = 512
C_MASK = 640
NCONST = 640 + 2048


class SemC:
    def __init__(self, sem, unit):
        self.sem = sem
        self.unit = unit
        self.count = 0
        self.retired = False


class Buf:
    __slots__ = ("name", "last_w", "readers")

    def __init__(self, name):
        self.name = name
        self.last_w = None
        self.readers = {}


class Tracker:
    ENG = ("tensor", "scalar", "vector", "gpsimd", "sync")

    def __init__(self, nc):
        self.nc = nc
        self.streams = {e: [] for e in self.ENG}
        self.sc = {}
        self.waited = {e: {} for e in self.ENG}
        self.dma_sems = []
        self.npartial = 0
        self._dcache = {}
        self._cms = []
        self.epoch = 0
        for e in self.ENG:
            self._new_eng_sem(e)

    def _new_eng_sem(self, e):
        cm = self.nc.semaphore("s_%s_%d" % (e, self.epoch))
        s = cm.__enter__()
        self._cms.append(cm)
        self.sc[e] = SemC(s, 1)

    def maybe_epoch(self, limit=20000):
        if max(self.sc[e].count for e in self.ENG) < limit:
            return
        self.barrier()
        self.epoch += 1
        for e in self.ENG:
            self.sc[e].retired = True
            self._new_eng_sem(e)

    def new_dma_sem(self, name):
        if name in self._dcache:
            return self._dcache[name]
        cm = self.nc.semaphore("d_" + name)
        s = cm.__enter__()
        self._cms.append(cm)
        sc = SemC(s, 16)
        self.dma_sems.append(sc)
        self._dcache[name] = sc
        return sc

    def close(self):
        for cm in reversed(self._cms):
            cm.__exit__(None, None, None)

    def emit(self, eng, fn, reads=(), writes=(), dsem=None):
        deps = []
        for b in reads:
            if b.last_w is not None:
                deps.append(b.last_w)
        for b in writes:
            if b.last_w is not None:
                deps.append(b.last_w)
            deps.extend(b.readers.items())
        own = self.sc[eng]
        waits = {}
        wd = self.waited[eng]
        for sc, val in deps:
            if sc.retired:
                continue
            if sc is own and eng == "tensor" and dsem is None:
                continue
            if wd.get(sc, 0) >= val:
                continue
            if waits.get(sc, 0) < val:
                waits[sc] = val
        st = self.streams[eng]
        for sc, val in waits.items():
            if sc.unit == 16 and val < sc.count:
                self.npartial += 1
                val = sc.count
            wd[sc] = val
            st.append(("w", sc.sem, val))
        sc = dsem if dsem is not None else own
        sc.count += sc.unit
        val = sc.count
        st.append(("i", fn, sc.sem, sc.unit))
        for b in reads:
            if b.readers.get(sc, 0) < val:
                b.readers[sc] = val
        for b in writes:
            b.last_w = (sc, val)
            b.readers = {}

    def barrier(self):
        allsc = [self.sc[e] for e in self.ENG] + self.dma_sems
        for e in self.ENG:
            wd = self.waited[e]
            for sc in allsc:
                if sc.retired or (sc is self.sc[e] and e == "tensor"):
                    continue
                if sc.count > 0 and wd.get(sc, 0) < sc.count:
                    wd[sc] = sc.count
                    self.streams[e].append(("w", sc.sem, sc.count))

    def replay(self, eng_name, e):
        for it in self.streams[eng_name]:
            if it[0] == "w":
                e.wait_ge(it[1], it[2])
            else:
                it[1](e).then_inc(it[2], it[3])


class Ctx:
    pass


_UID = [0]


def _alloc(K, stack, kind, name, shape, dt):
    _UID[0] += 1
    name = "%s_u%d" % (name, _UID[0])
    cm = (K.nc.sbuf_tensor if kind == "s" else K.nc.psum_tensor)(name, shape, dt)
    t = cm.__enter__()
    stack.append(cm)
    return t


def _free(stack):
    while stack:
        stack.pop().__exit__(None, None, None)


def build_fused():
    nc = bass.Bass("TRN2", target_bir_lowering=False)
    K = Ctx()
    K.nc = nc
    dr = {}

    def dram(name, shape, dt, kind):
        dr[name] = nc.dram_tensor(name, list(shape), dt, kind=kind).ap()
        return dr[name]

    dram("consts", [128, NCONST], F32, "ExternalInput")
    dram("is2", [128, 1], F32, "ExternalInput")
    dram("vecs", [128, 2 * NV], F32, "ExternalInput")
    dram("xT", [D, 2 * T], F32, "ExternalInput")
    dram("pT0", [256, 2 * T], F32, "ExternalInput")
    dram("pT1", [256, T], F32, "ExternalInput")
    dram("w_in", [2, D, 3072], F32, "ExternalInput")
    dram("w_o", [2, D, D], F32, "ExternalInput")
    dram("ple_gate_w", [2, D, D], F32, "ExternalInput")
    dram("ple_proj_w", [2, 256, D], F32, "ExternalInput")
    dram("dw1", [1, D, DFF], F32, "ExternalInput")
    dram("dw3", [1, D, DFF], F32, "ExternalInput")
    dram("dw2", [1, DFF, D], F32, "ExternalInput")
    dram("mw1", [NE, D, DFF], F32, "ExternalInput")
    dram("mw3", [NE, D, DFF], F32, "ExternalInput")
    dram("mw2", [NE, DFF, D], F32, "ExternalInput")
    dram("router_w", [D, NE], F32, "ExternalInput")
    dram("h1T", [D, 2 * T], F32, "Internal")
    dram("qT", [128, 4, 2 * T], BF16, "Internal")
    dram("kT", [128, 4, 2 * T], BF16, "Internal")
    dram("V", [2 * T, 512], BF16, "Internal")
    dram("yT", [128, 4, 2 * T], BF16, "Internal")
    dram("attnT", [128, 4, 2 * T], BF16, "Internal")
    dram("hTo", [D, T], F32, "ExternalOutput")

    tr = Tracker(nc)
    K.tr = tr
    K.dr = dr
    K.dbuf = {}

    def DB(name, blk=0):
        k = (name, blk)
        if k not in K.dbuf:
            K.dbuf[k] = Buf("%s_%s" % k)
        return K.dbuf[k]
    K.DB = DB

    base = []
    K.PS = _alloc(K, base, "p", "psall", [128, 8, 512], F32)
    K.P = [K.PS[:, i, :] for i in range(8)]
    K.PB = [Buf("ps%d" % i) for i in range(8)]
    K.cst = _alloc(K, base, "s", "cst", [128, NCONST], F32)
    K.cstb = _alloc(K, base, "s", "cstb", [128, 512], BF16)
    K.vec2 = _alloc(K, base, "s", "vec2", [128, 2 * NV], F32)
    K.is2 = _alloc(K, base, "s", "is2s", [128, 1], F32)
    K.Bc = Buf("consts")
    sem_c = tr.new_dma_sem("c")
    tr.emit("sync", lambda e: e.dma_start(out=K.cst[:], in_=dr["consts"][:, :]), writes=[K.Bc], dsem=sem_c)
    tr.emit("sync", lambda e: e.dma_start(out=K.vec2[:], in_=dr["vecs"][:, :]), writes=[K.Bc], dsem=sem_c)
    tr.emit("sync", lambda e: e.dma_start(out=K.is2[:], in_=dr["is2"][:, :]), writes=[K.Bc], dsem=sem_c)
    tr.emit("vector", lambda e: e.tensor_copy(out=K.cstb[:], in_=K.cst[:, 0:512]), reads=[K.Bc], writes=[K.Bc])

    for layer in range(2):
        K.vec = K.vec2[:, layer * NV:(layer + 1) * NV]
        L = Ctx()
        L.layer = layer
        L.hsrc = dr["xT"] if layer == 0 else dr["h1T"]
        L.w_in = dr["w_in"][layer]
        L.qblocks = list(range(16)) if layer == 0 else list(range(8, 16))
        L.blocks = L.qblocks
        L.hdst = dr["h1T"] if layer == 0 else dr["hTo"]
        L.o_off = 0 if layer == 0 else 8
        L.pT = dr["pT0"] if layer == 0 else dr["pT1"]
        L.p_off = 0 if layer == 0 else 8
        L.w_o = dr["w_o"][layer]
        L.wg = dr["ple_gate_w"][layer]
        L.wp = dr["ple_proj_w"][layer]
        if layer == 0:
            L.w1, L.w3, L.w2 = dr["dw1"], dr["dw3"], dr["dw2"]
        else:
            L.w1, L.w3, L.w2 = dr["mw1"], dr["mw3"], dr["mw2"]
        phase_A(K, L)
        tr.barrier()
        phase_B1(K, L)
        tr.barrier()
        phase_B2(K, L)
        tr.barrier()

    with nc.Block() as block:
        @block.sync
        def _(e):
            tr.replay("sync", e)

        @block.scalar
        def _(e):
            tr.replay("scalar", e)

        @block.vector
        def _(e):
            tr.replay("vector", e)

        @block.gpsimd
        def _(e):
            tr.replay("gpsimd", e)

        @block.tensor
        def _(e):
            tr.replay("tensor", e)
    _free(base)
    tr.close()
    return nc


def rms_stats(K, src, srcbuf, nch, ncols, sq, sqbuf, inv_n, dst_rstd, dstbuf, tmp, tmpbuf, pbank, ones_ap):
    tr = K.tr
    P, PB = K.P, K.PB
    for c in range(nch):
        tr.emit("scalar", lambda e, c=c: e.activation(out=sq[:, c, 0:ncols], in_=src[:, c, 0:ncols], func=AF.Square),
                reads=[srcbuf], writes=[sqbuf])
    for c in range(nch):
        tr.emit("tensor", lambda e, c=c: e.matmul(P[pbank][:, 0:ncols], ones_ap, sq[:, c, 0:ncols],
                                                   start=(c == 0), stop=(c == nch - 1)),
                reads=[sqbuf, K.Bc], writes=[PB[pbank]])
    tr.emit("scalar", lambda e: e.activation(out=tmp[:, 0:ncols], in_=P[pbank][:, 0:ncols], func=AF.Ln,
                                             bias=EPS, scale=inv_n),
            reads=[PB[pbank], K.Bc], writes=[tmpbuf])
    tr.emit("scalar", lambda e: e.activation(out=dst_rstd[:, 0:ncols], in_=tmp[:, 0:ncols], func=AF.Exp, scale=-0.5),
            reads=[tmpbuf], writes=[dstbuf])


def setup_eps(K, stack):
    pass


def phase_A(K, L):
    nc, tr, dr, P, PB, DB = K.nc, K.tr, K.dr, K.P, K.PB, K.DB
    st = []
    setup_eps(K, st)
    Win = _alloc(K, st, "s", "Win", [128, 8, 3072], BF16)
    BWin = Buf("Win")
    hb = [_alloc(K, st, "s", "hb%d" % i, [128, 8, TB], F32) for i in range(2)]
    Bhb = [Buf("hb%d" % i) for i in range(2)]
    sq = _alloc(K, st, "s", "sq", [128, 8, TB], BF16)
    Bsq = Buf("sq")
    aT = [_alloc(K, st, "s", "aT%d" % i, [128, 8, TB], BF16) for i in range(2)]
    BaT = [Buf("aT%d" % i) for i in range(2)]
    tmp = _alloc(K, st, "s", "tmpA", [128, TB], F32)
    Btmp = Buf("tmpA")
    sqq = [_alloc(K, st, "s", "sqq%d" % i, [128, 1, TB], BF16) for i in range(2)]
    Bsqq = [Buf("sqq%d" % i) for i in range(2)]
    rsq = [_alloc(K, st, "s", "rsq%d" % i, [128, TB], F32) for i in range(2)]
    Brsq = [Buf("rsq%d" % i) for i in range(2)]
    tmq = [_alloc(K, st, "s", "tmq%d" % i, [128, TB], F32) for i in range(2)]
    Btmq = [Buf("tmq%d" % i) for i in range(2)]
    qb_ = [_alloc(K, st, "s", "qblk%d" % i, [128, 4, TB], BF16) for i in range(2)]
    Bqb = [Buf("qblk%d" % i) for i in range(2)]
    kb_ = [_alloc(K, st, "s", "kblk%d" % i, [128, 4, TB], BF16) for i in range(2)]
    Bkb = [Buf("kblk%d" % i) for i in range(2)]
    vb_ = [_alloc(K, st, "s", "vblk%d" % i, [128, 4, 512], BF16) for i in range(2)]
    Bvb = [Buf("vblk%d" % i) for i in range(2)]
    usb = [_alloc(K, st, "s", "usb%d" % i, [128, TB], F32) for i in range(2)]
    Busb = [Buf("usb%d" % i) for i in range(2)]
    cu = _alloc(K, st, "s", "cu", [128, 4, TB + 2], F32)
    Bcu = Buf("cu")
    acc = [_alloc(K, st, "s", "acc%d" % i, [128, TB], F32) for i in range(2)]
    Bacc = [Buf("acc%d" % i) for i in range(2)]
    y = _alloc(K, st, "s", "y", [128, 4, TB], F32)
    By = Buf("y")
    sqy = _alloc(K, st, "s", "sqy", [128, 4, TB], BF16)
    Bsqy = Buf("sqy")
    rsy = _alloc(K, st, "s", "rsy", [128, TB], F32)
    Brsy = Buf("rsy")
    yn = [_alloc(K, st, "s", "yn%d" % i, [128, 4, TB], BF16) for i in range(2)]
    Byn = [Buf("yn%d" % i) for i in range(2)]

    ones_bf = K.cstb[:, 256:384]
    blk_bf = K.cstb[:, 384:512]
    vec = K.vec

    sW = tr.new_dma_sem("Win")
    wv = L.w_in.rearrange("(c p) n -> p c n", p=128)
    for g in range(6):
        tr.emit("gpsimd", lambda e, g=g: e.dma_start(out=Win[:, :, g * 512:(g + 1) * 512], in_=wv[:, :, g * 512:(g + 1) * 512]),
                writes=[BWin], dsem=sW)

    hv = L.hsrc.rearrange("(c p) t -> p c t", p=128)
    s_h = [tr.new_dma_sem("hb%d" % i) for i in range(2)]
    s_q = [tr.new_dma_sem("q%d" % i) for i in range(2)]
    s_k = [tr.new_dma_sem("k%d" % i) for i in range(2)]
    s_v = [tr.new_dma_sem("v%d" % i) for i in range(2)]
    s_y = [tr.new_dma_sem("y%d" % i) for i in range(2)]

    pr = [0]

    def pbank():
        pr[0] = (pr[0] + 1) % 4
        return 2 + pr[0]

    def norm_and_aT(src, srcbuf, ncols, dst, dstbuf):
        rms_stats(K, src, srcbuf, 8, ncols, sq, Bsq, 1.0 / D, P[1], PB[1], tmp, Btmp, 0, ones_bf)
        for c in range(8):
            tr.emit("vector", lambda e, c=c: e.scalar_tensor_tensor(
                out=dst[:, c, 0:ncols], in0=src[:, c, 0:ncols], scalar=vec[:, V_MIX + c:V_MIX + c + 1],
                in1=P[1][:, 0:ncols], op0=ALU.mult, op1=ALU.mult),
                reads=[srcbuf, PB[1], K.Bc], writes=[dstbuf])

    def proj(col0, a, abuf, ncols):
        pb = pbank()
        for kc in range(8):
            tr.emit("tensor", lambda e, kc=kc, pb=pb: e.matmul(P[pb][:, 0:ncols], Win[:, kc, col0:col0 + 128], a[:, kc, 0:ncols],
                                                               start=(kc == 0), stop=(kc == 7)),
                    reads=[BWin, abuf], writes=[PB[pb]])
        return pb

    tr.emit("vector", lambda e: e.memset(cu[:, :, 0:2], 0.0), writes=[Bcu])
    def load_and_norm(bb):
        ss = bb % 2
        tss = slice(bb * TB, (bb + 1) * TB)
        tr.emit("sync", lambda e, ss=ss, tss=tss: e.dma_start(out=hb[ss][:], in_=hv[:, :, tss]),
                reads=[DB("hsrc", bb)], writes=[Bhb[ss]], dsem=s_h[ss])
        norm_and_aT(hb[ss], Bhb[ss], TB, aT[ss], BaT[ss])

    load_and_norm(0)
    for b in range(2 * NB):
        tr.maybe_epoch()
        s = b % 2
        ts = slice(b * TB, (b + 1) * TB)
        full = (L.layer == 0) or (b >= NB - 1)
        for which, dst, dbuf, gcol in (("q", qb_[s], Bqb[s], V_QG), ("k", kb_[s], Bkb[s], V_KG)):
            if which == "q" and not full:
                continue
            c0 = 0 if which == "q" else 512
            for c in range(4):
                i2 = c % 2
                pq = proj(c0 + c * 128, aT[s], BaT[s], TB)
                tr.emit("scalar", lambda e, pq=pq, i2=i2: e.activation(out=sqq[i2][:, 0, :], in_=P[pq][:, :], func=AF.Square),
                        reads=[PB[pq]], writes=[Bsqq[i2]])
                tr.emit("tensor", lambda e, i2=i2: e.matmul(P[6 + i2][:, :], blk_bf, sqq[i2][:, 0, :], start=True, stop=True),
                        reads=[Bsqq[i2], K.Bc], writes=[PB[6 + i2]])
                tr.emit("scalar", lambda e, i2=i2: e.activation(out=tmq[i2][:], in_=P[6 + i2][:, :], func=AF.Ln, bias=EPS, scale=1.0 / 64),
                        reads=[PB[6 + i2], K.Bc], writes=[Btmq[i2]])
                tr.emit("scalar", lambda e, i2=i2: e.activation(out=rsq[i2][:], in_=tmq[i2][:], func=AF.Exp, scale=-0.5),
                        reads=[Btmq[i2]], writes=[Brsq[i2]])
                tr.emit("vector", lambda e, pq=pq, i2=i2, c=c, dst=dst, gcol=gcol: e.scalar_tensor_tensor(
                    out=dst[:, c, :], in0=P[pq][:, :], scalar=vec[:, gcol:gcol + 1], in1=rsq[i2][:],
                    op0=ALU.mult, op1=ALU.mult),
                    reads=[PB[pq], Brsq[i2], K.Bc], writes=[dbuf])
        if full:
            tr.emit("sync", lambda e, s=s, ts=ts: e.dma_start(out=dr["qT"][:, :, ts], in_=qb_[s][:]),
                    reads=[Bqb[s]], writes=[DB("qT", b)], dsem=s_q[s])
        tr.emit("sync", lambda e, s=s, ts=ts: e.dma_start(out=dr["kT"][:, :, ts], in_=kb_[s][:]),
                reads=[Bkb[s]], writes=[DB("kT", b)], dsem=s_k[s])
        for tt in range(4):
            pb = pbank()
            for kc in range(8):
                tr.emit("tensor", lambda e, kc=kc, pb=pb, tt=tt, s=s: e.matmul(P[pb][:, :], aT[s][:, kc, tt * 128:(tt + 1) * 128], Win[:, kc, 1024:1536],
                                                                               start=(kc == 0), stop=(kc == 7)),
                        reads=[BWin, BaT[s]], writes=[PB[pb]])
            tr.emit("vector", lambda e, pb=pb, tt=tt, s=s: e.tensor_copy(out=vb_[s][:, tt, :], in_=P[pb][:, :]),
                    reads=[PB[pb]], writes=[Bvb[s]])
        tr.emit("sync", lambda e, s=s, b=b: e.dma_start(out=dr["V"][b * TB:(b + 1) * TB, :].rearrange("(t p) n -> p t n", p=128), in_=vb_[s][:]),
                reads=[Bvb[s]], writes=[DB("V", b)], dsem=s_v[s])
        if b + 1 < 2 * NB:
            load_and_norm(b + 1)
        if not full:
            continue
        for c in range(4):
            i2 = c % 2
            pu = proj(1536 + c * 128, aT[s], BaT[s], TB)
            pc = proj(2048 + c * 128, aT[s], BaT[s], TB)
            pg = proj(2560 + c * 128, aT[s], BaT[s], TB)
            tr.emit("scalar", lambda e, pu=pu, i2=i2: e.activation(out=usb[i2][:], in_=P[pu][:, :], func=AF.Copy),
                    reads=[PB[pu]], writes=[Busb[i2]])
            tr.emit("vector", lambda e, c=c, pc=pc, i2=i2: e.tensor_tensor(out=cu[:, c, 2:TB + 2], in0=P[pc][:, :], in1=usb[i2][:], op=ALU.mult),
                    reads=[PB[pc], Busb[i2]], writes=[Bcu])
            tr.emit("gpsimd", lambda e, c=c, i2=i2: e.tensor_scalar(out=acc[i2][:], in0=cu[:, c, 2:TB + 2],
                                                                     scalar1=vec[:, V_CW + 8 + c:V_CW + 9 + c], scalar2=vec[:, V_CB + c:V_CB + c + 1],
                                                                     op0=ALU.mult, op1=ALU.add),
                    reads=[Bcu, K.Bc], writes=[Bacc[i2]])
            tr.emit("vector", lambda e, c=c, i2=i2: e.scalar_tensor_tensor(out=acc[i2][:], in0=cu[:, c, 1:TB + 1], scalar=vec[:, V_CW + 4 + c:V_CW + 5 + c],
                                                                           in1=acc[i2][:], op0=ALU.mult, op1=ALU.add),
                    reads=[Bcu, Bacc[i2], K.Bc], writes=[Bacc[i2]])
            tr.emit("vector", lambda e, c=c, i2=i2: e.scalar_tensor_tensor(out=acc[i2][:], in0=cu[:, c, 0:TB], scalar=vec[:, V_CW + c:V_CW + 1 + c],
                                                                           in1=acc[i2][:], op0=ALU.mult, op1=ALU.add),
                    reads=[Bcu, Bacc[i2], K.Bc], writes=[Bacc[i2]])
            tr.emit("vector", lambda e, c=c, pg=pg, i2=i2: e.tensor_tensor(out=y[:, c, :], in0=P[pg][:, :], in1=acc[i2][:], op=ALU.mult),
                    reads=[PB[pg], Bacc[i2]], writes=[By])
            if b == NB - 1:
                tr.emit("gpsimd", lambda e, c=c: e.tensor_scalar(out=cu[:, c, 0:2], in0=cu[:, c, TB:TB + 2], scalar1=K.is2[:, 0:1],
                                                                  scalar2=None, op0=ALU.mult),
                        reads=[Bcu, K.Bc], writes=[Bcu])
            else:
                tr.emit("gpsimd", lambda e, c=c: e.tensor_copy(out=cu[:, c, 0:2], in_=cu[:, c, TB:TB + 2]),
                        reads=[Bcu], writes=[Bcu])
        rms_stats(K, y, By, 4, TB, sqy, Bsqy, 1.0 / 512, rsy, Brsy, tmp, Btmp, 6, ones_bf)
        for c in range(4):
            tr.emit("vector", lambda e, c=c, s=s: e.scalar_tensor_tensor(out=yn[s][:, c, :], in0=y[:, c, :], scalar=vec[:, V_CVG + c:V_CVG + c + 1],
                                                                         in1=rsy[:], op0=ALU.mult, op1=ALU.mult),
                    reads=[By, Brsy, K.Bc], writes=[Byn[s]])
        tr.emit("sync", lambda e, s=s, ts=ts: e.dma_start(out=dr["yT"][:, :, ts], in_=yn[s][:]),
                reads=[Byn[s]], writes=[DB("yT", b)], dsem=s_y[s])
    _free(st)


def phase_B1(K, L):
    nc, tr, dr, P, PB, DB = K.nc, K.tr, K.dr, K.P, K.PB, K.DB
    st = []
    setup_eps(K, st)
    NKT = 2 * T // 128
    kTa = _alloc(K, st, "s", "kTa", [128, 4, 2 * T], BF16)
    BkT = Buf("kTa")
    Va = _alloc(K, st, "s", "Va", [128, NKT, 512], BF16)
    BVa = Buf("Va")
    qz = [_alloc(K, st, "s", "qz%d" % i, [128, 4, TB], BF16) for i in range(2)]
    Bq = Buf("qz")
    tr.emit("vector", lambda e: e.memset(qz[0][64:128, :, :], 0.0), writes=[Bq])
    tr.emit("vector", lambda e: e.memset(qz[1][0:64, :, :], 0.0), writes=[Bq])
    E = [_alloc(K, st, "s", "E%d" % i, [128, 2, TB], F32) for i in range(3)]
    BE = [Buf("E%d" % i) for i in range(3)]
    Lp = [_alloc(K, st, "s", "Lp%d" % i, [128, 2, TB], BF16) for i in range(2)]
    BLp = [Buf("Lp%d" % i) for i in range(2)]
    Xe = [_alloc(K, st, "s", "Xe%d" % i, [128, 2, TB], F32) for i in range(2)]
    BXe = [Buf("Xe%d" % i) for i in range(2)]
    Aw = [_alloc(K, st, "s", "Aw%d" % i, [128, 2, TB], BF16) for i in range(2)]
    BAw = [[Buf("Aw%d%d" % (i, ch)) for ch in range(2)] for i in range(2)]
    ablk = _alloc(K, st, "s", "ablk", [128, 4, TB], F32)
    Bab = Buf("ablk")
    sqa = _alloc(K, st, "s", "sqa", [128, 4, TB], BF16)
    Bsqa = Buf("sqa")
    rsa = _alloc(K, st, "s", "rsa", [128, TB], F32)
    Brsa = Buf("rsa")
    tmp = _alloc(K, st, "s", "tmpB", [128, TB], F32)
    Btmp = Buf("tmpB")
    an = [_alloc(K, st, "s", "an%d" % i, [128, 4, TB], BF16) for i in range(2)]
    Ban = [Buf("an%d" % i) for i in range(2)]

    Mneg = K.cstb[:, 0:128]
    M2neg = K.cstb[:, 128:256]
    ones_bf = K.cstb[:, 256:384]
    vec = K.vec

    s_kv = tr.new_dma_sem("kvV")
    s_kk = tr.new_dma_sem("kvK")
    for g in range(4):
        tr.emit("sync", lambda e, g=g: e.dma_start(out=kTa[:, g, :], in_=dr["kT"][:, g, :]),
                reads=[DB("kTall")], writes=[BkT], dsem=s_kk)
    Vv = dr["V"].rearrange("(t p) n -> p t n", p=128)
    for g in range(8):
        tr.emit("sync", lambda e, g=g: e.dma_start(out=Va[:, g * 8:(g + 1) * 8, :], in_=Vv[:, g * 8:(g + 1) * 8, :]),
                reads=[DB("Vall")], writes=[BVa], dsem=s_kv)
    for g in range(4):
        tr.emit("gpsimd", lambda e, g=g: e.tensor_scalar(out=Va[:, g * 8:(g + 1) * 8, :], in0=Va[:, g * 8:(g + 1) * 8, :],
                                                          scalar1=K.is2[:, 0:1], scalar2=None, op0=ALU.mult),
                reads=[BVa, K.Bc], writes=[BVa])

    s_q = tr.new_dma_sem("qb")
    s_an = [tr.new_dma_sem("an%d" % i) for i in range(2)]
    PS = K.PS
    XB = [4, 5]
    OB = [6, 7]
    elem = ["vector", "gpsimd"]
    prs = [slice(0, 64), slice(64, 128)]

    for qb in L.qblocks:
        ts = slice(qb * TB, (qb + 1) * TB)
        tr.emit("sync", lambda e, ts=ts: e.dma_start(out=qz[0][0:64, :, :], in_=dr["qT"][0:64, :, ts]),
                reads=[DB("qT", qb)], writes=[Bq], dsem=s_q)
        tr.emit("sync", lambda e, ts=ts: e.dma_start(out=qz[1][64:128, :, :], in_=dr["qT"][64:128, :, ts]),
                reads=[DB("qT", qb)], writes=[Bq], dsem=s_q)
        own0 = qb * 4
        tiles = [(own0 + j, j) for j in (3, 2, 1, 0)] + [(kt, None) for kt in range(own0 - 1, -1, -1)]
        nst = len(tiles)
        for hp in range(4):
            def zmm(k, hp=hp):
                kt = tiles[k][0]
                for ch in range(2):
                    zb = 2 * (k % 2) + ch
                    tr.emit("tensor", lambda e, ch=ch, kt=kt, zb=zb, hp=hp: e.matmul(P[zb][:, :], kTa[:, hp, kt * 128:(kt + 1) * 128],
                                                                                     qz[ch][:, hp, :], start=True, stop=True),
                            reads=[BkT, Bq], writes=[PB[zb]])

            def expx_and_mult(k):
                sl = k % 2
                s3 = k % 3
                tr.emit("scalar", lambda e, sl=sl: e.activation(out=Xe[sl][:], in_=PS[:, 4:6, :], func=AF.Exp),
                        reads=[PB[4], PB[5]], writes=[BXe[sl]])
                for ch in range(2):
                    tr.emit("vector", lambda e, ch=ch, sl=sl, s3=s3: e.tensor_tensor(out=Aw[sl][:, ch, :], in0=E[s3][:, ch, :], in1=Xe[sl][:, ch, :], op=ALU.mult),
                            reads=[BE[s3], BXe[sl]], writes=[BAw[sl][ch]])

            def av(k, last, hp=hp):
                sl = k % 2
                pkt = tiles[k][0]
                for ch in range(2):
                    h = 2 * hp + ch
                    tr.emit("tensor", lambda e, ch=ch, sl=sl, pkt=pkt, h=h, k=k, last=last, hp=hp: e.matmul(
                        P[OB[ch]][:, :], Va[:, pkt, hp * 128:(hp + 1) * 128], Aw[sl][:, ch, :], start=(k == 0), stop=last),
                        reads=[BVa, BAw[sl][ch]], writes=[PB[OB[ch]]])

            zmm(0)
            for k in range(nst):
                tr.maybe_epoch()
                sl = k % 2
                kt, dj = tiles[k]
                if k + 1 < nst:
                    zmm(k + 1)
                s3 = k % 3
                tr.emit("scalar", lambda e, sl=sl, s3=s3: e.activation(out=E[s3][:], in_=PS[:, 2 * sl:2 * sl + 2, :], func=AF.Exp, scale=0.125),
                        reads=[PB[2 * sl], PB[2 * sl + 1]], writes=[BE[s3]])
                if dj is not None:
                    for ch in range(2):
                        tr.emit("vector", lambda e, ch=ch, s3=s3, dj=dj: e.tensor_tensor(out=E[s3][:, ch, :], in0=E[s3][:, ch, :],
                                                                                          in1=K.cst[:, C_MASK + dj * 512:C_MASK + (dj + 1) * 512], op=ALU.mult),
                                reads=[BE[s3], K.Bc], writes=[BE[s3]])
                if k > 0:
                    expx_and_mult(k - 1)
                tr.emit("scalar", lambda e, sl=sl, s3=s3: e.activation(out=Lp[sl][:], in_=E[s3][:], func=AF.Ln, bias=1.0, scale=1.0),
                        reads=[BE[s3]], writes=[BLp[sl]])
                for ch in range(2):
                    if k > 0:
                        tr.emit("tensor", lambda e, ch=ch, sl=sl: e.matmul(P[XB[ch]][:, :], M2neg, Lp[1 - sl][:, ch, :], start=False, stop=False),
                                reads=[BLp[1 - sl], K.Bc], writes=[PB[XB[ch]]])
                    tr.emit("tensor", lambda e, ch=ch, sl=sl, k=k, nst=nst: e.matmul(P[XB[ch]][:, :], Mneg, Lp[sl][:, ch, :], start=(k == 0), stop=(k == nst - 1)),
                            reads=[BLp[sl], K.Bc], writes=[PB[XB[ch]]])
                if k > 0:
                    av(k - 1, False)
            expx_and_mult(nst - 1)
            av(nst - 1, True)
            for ch in range(2):
                tr.emit("vector" if ch == 0 else "scalar",
                        (lambda e, ch=ch, hp=hp: e.tensor_copy(out=ablk[prs[ch], hp, :], in_=P[OB[ch]][prs[ch], :])) if ch == 0 else
                        (lambda e, ch=ch, hp=hp: e.activation(out=ablk[prs[ch], hp, :], in_=P[OB[ch]][prs[ch], :], func=AF.Copy)),
                        reads=[PB[OB[ch]]], writes=[Bab])
        s = qb % 2
        rms_stats(K, ablk, Bab, 4, TB, sqa, Bsqa, 1.0 / 512, rsa, Brsa, tmp, Btmp, 0, ones_bf)
        for c in range(4):
            tr.emit("vector", lambda e, c=c, s=s: e.scalar_tensor_tensor(out=an[s][:, c, :], in0=ablk[:, c, :], scalar=vec[:, V_ATT + c:V_ATT + c + 1],
                                                                         in1=rsa[:], op0=ALU.mult, op1=ALU.mult),
                    reads=[Bab, Brsa, K.Bc], writes=[Ban[s]])
        tr.emit("sync", lambda e, s=s, ts=ts: e.dma_start(out=dr["attnT"][:, :, ts], in_=an[s][:]),
                reads=[Ban[s]], writes=[DB("attnT", qb)], dsem=s_an[s])
    _free(st)


def phase_B2(K, L):
    layer = L.layer
    nc, tr, dr, P, PB, DB = K.nc, K.tr, K.dr, K.P, K.PB, K.DB
    st = []
    setup_eps(K, st)
    moe = layer == 1
    hb = _alloc(K, st, "s", "hbB", [128, 8, TB], F32)
    Bhb = Buf("hbB")
    an = _alloc(K, st, "s", "anB", [128, 4, TB], BF16)
    Ban = Buf("anB")
    yn = _alloc(K, st, "s", "ynB", [128, 4, TB], BF16)
    Byn = Buf("ynB")
    Wsq = _alloc(K, st, "s", "Wsq", [128, 8, D], BF16)
    BWsq = Buf("Wsq")
    Wp = _alloc(K, st, "s", "Wp", [128, 2, D], BF16)
    BWp = Buf("Wp")
    pTb = _alloc(K, st, "s", "pTb", [128, 2, TB], BF16)
    BpT = Buf("pTb")
    sq = _alloc(K, st, "s", "sqB", [128, 8, TB], BF16)
    Bsq = Buf("sqB")
    tmp = _alloc(K, st, "s", "tmpC", [128, TB], F32)
    Btmp = Buf("tmpC")
    fT = _alloc(K, st, "s", "fT", [128, 8, TB], BF16)
    BfT = Buf("fT")
    act = _alloc(K, st, "s", "act", [128, NFF, TB], BF16)
    Bact = Buf("act")
    W13 = [[_alloc(K, st, "s", "W%d_%d" % (w, i), [128, 8, 256], BF16) for i in range(2)] for w in range(2)]
    BW13 = [[Buf("W%d_%d" % (w, i)) for i in range(2)] for w in range(2)]
    W2 = _alloc(K, st, "s", "W2", [128, NFF, D], BF16)
    BW2 = [[Buf("W2_%d_%d" % (hf, i)) for i in range(7)] for hf in range(2)]
    sil = [_alloc(K, st, "s", "sil%d" % i, [128, TB], F32) for i in range(2)]
    Bsil = [Buf("sil%d" % i) for i in range(2)]
    if moe:
        feT = [_alloc(K, st, "s", "feT%d" % i, [128, 8, TB], BF16) for i in range(2)]
        BfeT = [Buf("feT%d" % i) for i in range(2)]
        f32t = [_alloc(K, st, "s", "f32t%d" % i, [128, 8, 128], F32) for i in range(2)]
        Bf32 = [Buf("f32t%d" % i) for i in range(2)]
        Wr = _alloc(K, st, "s", "Wr", [128, 8, NE], F32)
        BWr = Buf("Wr")
        lg = _alloc(K, st, "s", "lg", [128, 4, NE], F32)
        top = _alloc(K, st, "s", "top", [128, 4, 8], F32)
        nm1 = _alloc(K, st, "s", "nm1", [128, 4], F32)
        msk = _alloc(K, st, "s", "msk", [128, 4, NE], F32)
        ex = _alloc(K, st, "s", "ex", [128, 4, NE], F32)
        den = _alloc(K, st, "s", "den", [128, 4], F32)
        comb = _alloc(K, st, "s", "comb", [128, 4, NE], F32)
        Brt = Buf("router")
        dg = [_alloc(K, st, "s", "dg%d" % i, [128, 128], F32) for i in range(2)]
        Bdg = [Buf("dg%d" % i) for i in range(2)]

    ones_bf = K.cstb[:, 256:384]
    ones_f = K.cst[:, C_ONES:C_ONES + 128]
    ident_f = K.cst[:, C_IDENT:C_IDENT + 128]
    vec = K.vec

    s_h = tr.new_dma_sem("hB")
    s_an = tr.new_dma_sem("anB")
    s_yn = tr.new_dma_sem("ynB")
    s_p = tr.new_dma_sem("pTb")
    s_ws = tr.new_dma_sem("Wsq")
    s_wg = tr.new_dma_sem("Wgt")
    s_wp = tr.new_dma_sem("Wp")
    s_w13 = [[tr.new_dma_sem("W%d_%d" % (w, i)) for i in range(2)] for w in range(2)]
    s_w2 = [[tr.new_dma_sem("W2_%d_%d" % (hf, i)) for i in range(7)] for hf in range(2)]

    hv = L.hsrc.rearrange("(c p) t -> p c t", p=128)
    hov = L.hdst.rearrange("(c p) t -> p c t", p=128)
    wov = L.w_o.rearrange("(c p) n -> p c n", p=128)
    wgv = L.wg.rearrange("(c p) n -> p c n", p=128)
    wpv = L.wp.rearrange("(c p) n -> p c n", p=128)
    pv = L.pT.rearrange("(c p) t -> p c t", p=128)

    tr.emit("gpsimd", lambda e: e.dma_start(out=Wp[:], in_=wpv), writes=[BWp], dsem=s_wp)
    if moe:
        s_wr = tr.new_dma_sem("Wr")
        tr.emit("sync", lambda e: e.dma_start(out=Wr[:], in_=dr["router_w"].rearrange("(c p) n -> p c n", p=128)),
                writes=[BWr], dsem=s_wr)

    pr = [0]

    def pbank():
        pr[0] = (pr[0] + 1) % 6
        return 2 + pr[0]

    def load_sq(wview):
        for g in range(2):
            tr.emit("gpsimd", lambda e, g=g: e.dma_start(out=Wsq[:, g * 4:(g + 1) * 4, :], in_=wview[:, g * 4:(g + 1) * 4, :]),
                    writes=[BWsq], dsem=s_ws)

    def norm_to(gcol0, dst, dbuf):
        rms_stats(K, hb, Bhb, 8, TB, sq, Bsq, 1.0 / D, P[1], PB[1], tmp, Btmp, 0, ones_bf)
        for c in range(8):
            tr.emit("vector", lambda e, c=c: e.scalar_tensor_tensor(out=dst[:, c, :], in0=hb[:, c, :], scalar=vec[:, gcol0 + c:gcol0 + c + 1],
                                                                     in1=P[1][:, :], op0=ALU.mult, op1=ALU.mult),
                    reads=[Bhb, PB[1], K.Bc], writes=[dbuf])

    def wviews(e_idx):
        return (L.w1[e_idx].rearrange("(c p) n -> p c n", p=128), L.w3[e_idx].rearrange("(c p) n -> p c n", p=128),
                L.w2[e_idx].rearrange("(c p) n -> p c n", p=128))

    def load_w13(e_idx, grp):
        w1v, w3v, _ = wviews(e_idx)
        sl = grp % 2
        cs = slice(grp * 256, (grp + 1) * 256)
        tr.emit("gpsimd", lambda e, sl=sl, cs=cs: e.dma_start(out=W13[0][sl][:], in_=w1v[:, :, cs]), writes=[BW13[0][sl]], dsem=s_w13[0][sl])
        tr.emit("gpsimd", lambda e, sl=sl, cs=cs: e.dma_start(out=W13[1][sl][:], in_=w3v[:, :, cs]), writes=[BW13[1][sl]], dsem=s_w13[1][sl])

    def load_w2(e_idx, hf, g):
        w2v = wviews(e_idx)[2]
        tr.emit("gpsimd", lambda e, g=g, hf=hf: e.dma_start(out=W2[:, g * 4:(g + 1) * 4, hf * 512:(hf + 1) * 512],
                                                             in_=w2v[:, g * 4:(g + 1) * 4, hf * 512:(hf + 1) * 512]),
                writes=[BW2[hf][g]], dsem=s_w2[hf][g])

    def prefetch_first(e_idx):
        load_w13(e_idx, 0)
        load_w13(e_idx, 1)
        for hf in range(2):
            for g in range(7):
                load_w2(e_idx, hf, g)

    def stage1(e_idx, xin, xbuf, xg, xgbuf):
        pr[0] = 5
        for grp in range(14):
            tr.maybe_epoch()
            sl = grp % 2
            for j in range(2):
                ff = grp * 2 + j
                pg = pbank()
                pu = pbank()
                for kc in range(8):
                    tr.emit("tensor", lambda e, kc=kc, pg=pg, sl=sl, j=j: e.matmul(P[pg][:, :], W13[0][sl][:, kc, j * 128:(j + 1) * 128], xg[:, kc, :],
                                                                                   start=(kc == 0), stop=(kc == 7)),
                            reads=[BW13[0][sl], xgbuf], writes=[PB[pg]])
                for kc in range(8):
                    tr.emit("tensor", lambda e, kc=kc, pu=pu, sl=sl, j=j: e.matmul(P[pu][:, :], W13[1][sl][:, kc, j * 128:(j + 1) * 128], xin[:, kc, :],
                                                                                   start=(kc == 0), stop=(kc == 7)),
                            reads=[BW13[1][sl], xbuf], writes=[PB[pu]])
                i2 = ff % 2
                tr.emit("scalar", lambda e, pg=pg, i2=i2: e.activation(out=sil[i2][:], in_=P[pg][:, :], func=AF.Silu),
                        reads=[PB[pg]], writes=[Bsil[i2]])
                tr.emit("vector", lambda e, pu=pu, i2=i2, ff=ff: e.tensor_tensor(out=act[:, ff, :], in0=P[pu][:, :], in1=sil[i2][:], op=ALU.mult),
                        reads=[PB[pu], Bsil[i2]], writes=[Bact])
            if grp + 2 < 14:
                load_w13(e_idx, grp + 2)

    def stage2_pass(hf, nxt):
        for g in range(7):
            for f4 in range(4):
                ff = g * 4 + f4
                for o4 in range(4):
                    oc = hf * 4 + o4
                    tr.emit("tensor", lambda e, ff=ff, o4=o4, oc=oc: e.matmul(P[4 + o4][:, :], W2[:, ff, oc * 128:(oc + 1) * 128], act[:, ff, :],
                                                                              start=(ff == 0), stop=(ff == NFF - 1)),
                            reads=[BW2[hf][g], Bact], writes=[PB[4 + o4]])
            if nxt is not None:
                load_w2(nxt, hf, g)
        for o4 in range(4):
            oc = hf * 4 + o4
            tr.emit("vector", lambda e, o4=o4, oc=oc: e.tensor_tensor(out=hb[:, oc, :], in0=P[4 + o4][:, :], in1=hb[:, oc, :], op=ALU.add),
                    reads=[PB[4 + o4], Bhb], writes=[Bhb])

    def prep_fe(ei):
        for tt in range(4):
            i2 = tt % 2
            tr.emit("vector", lambda e, tt=tt, i2=i2, ei=ei: e.tensor_scalar(out=dg[i2][:], in0=ident_f, scalar1=comb[:, tt, ei:ei + 1],
                                                                             scalar2=None, op0=ALU.mult),
                    reads=[Brt, K.Bc], writes=[Bdg[i2]])
            tr.emit("tensor", lambda e, tt=tt, i2=i2: e.matmul(P[0][:, tt * 128:(tt + 1) * 128], ones_f, dg[i2][:], start=True, stop=True),
                    reads=[Bdg[i2], K.Bc], writes=[PB[0]])
        fb = ei % 2
        for c in range(8):
            tr.emit("vector", lambda e, c=c, fb=fb: e.tensor_tensor(out=feT[fb][:, c, :], in0=P[0][:, :], in1=fT[:, c, :], op=ALU.mult),
                    reads=[PB[0], BfT], writes=[BfeT[fb]])

    def ffn_block(experts):
        for n, ei in enumerate(experts):
            nxt = experts[n + 1] if n + 1 < len(experts) else None
            if moe:
                if n == 0:
                    prep_fe(ei)
                stage1(ei, feT[ei % 2], BfeT[ei % 2], fT, BfT)
            else:
                stage1(ei, fT, BfT, fT, BfT)
            if nxt is not None:
                load_w13(nxt, 0)
                load_w13(nxt, 1)
            stage2_pass(0, nxt)
            if moe and nxt is not None:
                prep_fe(nxt)
            stage2_pass(1, nxt)

    load_sq(wov)
    Wgt = act[:, 0:16, :].rearrange("p (a b) c -> p a (b c)", b=2)

    for b in L.blocks:
        ts = slice(b * TB, (b + 1) * TB)
        tsp = slice((b - L.p_off) * TB, (b - L.p_off + 1) * TB)
        tso = slice((b - L.o_off) * TB, (b - L.o_off + 1) * TB)
        tr.emit("sync", lambda e, ts=ts: e.dma_start(out=hb[:], in_=hv[:, :, ts]), reads=[DB("hsrc", b)], writes=[Bhb], dsem=s_h)
        tr.emit("sync", lambda e, ts=ts: e.dma_start(out=an[:], in_=dr["attnT"][:, :, ts]), reads=[DB("attnT", b)], writes=[Ban], dsem=s_an)
        tr.emit("sync", lambda e, ts=ts: e.dma_start(out=yn[:], in_=dr["yT"][:, :, ts]), reads=[DB("yT", b)], writes=[Byn], dsem=s_yn)
        tr.emit("gpsimd", lambda e, tsp=tsp: e.dma_start(out=pTb[:], in_=pv[:, :, tsp]), writes=[BpT], dsem=s_p)
        prefetch_first(0)
        for oc in range(8):
            pb = pbank()
            for c in range(4):
                tr.emit("tensor", lambda e, c=c, pb=pb, oc=oc: e.matmul(P[pb][:, :], Wsq[:, c, oc * 128:(oc + 1) * 128], an[:, c, :],
                                                                        start=(c == 0), stop=False),
                        reads=[BWsq, Ban], writes=[PB[pb]])
            for c in range(4):
                tr.emit("tensor", lambda e, c=c, pb=pb, oc=oc: e.matmul(P[pb][:, :], Wsq[:, 4 + c, oc * 128:(oc + 1) * 128], yn[:, c, :],
                                                                        start=False, stop=(c == 3)),
                        reads=[BWsq, Byn], writes=[PB[pb]])
            tr.emit("vector", lambda e, pb=pb, oc=oc: e.tensor_tensor(out=hb[:, oc, :], in0=P[pb][:, :], in1=hb[:, oc, :], op=ALU.add),
                    reads=[PB[pb], Bhb], writes=[Bhb])
        norm_to(V_FFN, fT, BfT)
        if not moe:
            ffn_block([0])
        else:
            for tt in range(4):
                i2 = tt % 2
                for c in range(8):
                    tr.emit("vector", lambda e, c=c, tt=tt, i2=i2: e.scalar_tensor_tensor(
                        out=f32t[i2][:, c, :], in0=hb[:, c, tt * 128:(tt + 1) * 128], scalar=vec[:, V_FFN + c:V_FFN + c + 1],
                        in1=P[1][:, tt * 128:(tt + 1) * 128], op0=ALU.mult, op1=ALU.mult),
                        reads=[Bhb, PB[1], K.Bc], writes=[Bf32[i2]])
                pb = pbank()
                for c in range(8):
                    tr.emit("tensor", lambda e, c=c, pb=pb, i2=i2: e.matmul(P[pb][:, 0:NE], f32t[i2][:, c, :], Wr[:, c, :], start=(c == 0), stop=(c == 7)),
                            reads=[Bf32[i2], BWr], writes=[PB[pb]])
                tr.emit("vector", lambda e, pb=pb, tt=tt: e.tensor_copy(out=lg[:, tt, :], in_=P[pb][:, 0:NE]), reads=[PB[pb]], writes=[Brt])
                tr.emit("vector", lambda e, tt=tt: e.max(out=top[:, tt, :], in_=lg[:, tt, :]), reads=[Brt], writes=[Brt])
                tr.emit("vector", lambda e, tt=tt: e.tensor_scalar(out=nm1[:, tt:tt + 1], in0=top[:, tt, 0:1], scalar1=-1.0, scalar2=None, op0=ALU.mult),
                        reads=[Brt], writes=[Brt])
                tr.emit("vector", lambda e, tt=tt: e.tensor_scalar(out=msk[:, tt, :], in0=lg[:, tt, :], scalar1=top[:, tt, 1:2], scalar2=None, op0=ALU.is_ge),
                        reads=[Brt], writes=[Brt])
                tr.emit("scalar", lambda e, tt=tt: e.activation(out=ex[:, tt, :], in_=lg[:, tt, :], func=AF.Exp, bias=nm1[:, tt:tt + 1], scale=1.0),
                        reads=[Brt], writes=[Brt])
                tr.emit("vector", lambda e, tt=tt: e.tensor_tensor(out=ex[:, tt, :], in0=ex[:, tt, :], in1=msk[:, tt, :], op=ALU.mult),
                        reads=[Brt], writes=[Brt])
                tr.emit("vector", lambda e, tt=tt: e.tensor_reduce(out=den[:, tt:tt + 1], in_=ex[:, tt, :], axis=mybir.AxisListType.X, op=ALU.add),
                        reads=[Brt], writes=[Brt])
                tr.emit("vector", lambda e, tt=tt: e.reciprocal(out=den[:, tt:tt + 1], in_=den[:, tt:tt + 1]), reads=[Brt], writes=[Brt])
                tr.emit("vector", lambda e, tt=tt: e.tensor_scalar(out=comb[:, tt, :], in0=ex[:, tt, :], scalar1=den[:, tt:tt + 1], scalar2=None, op0=ALU.mult),
                        reads=[Brt], writes=[Brt])
            ffn_block(list(range(NE)))
        for g in range(2):
            tr.emit("gpsimd", lambda e, g=g: e.dma_start(out=Wgt[:, g * 4:(g + 1) * 4, :], in_=wgv[:, g * 4:(g + 1) * 4, :]),
                    writes=[Bact], dsem=s_wg)
        norm_to(V_PLE, fT, BfT)
        for oc in range(8):
            pg = pbank()
            pp = pbank()
            for c in range(8):
                tr.emit("tensor", lambda e, c=c, pg=pg, oc=oc: e.matmul(P[pg][:, :], Wgt[:, c, oc * 128:(oc + 1) * 128], fT[:, c, :],
                                                                        start=(c == 0), stop=(c == 7)),
                        reads=[Bact, BfT], writes=[PB[pg]])
            for c in range(2):
                tr.emit("tensor", lambda e, c=c, pp=pp, oc=oc: e.matmul(P[pp][:, :], Wp[:, c, oc * 128:(oc + 1) * 128], pTb[:, c, :],
                                                                        start=(c == 0), stop=(c == 1)),
                        reads=[BWp, BpT], writes=[PB[pp]])
            i2 = oc % 2
            tr.emit("scalar", lambda e, pg=pg, i2=i2: e.activation(out=sil[i2][:], in_=P[pg][:, :], func=AF.Sigmoid),
                    reads=[PB[pg]], writes=[Bsil[i2]])
            tr.emit("vector", lambda e, pp=pp, i2=i2: e.tensor_tensor(out=sil[i2][:], in0=P[pp][:, :], in1=sil[i2][:], op=ALU.mult),
                    reads=[PB[pp], Bsil[i2]], writes=[Bsil[i2]])
            tr.emit("gpsimd", lambda e, oc=oc, i2=i2: e.tensor_tensor(out=hb[:, oc, :], in0=hb[:, oc, :], in1=sil[i2][:], op=ALU.add),
                    reads=[Bhb, Bsil[i2]], writes=[Bhb])
        tr.emit("sync", lambda e, tso=tso: e.dma_start(out=hov[:, :, tso], in_=hb[:]), reads=[Bhb], writes=[DB("hdst", b)], dsem=s_h)
    _free(st)


def _consts():
    c = np.zeros((128, NCONST), np.float32)
    jp = np.arange(128)[:, None]
    j = np.arange(128)[None, :]
    c[:, C_MNEG:C_MNEG + 128] = np.where(jp >= j, -1.0, 0.0)
    c[:, C_M2NEG:C_M2NEG + 128] = np.where(jp < j, -1.0, 0.0)
    c[:, C_ONES:C_ONES + 128] = 1.0
    c[0:64, C_BLK:C_BLK + 64] = 1.0
    c[64:128, C_BLK + 64:C_BLK + 128] = 1.0
    c[:, C_IDENT:C_IDENT + 128] = np.eye(128, dtype=np.float32)
    t = np.arange(512)[None, :]
    for dj in range(4):
        s = dj * 128 + np.arange(128)[:, None]
        c[:, C_MASK + dj * 512:C_MASK + (dj + 1) * 512] = (t > s).astype(np.float32)
    return c


def _vecs(i, mix_norm_g, ffn_norm_g, ple_norm_g, attn_out_g, conv_out_g, conv_w, conv_b, q_norm_g, k_norm_g):
    v = np.zeros((128, NV), np.float32)
    v[:, V_MIX:V_MIX + 8] = mix_norm_g[i].reshape(8, 128).T
    v[:, V_FFN:V_FFN + 8] = ffn_norm_g[i].reshape(8, 128).T
    v[:, V_PLE:V_PLE + 8] = ple_norm_g[i].reshape(8, 128).T
    v[:, V_ATT:V_ATT + 4] = attn_out_g[i].reshape(4, 128).T
    v[:, V_CVG:V_CVG + 4] = conv_out_g[i].reshape(4, 128).T
    for k in range(3):
        v[:, V_CW + 4 * k:V_CW + 4 * k + 4] = conv_w[i, k].reshape(4, 128).T
    v[:, V_CB:V_CB + 4] = conv_b[i].reshape(4, 128).T
    v[:, V_QG] = np.tile(q_norm_g[i], 2)
    v[:, V_KG] = np.tile(k_norm_g[i], 2)
    return v


_PROG = []


def kernel(x, p, mix_norm_g, w_in, q_norm_g, k_norm_g, conv_w, conv_b, attn_out_g, conv_out_g, w_o,
           ffn_norm_g, dense_w1, dense_w3, dense_w2, router_w, moe_w1, moe_w3, moe_w2,
           ple_norm_g, ple_gate_w, ple_proj_w):
    f = lambda a: np.ascontiguousarray(np.asarray(a, dtype=np.float32))
    x, p = f(x), f(p)
    consts = _consts()
    cores = list(range(NCORES))
    args = (f(mix_norm_g), f(ffn_norm_g), f(ple_norm_g), f(attn_out_g), f(conv_out_g), f(conv_w), f(conv_b), f(q_norm_g), f(k_norm_g))
    vecs = np.ascontiguousarray(np.concatenate([_vecs(0, *args), _vecs(1, *args)], axis=1))
    shared = {"consts": consts, "vecs": vecs, "w_in": f(w_in), "w_o": f(w_o), "ple_gate_w": f(ple_gate_w), "ple_proj_w": f(ple_proj_w),
              "dw1": f(dense_w1), "dw3": f(dense_w3), "dw2": f(dense_w2), "mw1": f(moe_w1[0]), "mw3": f(moe_w3[0]), "mw2": f(moe_w2[0]),
              "router_w": f(router_w[0])}
    in_maps = []
    for c in cores:
        b, hf = c // 2, c % 2
        if hf == 1:
            xT = np.ascontiguousarray(x[b].T)
            pT0 = np.ascontiguousarray(p[0, b].T)
        else:
            xT = np.ascontiguousarray(np.concatenate([np.zeros((D, T), np.float32), x[b, :T].T], axis=1))
            pT0 = np.ascontiguousarray(np.concatenate([np.zeros((256, T), np.float32), p[0, b, :T].T], axis=1))
        pT1 = np.ascontiguousarray(p[1, b, hf * T:(hf + 1) * T].T)
        d = dict(shared)
        d.update({"is2": np.full((128, 1), float(hf), np.float32), "xT": xT, "pT0": pT0, "pT1": pT1})
        in_maps.append(d)
    if not _PROG:
        _PROG.append(build_fused())
    res = run_bass_kernel_spmd(_PROG[0], in_maps, core_ids=cores).results
    out = np.empty((4, 2 * T, D), np.float32)
    for c in cores:
        out[c // 2, (c % 2) * T:(c % 2 + 1) * T, :] = np.asarray(res[c]["hTo"]).T
    return out
```

```python
import numpy as np
import ml_dtypes
import concourse.bass as bass
import concourse.mybir as mybir
from concourse.bass_utils import run_bass_kernel_spmd

F32 = mybir.dt.float32
BF16 = mybir.dt.bfloat16
AF = mybir.ActivationFunctionType
ALU = mybir.AluOpType

D = 1024
T = 4096
NB = 8
TB = 512
DFF = 3584
NFF = 28
NE = 8
EPS = 1e-6
NCORES = 8

V_MIX = 0
V_FFN = 8
V_PLE = 16
V_ATT = 24
V_CVG = 28
V_CW = 32
V_CB = 44
V_QG = 48
V_KG = 49
NV = 50

C_MNEG = 0
C_M2NEG = 128
C_ONES = 256
C_BLK = 384
C_IDENT = 512
C_MASK = 640
NCONST = 640 + 2048


class SemC:
    def __init__(self, sem, unit):
        self.sem = sem
        self.unit = unit
        self.count = 0
        self.retired = False


class Buf:
    __slots__ = ("name", "last_w", "readers")

    def __init__(self, name):
        self.name = name
        self.last_w = None
        self.readers = {}


class Tracker:
    ENG = ("tensor", "scalar", "vector", "gpsimd", "sync")

    def __init__(self, nc):
        self.nc = nc
        self.streams = {e: [] for e in self.ENG}
        self.sc = {}
        self.waited = {e: {} for e in self.ENG}
        self.dma_sems = []
        self.npartial = 0
        self._dcache = {}
        self._cms = []
        self.epoch = 0
        for e in self.ENG:
            self._new_eng_sem(e)

    def _new_eng_sem(self, e):
        cm = self.nc.semaphore("s_%s_%d" % (e, self.epoch))
        s = cm.__enter__()
        self._cms.append(cm)
        self.sc[e] = SemC(s, 1)

    def maybe_epoch(self, limit=20000):
        if max(self.sc[e].count for e in self.ENG) < limit:
            return
        self.barrier()
        self.epoch += 1
        for e in self.ENG:
            self.sc[e].retired = True
            self._new_eng_sem(e)

    def new_dma_sem(self, name):
        if name in self._dcache:
            return self._dcache[name]
        cm = self.nc.semaphore("d_" + name)
        s = cm.__enter__()
        self._cms.append(cm)
        sc = SemC(s, 16)
        self.dma_sems.append(sc)
        self._dcache[name] = sc
        return sc

    def close(self):
        for cm in reversed(self._cms):
            cm.__exit__(None, None, None)

    def emit(self, eng, fn, reads=(), writes=(), dsem=None):
        deps = []
        for b in reads:
            if b.last_w is not None:
                deps.append(b.last_w)
        for b in writes:
            if b.last_w is not None:
                deps.append(b.last_w)
            deps.extend(b.readers.items())
        own = self.sc[eng]
        waits = {}
        wd = self.waited[eng]
        for sc, val in deps:
            if sc.retired:
                continue
            if sc is own and eng == "tensor" and dsem is None:
                continue
            if wd.get(sc, 0) >= val:
                continue
            if waits.get(sc, 0) < val:
                waits[sc] = val
        st = self.streams[eng]
        for sc, val in waits.items():
            if sc.unit == 16 and val < sc.count:
                self.npartial += 1
                val = sc.count
            wd[sc] = val
            st.append(("w", sc.sem, val))
        sc = dsem if dsem is not None else own
        sc.count += sc.unit
        val = sc.count
        st.append(("i", fn, sc.sem, sc.unit))
        for b in reads:
            if b.readers.get(sc, 0) < val:
                b.readers[sc] = val
        for b in writes:
            b.last_w = (sc, val)
            b.readers = {}

    def barrier(self):
        allsc = [self.sc[e] for e in self.ENG] + self.dma_sems
        for e in self.ENG:
            wd = self.waited[e]
            for sc in allsc:
                if sc.retired or (sc is self.sc[e] and e == "tensor"):
                    continue
                if sc.count > 0 and wd.get(sc, 0) < sc.count:
                    wd[sc] = sc.count
                    self.streams[e].append(("w", sc.sem, sc.count))

    def replay(self, eng_name, e):
        for it in self.streams[eng_name]:
            if it[0] == "w":
                e.wait_ge(it[1], it[2])
            else:
                it[1](e).then_inc(it[2], it[3])


class Ctx:
    pass


_UID = [0]


def _alloc(K, stack, kind, name, shape, dt):
    _UID[0] += 1
    name = "%s_u%d" % (name, _UID[0])
    cm = (K.nc.sbuf_tensor if kind == "s" else K.nc.psum_tensor)(name, shape, dt)
    t = cm.__enter__()
    stack.append(cm)
    return t


def _free(stack):
    while stack:
        stack.pop().__exit__(None, None, None)


def build_fused():
    nc = bass.Bass("TRN2", target_bir_lowering=False)
    K = Ctx()
    K.nc = nc
    dr = {}

    def dram(name, shape, dt, kind):
        dr[name] = nc.dram_tensor(name, list(shape), dt, kind=kind).ap()
        return dr[name]

    dram("consts", [128, NCONST], F32, "ExternalInput")
    dram("is2", [128, 1], F32, "ExternalInput")
    dram("vecs", [128, 2 * NV], F32, "ExternalInput")
    dram("xT", [D, 2 * T], F32, "ExternalInput")
    dram("pT0", [256, 2 * T], F32, "ExternalInput")
    dram("pT1", [256, T], F32, "ExternalInput")
    dram("w_in", [2, D, 3072], F32, "ExternalInput")
    dram("w_o", [2, D, D], F32, "ExternalInput")
    dram("ple_gate_w", [2, D, D], F32, "ExternalInput")
    dram("ple_proj_w", [2, 256, D], F32, "ExternalInput")
    dram("dw1", [1, D, DFF], F32, "ExternalInput")
    dram("dw3", [1, D, DFF], F32, "ExternalInput")
    dram("dw2", [1, DFF, D], F32, "ExternalInput")
    dram("mw1", [NE, D, DFF], F32, "ExternalInput")
    dram("mw3", [NE, D, DFF], F32, "ExternalInput")
    dram("mw2", [NE, DFF, D], F32, "ExternalInput")
    dram("router_w", [D, NE], F32, "ExternalInput")
    dram("h1T", [D, 2 * T], F32, "Internal")
    dram("qT", [128, 4, 2 * T], BF16, "Internal")
    dram("kT", [128, 4, 2 * T], BF16, "Internal")
    dram("V", [2 * T, 512], BF16, "Internal")
    dram("yT", [128, 4, 2 * T], BF16, "Internal")
    dram("attnT", [128, 4, 2 * T], BF16, "Internal")
    dram("hTo", [D, T], F32, "ExternalOutput")

    tr = Tracker(nc)
    K.tr = tr
    K.dr = dr
    K.dbuf = {}

    def DB(name, blk=0):
        k = (name, blk)
        if k not in K.dbuf:
            K.dbuf[k] = Buf("%s_%s" % k)
        return K.dbuf[k]
    K.DB = DB

    base = []
    K.PS = _alloc(K, base, "p", "psall", [128, 8, 512], F32)
    K.P = [K.PS[:, i, :] for i in range(8)]
    K.PB = [Buf("ps%d" % i) for i in range(8)]
    K.cst = _alloc(K, base, "s", "cst", [128, NCONST], F32)
    K.cstb = _alloc(K, base, "s", "cstb", [128, 512], BF16)
    K.vec2 = _alloc(K, base, "s", "vec2", [128, 2 * NV], F32)
    K.is2 = _alloc(K, base, "s", "is2s", [128, 1], F32)
    K.Bc = Buf("consts")
    sem_c = tr.new_dma_sem("c")
    tr.emit("sync", lambda e: e.dma_start(out=K.cst[:], in_=dr["consts"][:, :]), writes=[K.Bc], dsem=sem_c)
    tr.emit("sync", lambda e: e.dma_start(out=K.vec2[:], in_=dr["vecs"][:, :]), writes=[K.Bc], dsem=sem_c)
    tr.emit("sync", lambda e: e.dma_start(out=K.is2[:], in_=dr["is2"][:, :]), writes=[K.Bc], dsem=sem_c)
    tr.emit("vector", lambda e: e.tensor_copy(out=K.cstb[:], in_=K.cst[:, 0:512]), reads=[K.Bc], writes=[K.Bc])

    for layer in range(2):
        K.vec = K.vec2[:, layer * NV:(layer + 1) * NV]
        L = Ctx()
        L.layer = layer
        L.hsrc = dr["xT"] if layer == 0 else dr["h1T"]
        L.w_in = dr["w_in"][layer]
        L.qblocks = list(range(16)) if layer == 0 else list(range(8, 16))
        L.blocks = L.qblocks
        L.hdst = dr["h1T"] if layer == 0 else dr["hTo"]
        L.o_off = 0 if layer == 0 else 8
        L.pT = dr["pT0"] if layer == 0 else dr["pT1"]
        L.p_off = 0 if layer == 0 else 8
        L.w_o = dr["w_o"][layer]
        L.wg = dr["ple_gate_w"][layer]
        L.wp = dr["ple_proj_w"][layer]
        if layer == 0:
            L.w1, L.w3, L.w2 = dr["dw1"], dr["dw3"], dr["dw2"]
        else:
            L.w1, L.w3, L.w2 = dr["mw1"], dr["mw3"], dr["mw2"]
        phase_A(K, L)
        tr.barrier()
        phase_B1(K, L)
        tr.barrier()
        phase_B2(K, L)
        tr.barrier()

    with nc.Block() as block:
        @block.sync
        def _(e):
            tr.replay("sync", e)

        @block.scalar
        def _(e):
            tr.replay("scalar", e)

        @block.vector
        def _(e):
            tr.replay("vector", e)

        @block.gpsimd
        def _(e):
            tr.replay("gpsimd", e)

        @block.tensor
        def _(e):
            tr.replay("tensor", e)
    _free(base)
    tr.close()
    return nc


def rms_stats(K, src, srcbuf, nch, ncols, sq, sqbuf, inv_n, dst_rstd, dstbuf, tmp, tmpbuf, pbank, ones_ap):
    tr = K.tr
    P, PB = K.P, K.PB
    for c in range(nch):
        tr.emit("scalar", lambda e, c=c: e.activation(out=sq[:, c, 0:ncols], in_=src[:, c, 0:ncols], func=AF.Square),
                reads=[srcbuf], writes=[sqbuf])
    for c in range(nch):
        tr.emit("tensor", lambda e, c=c: e.matmul(P[pbank][:, 0:ncols], ones_ap, sq[:, c, 0:ncols],
                                                   start=(c == 0), stop=(c == nch - 1)),
                reads=[sqbuf, K.Bc], writes=[PB[pbank]])
    tr.emit("scalar", lambda e: e.activation(out=tmp[:, 0:ncols], in_=P[pbank][:, 0:ncols], func=AF.Ln,
                                             bias=EPS, scale=inv_n),
            reads=[PB[pbank], K.Bc], writes=[tmpbuf])
    tr.emit("scalar", lambda e: e.activation(out=dst_rstd[:, 0:ncols], in_=tmp[:, 0:ncols], func=AF.Exp, scale=-0.5),
            reads=[tmpbuf], writes=[dstbuf])


def setup_eps(K, stack):
    pass


def phase_A(K, L):
    nc, tr, dr, P, PB, DB = K.nc, K.tr, K.dr, K.P, K.PB, K.DB
    st = []
    setup_eps(K, st)
    Win = _alloc(K, st, "s", "Win", [128, 8, 3072], BF16)
    BWin = Buf("Win")
    hb = [_alloc(K, st, "s", "hb%d" % i, [128, 8, TB], F32) for i in range(2)]
    Bhb = [Buf("hb%d" % i) for i in range(2)]
    sq = _alloc(K, st, "s", "sq", [128, 8, TB], BF16)
    Bsq = Buf("sq")
    aT = [_alloc(K, st, "s", "aT%d" % i, [128, 8, TB], BF16) for i in range(2)]
    BaT = [Buf("aT%d" % i) for i in range(2)]
    tmp = _alloc(K, st, "s", "tmpA", [128, TB], F32)
    Btmp = Buf("tmpA")
    sqq = [_alloc(K, st, "s", "sqq%d" % i, [128, 1, TB], BF16) for i in range(2)]
    Bsqq = [Buf("sqq%d" % i) for i in range(2)]
    rsq = [_alloc(K, st, "s", "rsq%d" % i, [128, TB], F32) for i in range(2)]
    Brsq = [Buf("rsq%d" % i) for i in range(2)]
    tmq = [_alloc(K, st, "s", "tmq%d" % i, [128, TB], F32) for i in range(2)]
    Btmq = [Buf("tmq%d" % i) for i in range(2)]
    qb_ = [_alloc(K, st, "s", "qblk%d" % i, [128, 4, TB], BF16) for i in range(2)]
    Bqb = [Buf("qblk%d" % i) for i in range(2)]
    kb_ = [_alloc(K, st, "s", "kblk%d" % i, [128, 4, TB], BF16) for i in range(2)]
    Bkb = [Buf("kblk%d" % i) for i in range(2)]
    vb_ = [_alloc(K, st, "s", "vblk%d" % i, [128, 4, 512], BF16) for i in range(2)]
    Bvb = [Buf("vblk%d" % i) for i in range(2)]
    usb = [_alloc(K, st, "s", "usb%d" % i, [128, TB], F32) for i in range(2)]
    Busb = [Buf("usb%d" % i) for i in range(2)]
    cu = _alloc(K, st, "s", "cu", [128, 4, TB + 2], F32)
    Bcu = Buf("cu")
    acc = [_alloc(K, st, "s", "acc%d" % i, [128, TB], F32) for i in range(2)]
    Bacc = [Buf("acc%d" % i) for i in range(2)]
    y = _alloc(K, st, "s", "y", [128, 4, TB], F32)
    By = Buf("y")
    sqy = _alloc(K, st, "s", "sqy", [128, 4, TB], BF16)
    Bsqy = Buf("sqy")
    rsy = _alloc(K, st, "s", "rsy", [128, TB], F32)
    Brsy = Buf("rsy")
    yn = [_alloc(K, st, "s", "yn%d" % i, [128, 4, TB], BF16) for i in range(2)]
    Byn = [Buf("yn%d" % i) for i in range(2)]

    ones_bf = K.cstb[:, 256:384]
    blk_bf = K.cstb[:, 384:512]
    vec = K.vec

    sW = tr.new_dma_sem("Win")
    wv = L.w_in.rearrange("(c p) n -> p c n", p=128)
    for g in range(6):
        tr.emit("gpsimd", lambda e, g=g: e.dma_start(out=Win[:, :, g * 512:(g + 1) * 512], in_=wv[:, :, g * 512:(g + 1) * 512]),
                writes=[BWin], dsem=sW)

    hv = L.hsrc.rearrange("(c p) t -> p c t", p=128)
    s_h = [tr.new_dma_sem("hb%d" % i) for i in range(2)]
    s_q = [tr.new_dma_sem("q%d" % i) for i in range(2)]
    s_k = [tr.new_dma_sem("k%d" % i) for i in range(2)]
    s_v = [tr.new_dma_sem("v%d" % i) for i in range(2)]
    s_y = [tr.new_dma_sem("y%d" % i) for i in range(2)]

    pr = [0]

    def pbank():
        pr[0] = (pr[0] + 1) % 4
        return 2 + pr[0]

    def norm_and_aT(src, srcbuf, ncols, dst, dstbuf):
        rms_stats(K, src, srcbuf, 8, ncols, sq, Bsq, 1.0 / D, P[1], PB[1], tmp, Btmp, 0, ones_bf)
        for c in range(8):
            tr.emit("vector", lambda e, c=c: e.scalar_tensor_tensor(
                out=dst[:, c, 0:ncols], in0=src[:, c, 0:ncols], scalar=vec[:, V_MIX + c:V_MIX + c + 1],
                in1=P[1][:, 0:ncols], op0=ALU.mult, op1=ALU.mult),
                reads=[srcbuf, PB[1], K.Bc], writes=[dstbuf])

    def proj(col0, a, abuf, ncols):
        pb = pbank()
        for kc in range(8):
            tr.emit("tensor", lambda e, kc=kc, pb=pb: e.matmul(P[pb][:, 0:ncols], Win[:, kc, col0:col0 + 128], a[:, kc, 0:ncols],
                                                               start=(kc == 0), stop=(kc == 7)),
                    reads=[BWin, abuf], writes=[PB[pb]])
        return pb

    tr.emit("vector", lambda e: e.memset(cu[:, :, 0:2], 0.0), writes=[Bcu])
    for b in range(2 * NB):
        tr.maybe_epoch()
        s = b % 2
        ts = slice(b * TB, (b + 1) * TB)
        tr.emit("sync", lambda e, s=s, ts=ts: e.dma_start(out=hb[s][:], in_=hv[:, :, ts]),
                reads=[DB("hsrc", b)], writes=[Bhb[s]], dsem=s_h[s])
        norm_and_aT(hb[s], Bhb[s], TB, aT[s], BaT[s])
        full = (L.layer == 0) or (b >= NB - 1)
        for which, dst, dbuf, gcol in (("q", qb_[s], Bqb[s], V_QG), ("k", kb_[s], Bkb[s], V_KG)):
            if which == "q" and not full:
                continue
            c0 = 0 if which == "q" else 512
            for c in range(4):
                i2 = c % 2
                pq = proj(c0 + c * 128, aT[s], BaT[s], TB)
                tr.emit("scalar", lambda e, pq=pq, i2=i2: e.activation(out=sqq[i2][:, 0, :], in_=P[pq][:, :], func=AF.Square),
                        reads=[PB[pq]], writes=[Bsqq[i2]])
                tr.emit("tensor", lambda e, i2=i2: e.matmul(P[6 + i2][:, :], blk_bf, sqq[i2][:, 0, :], start=True, stop=True),
                        reads=[Bsqq[i2], K.Bc], writes=[PB[6 + i2]])
                tr.emit("scalar", lambda e, i2=i2: e.activation(out=tmq[i2][:], in_=P[6 + i2][:, :], func=AF.Ln, bias=EPS, scale=1.0 / 64),
                        reads=[PB[6 + i2], K.Bc], writes=[Btmq[i2]])
                tr.emit("scalar", lambda e, i2=i2: e.activation(out=rsq[i2][:], in_=tmq[i2][:], func=AF.Exp, scale=-0.5),
                        reads=[Btmq[i2]], writes=[Brsq[i2]])
                tr.emit("vector", lambda e, pq=pq, i2=i2, c=c, dst=dst, gcol=gcol: e.scalar_tensor_tensor(
                    out=dst[:, c, :], in0=P[pq][:, :], scalar=vec[:, gcol:gcol + 1], in1=rsq[i2][:],
                    op0=ALU.mult, op1=ALU.mult),
                    reads=[PB[pq], Brsq[i2], K.Bc], writes=[dbuf])
        if full:
            tr.emit("sync", lambda e, s=s, ts=ts: e.dma_start(out=dr["qT"][:, :, ts], in_=qb_[s][:]),
                    reads=[Bqb[s]], writes=[DB("qT", b)], dsem=s_q[s])
        tr.emit("sync", lambda e, s=s, ts=ts: e.dma_start(out=dr["kT"][:, :, ts], in_=kb_[s][:]),
                reads=[Bkb[s]], writes=[DB("kT", b)], dsem=s_k[s])
        for tt in range(4):
            pb = pbank()
            for kc in range(8):
                tr.emit("tensor", lambda e, kc=kc, pb=pb, tt=tt, s=s: e.matmul(P[pb][:, :], aT[s][:, kc, tt * 128:(tt + 1) * 128], Win[:, kc, 1024:1536],
                                                                               start=(kc == 0), stop=(kc == 7)),
                        reads=[BWin, BaT[s]], writes=[PB[pb]])
            tr.emit("vector", lambda e, pb=pb, tt=tt, s=s: e.tensor_copy(out=vb_[s][:, tt, :], in_=P[pb][:, :]),
                    reads=[PB[pb]], writes=[Bvb[s]])
        tr.emit("sync", lambda e, s=s, b=b: e.dma_start(out=dr["V"][b * TB:(b + 1) * TB, :].rearrange("(t p) n -> p t n", p=128), in_=vb_[s][:]),
                reads=[Bvb[s]], writes=[DB("V", b)], dsem=s_v[s])
        if not full:
            continue
        for c in range(4):
            i2 = c % 2
            pu = proj(1536 + c * 128, aT[s], BaT[s], TB)
            pc = proj(2048 + c * 128, aT[s], BaT[s], TB)
            pg = proj(2560 + c * 128, aT[s], BaT[s], TB)
            tr.emit("scalar", lambda e, pu=pu, i2=i2: e.activation(out=usb[i2][:], in_=P[pu][:, :], func=AF.Copy),
                    reads=[PB[pu]], writes=[Busb[i2]])
            tr.emit("vector", lambda e, c=c, pc=pc, i2=i2: e.tensor_tensor(out=cu[:, c, 2:TB + 2], in0=P[pc][:, :], in1=usb[i2][:], op=ALU.mult),
                    reads=[PB[pc], Busb[i2]], writes=[Bcu])
            tr.emit("gpsimd", lambda e, c=c, i2=i2: e.tensor_scalar(out=acc[i2][:], in0=cu[:, c, 2:TB + 2],
                                                                     scalar1=vec[:, V_CW + 8 + c:V_CW + 9 + c], scalar2=vec[:, V_CB + c:V_CB + c + 1],
                                                                     op0=ALU.mult, op1=ALU.add),
                    reads=[Bcu, K.Bc], writes=[Bacc[i2]])
            tr.emit("vector", lambda e, c=c, i2=i2: e.scalar_tensor_tensor(out=acc[i2][:], in0=cu[:, c, 1:TB + 1], scalar=vec[:, V_CW + 4 + c:V_CW + 5 + c],
                                                                           in1=acc[i2][:], op0=ALU.mult, op1=ALU.add),
                    reads=[Bcu, Bacc[i2], K.Bc], writes=[Bacc[i2]])
            tr.emit("vector", lambda e, c=c, i2=i2: e.scalar_tensor_tensor(out=acc[i2][:], in0=cu[:, c, 0:TB], scalar=vec[:, V_CW + c:V_CW + 1 + c],
                                                                           in1=acc[i2][:], op0=ALU.mult, op1=ALU.add),
                    reads=[Bcu, Bacc[i2], K.Bc], writes=[Bacc[i2]])
            tr.emit("vector", lambda e, c=c, pg=pg, i2=i2: e.tensor_tensor(out=y[:, c, :], in0=P[pg][:, :], in1=acc[i2][:], op=ALU.mult),
                    reads=[PB[pg], Bacc[i2]], writes=[By])
            if b == NB - 1:
                tr.emit("gpsimd", lambda e, c=c: e.tensor_scalar(out=cu[:, c, 0:2], in0=cu[:, c, TB:TB + 2], scalar1=K.is2[:, 0:1],
                                                                  scalar2=None, op0=ALU.mult),
                        reads=[Bcu, K.Bc], writes=[Bcu])
            else:
                tr.emit("gpsimd", lambda e, c=c: e.tensor_copy(out=cu[:, c, 0:2], in_=cu[:, c, TB:TB + 2]),
                        reads=[Bcu], writes=[Bcu])
        rms_stats(K, y, By, 4, TB, sqy, Bsqy, 1.0 / 512, rsy, Brsy, tmp, Btmp, 6, ones_bf)
        for c in range(4):
            tr.emit("vector", lambda e, c=c, s=s: e.scalar_tensor_tensor(out=yn[s][:, c, :], in0=y[:, c, :], scalar=vec[:, V_CVG + c:V_CVG + c + 1],
                                                                         in1=rsy[:], op0=ALU.mult, op1=ALU.mult),
                    reads=[By, Brsy, K.Bc], writes=[Byn[s]])
        tr.emit("sync", lambda e, s=s, ts=ts: e.dma_start(out=dr["yT"][:, :, ts], in_=yn[s][:]),
                reads=[Byn[s]], writes=[DB("yT", b)], dsem=s_y[s])
    _free(st)


def phase_B1(K, L):
    nc, tr, dr, P, PB, DB = K.nc, K.tr, K.dr, K.P, K.PB, K.DB
    st = []
    setup_eps(K, st)
    NKT = 2 * T // 128
    kTa = _alloc(K, st, "s", "kTa", [128, 4, 2 * T], BF16)
    BkT = Buf("kTa")
    Va = _alloc(K, st, "s", "Va", [128, NKT, 512], BF16)
    BVa = Buf("Va")
    qz = [_alloc(K, st, "s", "qz%d" % i, [128, 4, TB], BF16) for i in range(2)]
    Bq = Buf("qz")
    tr.emit("vector", lambda e: e.memset(qz[0][64:128, :, :], 0.0), writes=[Bq])
    tr.emit("vector", lambda e: e.memset(qz[1][0:64, :, :], 0.0), writes=[Bq])
    E = [_alloc(K, st, "s", "E%d" % i, [128, 2, TB], F32) for i in range(3)]
    BE = [Buf("E%d" % i) for i in range(3)]
    Lp = [_alloc(K, st, "s", "Lp%d" % i, [128, 2, TB], BF16) for i in range(2)]
    BLp = [Buf("Lp%d" % i) for i in range(2)]
    Xe = [_alloc(K, st, "s", "Xe%d" % i, [128, 2, TB], BF16) for i in range(2)]
    BXe = [Buf("Xe%d" % i) for i in range(2)]
    Aw = [_alloc(K, st, "s", "Aw%d" % i, [128, 2, TB], BF16) for i in range(2)]
    BAw = [[Buf("Aw%d%d" % (i, ch)) for ch in range(2)] for i in range(2)]
    ablk = _alloc(K, st, "s", "ablk", [128, 4, TB], F32)
    Bab = Buf("ablk")
    sqa = _alloc(K, st, "s", "sqa", [128, 4, TB], BF16)
    Bsqa = Buf("sqa")
    rsa = _alloc(K, st, "s", "rsa", [128, TB], F32)
    Brsa = Buf("rsa")
    tmp = _alloc(K, st, "s", "tmpB", [128, TB], F32)
    Btmp = Buf("tmpB")
    an = [_alloc(K, st, "s", "an%d" % i, [128, 4, TB], BF16) for i in range(2)]
    Ban = [Buf("an%d" % i) for i in range(2)]

    Mneg = K.cstb[:, 0:128]
    M2neg = K.cstb[:, 128:256]
    ones_bf = K.cstb[:, 256:384]
    vec = K.vec

    s_kv = tr.new_dma_sem("kvV")
    s_kk = tr.new_dma_sem("kvK")
    for g in range(4):
        tr.emit("sync", lambda e, g=g: e.dma_start(out=kTa[:, g, :], in_=dr["kT"][:, g, :]),
                reads=[DB("kTall")], writes=[BkT], dsem=s_kk)
    Vv = dr["V"].rearrange("(t p) n -> p t n", p=128)
    for g in range(8):
        tr.emit("sync", lambda e, g=g: e.dma_start(out=Va[:, g * 8:(g + 1) * 8, :], in_=Vv[:, g * 8:(g + 1) * 8, :]),
                reads=[DB("Vall")], writes=[BVa], dsem=s_kv)
    for g in range(4):
        tr.emit("gpsimd", lambda e, g=g: e.tensor_scalar(out=Va[:, g * 8:(g + 1) * 8, :], in0=Va[:, g * 8:(g + 1) * 8, :],
                                                          scalar1=K.is2[:, 0:1], scalar2=None, op0=ALU.mult),
                reads=[BVa, K.Bc], writes=[BVa])

    s_q = tr.new_dma_sem("qb")
    s_an = [tr.new_dma_sem("an%d" % i) for i in range(2)]
    PS = K.PS
    XB = [4, 5]
    OB = [6, 7]
    elem = ["vector", "gpsimd"]
    prs = [slice(0, 64), slice(64, 128)]

    for qb in L.qblocks:
        ts = slice(qb * TB, (qb + 1) * TB)
        tr.emit("sync", lambda e, ts=ts: e.dma_start(out=qz[0][0:64, :, :], in_=dr["qT"][0:64, :, ts]),
                reads=[DB("qT", qb)], writes=[Bq], dsem=s_q)
        tr.emit("sync", lambda e, ts=ts: e.dma_start(out=qz[1][64:128, :, :], in_=dr["qT"][64:128, :, ts]),
                reads=[DB("qT", qb)], writes=[Bq], dsem=s_q)
        own0 = qb * 4
        tiles = [(own0 + j, j) for j in (3, 2, 1, 0)] + [(kt, None) for kt in range(own0 - 1, -1, -1)]
        nst = len(tiles)
        for hp in range(4):
            def zmm(k, hp=hp):
                kt = tiles[k][0]
                for ch in range(2):
                    zb = 2 * (k % 2) + ch
                    tr.emit("tensor", lambda e, ch=ch, kt=kt, zb=zb, hp=hp: e.matmul(P[zb][:, :], kTa[:, hp, kt * 128:(kt + 1) * 128],
                                                                                     qz[ch][:, hp, :], start=True, stop=True),
                            reads=[BkT, Bq], writes=[PB[zb]])

            def expx_and_mult(k):
                sl = k % 2
                s3 = k % 3
                tr.emit("scalar", lambda e, sl=sl: e.activation(out=Xe[sl][:], in_=PS[:, 4:6, :], func=AF.Exp),
                        reads=[PB[4], PB[5]], writes=[BXe[sl]])
                for ch in range(2):
                    tr.emit("vector", lambda e, ch=ch, sl=sl, s3=s3: e.tensor_tensor(out=Aw[sl][:, ch, :], in0=E[s3][:, ch, :], in1=Xe[sl][:, ch, :], op=ALU.mult),
                            reads=[BE[s3], BXe[sl]], writes=[BAw[sl][ch]])

            def av(k, last, hp=hp):
                sl = k % 2
                pkt = tiles[k][0]
                for ch in range(2):
                    h = 2 * hp + ch
                    tr.emit("tensor", lambda e, ch=ch, sl=sl, pkt=pkt, h=h, k=k, last=last, hp=hp: e.matmul(
                        P[OB[ch]][:, :], Va[:, pkt, hp * 128:(hp + 1) * 128], Aw[sl][:, ch, :], start=(k == 0), stop=last),
                        reads=[BVa, BAw[sl][ch]], writes=[PB[OB[ch]]])

            zmm(0)
            for k in range(nst):
                tr.maybe_epoch()
                sl = k % 2
                kt, dj = tiles[k]
                if k + 1 < nst:
                    zmm(k + 1)
                s3 = k % 3
                tr.emit("scalar", lambda e, sl=sl, s3=s3: e.activation(out=E[s3][:], in_=PS[:, 2 * sl:2 * sl + 2, :], func=AF.Exp, scale=0.125),
                        reads=[PB[2 * sl], PB[2 * sl + 1]], writes=[BE[s3]])
                if dj is not None:
                    for ch in range(2):
                        tr.emit("vector", lambda e, ch=ch, s3=s3, dj=dj: e.tensor_tensor(out=E[s3][:, ch, :], in0=E[s3][:, ch, :],
                                                                                          in1=K.cst[:, C_MASK + dj * 512:C_MASK + (dj + 1) * 512], op=ALU.mult),
                                reads=[BE[s3], K.Bc], writes=[BE[s3]])
                if k > 0:
                    expx_and_mult(k - 1)
                tr.emit("scalar", lambda e, sl=sl, s3=s3: e.activation(out=Lp[sl][:], in_=E[s3][:], func=AF.Ln, bias=1.0, scale=1.0),
                        reads=[BE[s3]], writes=[BLp[sl]])
                for ch in range(2):
                    if k > 0:
                        tr.emit("tensor", lambda e, ch=ch, sl=sl: e.matmul(P[XB[ch]][:, :], M2neg, Lp[1 - sl][:, ch, :], start=False, stop=False),
                                reads=[BLp[1 - sl], K.Bc], writes=[PB[XB[ch]]])
                    tr.emit("tensor", lambda e, ch=ch, sl=sl, k=k, nst=nst: e.matmul(P[XB[ch]][:, :], Mneg, Lp[sl][:, ch, :], start=(k == 0), stop=(k == nst - 1)),
                            reads=[BLp[sl], K.Bc], writes=[PB[XB[ch]]])
                if k > 0:
                    av(k - 1, False)
            expx_and_mult(nst - 1)
            av(nst - 1, True)
            for ch in range(2):
                tr.emit("vector", lambda e, ch=ch, hp=hp: e.tensor_copy(out=ablk[prs[ch], hp, :], in_=P[OB[ch]][prs[ch], :]),
                        reads=[PB[OB[ch]]], writes=[Bab])
        s = qb % 2
        rms_stats(K, ablk, Bab, 4, TB, sqa, Bsqa, 1.0 / 512, rsa, Brsa, tmp, Btmp, 0, ones_bf)
        for c in range(4):
            tr.emit("vector", lambda e, c=c, s=s: e.scalar_tensor_tensor(out=an[s][:, c, :], in0=ablk[:, c, :], scalar=vec[:, V_ATT + c:V_ATT + c + 1],
                                                                         in1=rsa[:], op0=ALU.mult, op1=ALU.mult),
                    reads=[Bab, Brsa, K.Bc], writes=[Ban[s]])
        tr.emit("sync", lambda e, s=s, ts=ts: e.dma_start(out=dr["attnT"][:, :, ts], in_=an[s][:]),
                reads=[Ban[s]], writes=[DB("attnT", qb)], dsem=s_an[s])
    _free(st)


def phase_B2(K, L):
    layer = L.layer
    nc, tr, dr, P, PB, DB = K.nc, K.tr, K.dr, K.P, K.PB, K.DB
    st = []
    setup_eps(K, st)
    moe = layer == 1
    hb = _alloc(K, st, "s", "hbB", [128, 8, TB], F32)
    Bhb = Buf("hbB")
    an = _alloc(K, st, "s", "anB", [128, 4, TB], BF16)
    Ban = Buf("anB")
    yn = _alloc(K, st, "s", "ynB", [128, 4, TB], BF16)
    Byn = Buf("ynB")
    Wsq = _alloc(K, st, "s", "Wsq", [128, 8, D], BF16)
    BWsq = Buf("Wsq")
    Wp = _alloc(K, st, "s", "Wp", [128, 2, D], BF16)
    BWp = Buf("Wp")
    pTb = _alloc(K, st, "s", "pTb", [128, 2, TB], BF16)
    BpT = Buf("pTb")
    sq = _alloc(K, st, "s", "sqB", [128, 8, TB], BF16)
    Bsq = Buf("sqB")
    tmp = _alloc(K, st, "s", "tmpC", [128, TB], F32)
    Btmp = Buf("tmpC")
    fT = _alloc(K, st, "s", "fT", [128, 8, TB], BF16)
    BfT = Buf("fT")
    act = _alloc(K, st, "s", "act", [128, NFF, TB], BF16)
    Bact = Buf("act")
    W13 = [[_alloc(K, st, "s", "W%d_%d" % (w, i), [128, 8, 256], BF16) for i in range(2)] for w in range(2)]
    BW13 = [[Buf("W%d_%d" % (w, i)) for i in range(2)] for w in range(2)]
    W2 = _alloc(K, st, "s", "W2", [128, NFF, D], BF16)
    BW2 = [[Buf("W2_%d_%d" % (hf, i)) for i in range(7)] for hf in range(2)]
    sil = [_alloc(K, st, "s", "sil%d" % i, [128, TB], F32) for i in range(2)]
    Bsil = [Buf("sil%d" % i) for i in range(2)]
    if moe:
        feT = [_alloc(K, st, "s", "feT%d" % i, [128, 8, TB], BF16) for i in range(2)]
        BfeT = [Buf("feT%d" % i) for i in range(2)]
        f32t = [_alloc(K, st, "s", "f32t%d" % i, [128, 8, 128], F32) for i in range(2)]
        Bf32 = [Buf("f32t%d" % i) for i in range(2)]
        Wr = _alloc(K, st, "s", "Wr", [128, 8, NE], F32)
        BWr = Buf("Wr")
        lg = _alloc(K, st, "s", "lg", [128, 4, NE], F32)
        top = _alloc(K, st, "s", "top", [128, 4, 8], F32)
        nm1 = _alloc(K, st, "s", "nm1", [128, 4], F32)
        msk = _alloc(K, st, "s", "msk", [128, 4, NE], F32)
        ex = _alloc(K, st, "s", "ex", [128, 4, NE], F32)
        den = _alloc(K, st, "s", "den", [128, 4], F32)
        comb = _alloc(K, st, "s", "comb", [128, 4, NE], F32)
        Brt = Buf("router")
        dg = [_alloc(K, st, "s", "dg%d" % i, [128, 128], F32) for i in range(2)]
        Bdg = [Buf("dg%d" % i) for i in range(2)]

    ones_bf = K.cstb[:, 256:384]
    ones_f = K.cst[:, C_ONES:C_ONES + 128]
    ident_f = K.cst[:, C_IDENT:C_IDENT + 128]
    vec = K.vec

    s_h = tr.new_dma_sem("hB")
    s_an = tr.new_dma_sem("anB")
    s_yn = tr.new_dma_sem("ynB")
    s_p = tr.new_dma_sem("pTb")
    s_ws = tr.new_dma_sem("Wsq")
    s_wg = tr.new_dma_sem("Wgt")
    s_wp = tr.new_dma_sem("Wp")
    s_w13 = [[tr.new_dma_sem("W%d_%d" % (w, i)) for i in range(2)] for w in range(2)]
    s_w2 = [[tr.new_dma_sem("W2_%d_%d" % (hf, i)) for i in range(7)] for hf in range(2)]

    hv = L.hsrc.rearrange("(c p) t -> p c t", p=128)
    hov = L.hdst.rearrange("(c p) t -> p c t", p=128)
    wov = L.w_o.rearrange("(c p) n -> p c n", p=128)
    wgv = L.wg.rearrange("(c p) n -> p c n", p=128)
    wpv = L.wp.rearrange("(c p) n -> p c n", p=128)
    pv = L.pT.rearrange("(c p) t -> p c t", p=128)

    tr.emit("gpsimd", lambda e: e.dma_start(out=Wp[:], in_=wpv), writes=[BWp], dsem=s_wp)
    if moe:
        s_wr = tr.new_dma_sem("Wr")
        tr.emit("sync", lambda e: e.dma_start(out=Wr[:], in_=dr["router_w"].rearrange("(c p) n -> p c n", p=128)),
                writes=[BWr], dsem=s_wr)

    pr = [0]

    def pbank():
        pr[0] = (pr[0] + 1) % 6
        return 2 + pr[0]

    def load_sq(wview):
        for g in range(2):
            tr.emit("gpsimd", lambda e, g=g: e.dma_start(out=Wsq[:, g * 4:(g + 1) * 4, :], in_=wview[:, g * 4:(g + 1) * 4, :]),
                    writes=[BWsq], dsem=s_ws)

    def norm_to(gcol0, dst, dbuf):
        rms_stats(K, hb, Bhb, 8, TB, sq, Bsq, 1.0 / D, P[1], PB[1], tmp, Btmp, 0, ones_bf)
        for c in range(8):
            tr.emit("vector", lambda e, c=c: e.scalar_tensor_tensor(out=dst[:, c, :], in0=hb[:, c, :], scalar=vec[:, gcol0 + c:gcol0 + c + 1],
                                                                     in1=P[1][:, :], op0=ALU.mult, op1=ALU.mult),
                    reads=[Bhb, PB[1], K.Bc], writes=[dbuf])

    def wviews(e_idx):
        return (L.w1[e_idx].rearrange("(c p) n -> p c n", p=128), L.w3[e_idx].rearrange("(c p) n -> p c n", p=128),
                L.w2[e_idx].rearrange("(c p) n -> p c n", p=128))

    def load_w13(e_idx, grp):
        w1v, w3v, _ = wviews(e_idx)
        sl = grp % 2
        cs = slice(grp * 256, (grp + 1) * 256)
        tr.emit("gpsimd", lambda e, sl=sl, cs=cs: e.dma_start(out=W13[0][sl][:], in_=w1v[:, :, cs]), writes=[BW13[0][sl]], dsem=s_w13[0][sl])
        tr.emit("gpsimd", lambda e, sl=sl, cs=cs: e.dma_start(out=W13[1][sl][:], in_=w3v[:, :, cs]), writes=[BW13[1][sl]], dsem=s_w13[1][sl])

    def load_w2(e_idx, hf, g):
        w2v = wviews(e_idx)[2]
        tr.emit("gpsimd", lambda e, g=g, hf=hf: e.dma_start(out=W2[:, g * 4:(g + 1) * 4, hf * 512:(hf + 1) * 512],
                                                             in_=w2v[:, g * 4:(g + 1) * 4, hf * 512:(hf + 1) * 512]),
                writes=[BW2[hf][g]], dsem=s_w2[hf][g])

    def prefetch_first(e_idx):
        load_w13(e_idx, 0)
        load_w13(e_idx, 1)
        for hf in range(2):
            for g in range(7):
                load_w2(e_idx, hf, g)

    def stage1(e_idx, xin, xbuf, xg, xgbuf):
        pr[0] = 5
        for grp in range(14):
            tr.maybe_epoch()
            sl = grp % 2
            for j in range(2):
                ff = grp * 2 + j
                pg = pbank()
                pu = pbank()
                for kc in range(8):
                    tr.emit("tensor", lambda e, kc=kc, pg=pg, sl=sl, j=j: e.matmul(P[pg][:, :], W13[0][sl][:, kc, j * 128:(j + 1) * 128], xg[:, kc, :],
                                                                                   start=(kc == 0), stop=(kc == 7)),
                            reads=[BW13[0][sl], xgbuf], writes=[PB[pg]])
                for kc in range(8):
                    tr.emit("tensor", lambda e, kc=kc, pu=pu, sl=sl, j=j: e.matmul(P[pu][:, :], W13[1][sl][:, kc, j * 128:(j + 1) * 128], xin[:, kc, :],
                                                                                   start=(kc == 0), stop=(kc == 7)),
                            reads=[BW13[1][sl], xbuf], writes=[PB[pu]])
                i2 = ff % 2
                tr.emit("scalar", lambda e, pg=pg, i2=i2: e.activation(out=sil[i2][:], in_=P[pg][:, :], func=AF.Silu),
                        reads=[PB[pg]], writes=[Bsil[i2]])
                tr.emit("vector", lambda e, pu=pu, i2=i2, ff=ff: e.tensor_tensor(out=act[:, ff, :], in0=P[pu][:, :], in1=sil[i2][:], op=ALU.mult),
                        reads=[PB[pu], Bsil[i2]], writes=[Bact])
            if grp + 2 < 14:
                load_w13(e_idx, grp + 2)

    def stage2_pass(hf, nxt):
        for g in range(7):
            for f4 in range(4):
                ff = g * 4 + f4
                for o4 in range(4):
                    oc = hf * 4 + o4
                    tr.emit("tensor", lambda e, ff=ff, o4=o4, oc=oc: e.matmul(P[4 + o4][:, :], W2[:, ff, oc * 128:(oc + 1) * 128], act[:, ff, :],
                                                                              start=(ff == 0), stop=(ff == NFF - 1)),
                            reads=[BW2[hf][g], Bact], writes=[PB[4 + o4]])
            if nxt is not None:
                load_w2(nxt, hf, g)
        for o4 in range(4):
            oc = hf * 4 + o4
            tr.emit("vector", lambda e, o4=o4, oc=oc: e.tensor_tensor(out=hb[:, oc, :], in0=P[4 + o4][:, :], in1=hb[:, oc, :], op=ALU.add),
                    reads=[PB[4 + o4], Bhb], writes=[Bhb])

    def prep_fe(ei):
        for tt in range(4):
            i2 = tt % 2
            tr.emit("vector", lambda e, tt=tt, i2=i2, ei=ei: e.tensor_scalar(out=dg[i2][:], in0=ident_f, scalar1=comb[:, tt, ei:ei + 1],
                                                                             scalar2=None, op0=ALU.mult),
                    reads=[Brt, K.Bc], writes=[Bdg[i2]])
            tr.emit("tensor", lambda e, tt=tt, i2=i2: e.matmul(P[0][:, tt * 128:(tt + 1) * 128], ones_f, dg[i2][:], start=True, stop=True),
                    reads=[Bdg[i2], K.Bc], writes=[PB[0]])
        fb = ei % 2
        for c in range(8):
            tr.emit("vector", lambda e, c=c, fb=fb: e.tensor_tensor(out=feT[fb][:, c, :], in0=P[0][:, :], in1=fT[:, c, :], op=ALU.mult),
                    reads=[PB[0], BfT], writes=[BfeT[fb]])

    def ffn_block(experts):
        for n, ei in enumerate(experts):
            nxt = experts[n + 1] if n + 1 < len(experts) else None
            if moe:
                if n == 0:
                    prep_fe(ei)
                stage1(ei, feT[ei % 2], BfeT[ei % 2], fT, BfT)
            else:
                stage1(ei, fT, BfT, fT, BfT)
            if nxt is not None:
                load_w13(nxt, 0)
                load_w13(nxt, 1)
            stage2_pass(0, nxt)
            if moe and nxt is not None:
                prep_fe(nxt)
            stage2_pass(1, nxt)

    load_sq(wov)
    Wgt = act[:, 0:16, :].rearrange("p (a b) c -> p a (b c)", b=2)

    for b in L.blocks:
        ts = slice(b * TB, (b + 1) * TB)
        tsp = slice((b - L.p_off) * TB, (b - L.p_off + 1) * TB)
        tso = slice((b - L.o_off) * TB, (b - L.o_off + 1) * TB)
        tr.emit("sync", lambda e, ts=ts: e.dma_start(out=hb[:], in_=hv[:, :, ts]), reads=[DB("hsrc", b)], writes=[Bhb], dsem=s_h)
        tr.emit("sync", lambda e, ts=ts: e.dma_start(out=an[:], in_=dr["attnT"][:, :, ts]), reads=[DB("attnT", b)], writes=[Ban], dsem=s_an)
        tr.emit("sync", lambda e, ts=ts: e.dma_start(out=yn[:], in_=dr["yT"][:, :, ts]), reads=[DB("yT", b)], writes=[Byn], dsem=s_yn)
        tr.emit("gpsimd", lambda e, tsp=tsp: e.dma_start(out=pTb[:], in_=pv[:, :, tsp]), writes=[BpT], dsem=s_p)
        prefetch_first(0)
        for oc in range(8):
            pb = pbank()
            for c in range(4):
                tr.emit("tensor", lambda e, c=c, pb=pb, oc=oc: e.matmul(P[pb][:, :], Wsq[:, c, oc * 128:(oc + 1) * 128], an[:, c, :],
                                                                        start=(c == 0), stop=False),
                        reads=[BWsq, Ban], writes=[PB[pb]])
            for c in range(4):
                tr.emit("tensor", lambda e, c=c, pb=pb, oc=oc: e.matmul(P[pb][:, :], Wsq[:, 4 + c, oc * 128:(oc + 1) * 128], yn[:, c, :],
                                                                        start=False, stop=(c == 3)),
                        reads=[BWsq, Byn], writes=[PB[pb]])
            tr.emit("vector", lambda e, pb=pb, oc=oc: e.tensor_tensor(out=hb[:, oc, :], in0=P[pb][:, :], in1=hb[:, oc, :], op=ALU.add),
                    reads=[PB[pb], Bhb], writes=[Bhb])
        norm_to(V_FFN, fT, BfT)
        if not moe:
            ffn_block([0])
        else:
            for tt in range(4):
                i2 = tt % 2
                for c in range(8):
                    tr.emit("vector", lambda e, c=c, tt=tt, i2=i2: e.scalar_tensor_tensor(
                        out=f32t[i2][:, c, :], in0=hb[:, c, tt * 128:(tt + 1) * 128], scalar=vec[:, V_FFN + c:V_FFN + c + 1],
                        in1=P[1][:, tt * 128:(tt + 1) * 128], op0=ALU.mult, op1=ALU.mult),
                        reads=[Bhb, PB[1], K.Bc], writes=[Bf32[i2]])
                pb = pbank()
                for c in range(8):
                    tr.emit("tensor", lambda e, c=c, pb=pb, i2=i2: e.matmul(P[pb][:, 0:NE], f32t[i2][:, c, :], Wr[:, c, :], start=(c == 0), stop=(c == 7)),
                            reads=[Bf32[i2], BWr], writes=[PB[pb]])
                tr.emit("vector", lambda e, pb=pb, tt=tt: e.tensor_copy(out=lg[:, tt, :], in_=P[pb][:, 0:NE]), reads=[PB[pb]], writes=[Brt])
                tr.emit("vector", lambda e, tt=tt: e.max(out=top[:, tt, :], in_=lg[:, tt, :]), reads=[Brt], writes=[Brt])
                tr.emit("vector", lambda e, tt=tt: e.tensor_scalar(out=nm1[:, tt:tt + 1], in0=top[:, tt, 0:1], scalar1=-1.0, scalar2=None, op0=ALU.mult),
                        reads=[Brt], writes=[Brt])
                tr.emit("vector", lambda e, tt=tt: e.tensor_scalar(out=msk[:, tt, :], in0=lg[:, tt, :], scalar1=top[:, tt, 1:2], scalar2=None, op0=ALU.is_ge),
                        reads=[Brt], writes=[Brt])
                tr.emit("scalar", lambda e, tt=tt: e.activation(out=ex[:, tt, :], in_=lg[:, tt, :], func=AF.Exp, bias=nm1[:, tt:tt + 1], scale=1.0),
                        reads=[Brt], writes=[Brt])
                tr.emit("vector", lambda e, tt=tt: e.tensor_tensor(out=ex[:, tt, :], in0=ex[:, tt, :], in1=msk[:, tt, :], op=ALU.mult),
                        reads=[Brt], writes=[Brt])
                tr.emit("vector", lambda e, tt=tt: e.tensor_reduce(out=den[:, tt:tt + 1], in_=ex[:, tt, :], axis=mybir.AxisListType.X, op=ALU.add),
                        reads=[Brt], writes=[Brt])
                tr.emit("vector", lambda e, tt=tt: e.reciprocal(out=den[:, tt:tt + 1], in_=den[:, tt:tt + 1]), reads=[Brt], writes=[Brt])
                tr.emit("vector", lambda e, tt=tt: e.tensor_scalar(out=comb[:, tt, :], in0=ex[:, tt, :], scalar1=den[:, tt:tt + 1], scalar2=None, op0=ALU.mult),
                        reads=[Brt], writes=[Brt])
            ffn_block(list(range(NE)))
        for g in range(2):
            tr.emit("gpsimd", lambda e, g=g: e.dma_start(out=Wgt[:, g * 4:(g + 1) * 4, :], in_=wgv[:, g * 4:(g + 1) * 4, :]),
                    writes=[Bact], dsem=s_wg)
        norm_to(V_PLE, fT, BfT)
        for oc in range(8):
            pg = pbank()
            pp = pbank()
            for c in range(8):
                tr.emit("tensor", lambda e, c=c, pg=pg, oc=oc: e.matmul(P[pg][:, :], Wgt[:, c, oc * 128:(oc + 1) * 128], fT[:, c, :],
                                                                        start=(c == 0), stop=(c == 7)),
                        reads=[Bact, BfT], writes=[PB[pg]])
            for c in range(2):
                tr.emit("tensor", lambda e, c=c, pp=pp, oc=oc: e.matmul(P[pp][:, :], Wp[:, c, oc * 128:(oc + 1) * 128], pTb[:, c, :],
                                                                        start=(c == 0), stop=(c == 1)),
                        reads=[BWp, BpT], writes=[PB[pp]])
            i2 = oc % 2
            tr.emit("scalar", lambda e, pg=pg, i2=i2: e.activation(out=sil[i2][:], in_=P[pg][:, :], func=AF.Sigmoid),
                    reads=[PB[pg]], writes=[Bsil[i2]])
            tr.emit("vector", lambda e, pp=pp, i2=i2: e.tensor_tensor(out=sil[i2][:], in0=P[pp][:, :], in1=sil[i2][:], op=ALU.mult),
                    reads=[PB[pp], Bsil[i2]], writes=[Bsil[i2]])
            tr.emit("gpsimd", lambda e, oc=oc, i2=i2: e.tensor_tensor(out=hb[:, oc, :], in0=hb[:, oc, :], in1=sil[i2][:], op=ALU.add),
                    reads=[Bhb, Bsil[i2]], writes=[Bhb])
        tr.emit("sync", lambda e, tso=tso: e.dma_start(out=hov[:, :, tso], in_=hb[:]), reads=[Bhb], writes=[DB("hdst", b)], dsem=s_h)
    _free(st)


def _consts():
    c = np.zeros((128, NCONST), np.float32)
    jp = np.arange(128)[:, None]
    j = np.arange(128)[None, :]
    c[:, C_MNEG:C_MNEG + 128] = np.where(jp >= j, -1.0, 0.0)
    c[:, C_M2NEG:C_M2NEG + 128] = np.where(jp < j, -1.0, 0.0)
    c[:, C_ONES:C_ONES + 128] = 1.0
    c[0:64, C_BLK:C_BLK + 64] = 1.0
    c[64:128, C_BLK + 64:C_BLK + 128] = 1.0
    c[:, C_IDENT:C_IDENT + 128] = np.eye(128, dtype=np.float32)
    t = np.arange(512)[None, :]
    for dj in range(4):
        s = dj * 128 + np.arange(128)[:, None]
        c[:, C_MASK + dj * 512:C_MASK + (dj + 1) * 512] = (t > s).astype(np.float32)
    return c


def _vecs(i, mix_norm_g, ffn_norm_g, ple_norm_g, attn_out_g, conv_out_g, conv_w, conv_b, q_norm_g, k_norm_g):
    v = np.zeros((128, NV), np.float32)
    v[:, V_MIX:V_MIX + 8] = mix_norm_g[i].reshape(8, 128).T
    v[:, V_FFN:V_FFN + 8] = ffn_norm_g[i].reshape(8, 128).T
    v[:, V_PLE:V_PLE + 8] = ple_norm_g[i].reshape(8, 128).T
    v[:, V_ATT:V_ATT + 4] = attn_out_g[i].reshape(4, 128).T
    v[:, V_CVG:V_CVG + 4] = conv_out_g[i].reshape(4, 128).T
    for k in range(3):
        v[:, V_CW + 4 * k:V_CW + 4 * k + 4] = conv_w[i, k].reshape(4, 128).T
    v[:, V_CB:V_CB + 4] = conv_b[i].reshape(4, 128).T
    v[:, V_QG] = np.tile(q_norm_g[i], 2)
    v[:, V_KG] = np.tile(k_norm_g[i], 2)
    return v


_PROG = []


def kernel(x, p, mix_norm_g, w_in, q_norm_g, k_norm_g, conv_w, conv_b, attn_out_g, conv_out_g, w_o,
           ffn_norm_g, dense_w1, dense_w3, dense_w2, router_w, moe_w1, moe_w3, moe_w2,
           ple_norm_g, ple_gate_w, ple_proj_w):
    f = lambda a: np.ascontiguousarray(np.asarray(a, dtype=np.float32))
    x, p = f(x), f(p)
    consts = _consts()
    cores = list(range(NCORES))
    args = (f(mix_norm_g), f(ffn_norm_g), f(ple_norm_g), f(attn_out_g), f(conv_out_g), f(conv_w), f(conv_b), f(q_norm_g), f(k_norm_g))
    vecs = np.ascontiguousarray(np.concatenate([_vecs(0, *args), _vecs(1, *args)], axis=1))
    shared = {"consts": consts, "vecs": vecs, "w_in": f(w_in), "w_o": f(w_o), "ple_gate_w": f(ple_gate_w), "ple_proj_w": f(ple_proj_w),
              "dw1": f(dense_w1), "dw3": f(dense_w3), "dw2": f(dense_w2), "mw1": f(moe_w1[0]), "mw3": f(moe_w3[0]), "mw2": f(moe_w2[0]),
              "router_w": f(router_w[0])}
    in_maps = []
    for c in cores:
        b, hf = c // 2, c % 2
        if hf == 1:
            xT = np.ascontiguousarray(x[b].T)
            pT0 = np.ascontiguousarray(p[0, b].T)
        else:
            xT = np.ascontiguousarray(np.concatenate([np.zeros((D, T), np.float32), x[b, :T].T], axis=1))
            pT0 = np.ascontiguousarray(np.concatenate([np.zeros((256, T), np.float32), p[0, b, :T].T], axis=1))
        pT1 = np.ascontiguousarray(p[1, b, hf * T:(hf + 1) * T].T)
        d = dict(shared)
        d.update({"is2": np.full((128, 1), float(hf), np.float32), "xT": xT, "pT0": pT0, "pT1": pT1})
        in_maps.append(d)
    if not _PROG:
        _PROG.append(build_fused())
    res = run_bass_kernel_spmd(_PROG[0], in_maps, core_ids=cores).results
    out = np.empty((4, 2 * T, D), np.float32)
    for c in cores:
        out[c // 2, (c % 2) * T:(c % 2 + 1) * T, :] = np.asarray(res[c]["hTo"]).T
    return out
```
